# Optimizing a Trainium2 kernel written in Bass

```python
import jax, jax.numpy as jnp
from jax import lax
import numpy as np

D_MODEL = 1024
BATCH = 8
SEQ = 4096
DEPTH = 1

GRID_W = 64
CTX_LEN = 256
EPS = 1e-6
ROPE_THETA = 10000.0
Q_BLOCK = 128

GQA_HEADS = 8
GQA_KV_HEADS = 2
GQA_GROUP = GQA_HEADS // GQA_KV_HEADS
GQA_HEAD_DIM = 128
GQA_Q_W = GQA_HEADS * GQA_HEAD_DIM
GQA_KV_W = GQA_KV_HEADS * GQA_HEAD_DIM

MLA_HEADS = 8
MLA_Q_RANK = 256
MLA_KV_RANK = 128
MLA_NOPE_DIM = 128
MLA_ROPE_DIM = 64
MLA_V_DIM = 128
MLA_QK_DIM = MLA_NOPE_DIM + MLA_ROPE_DIM
MLA_KVA_W = MLA_KV_RANK + MLA_ROPE_DIM
MLA_V_W = MLA_HEADS * MLA_V_DIM

N_BRANCH = 2
OFF_GQA_K = GQA_Q_W
OFF_GQA_V = OFF_GQA_K + GQA_KV_W
OFF_MLA_QA = OFF_GQA_V + GQA_KV_W
OFF_MLA_KVA = OFF_MLA_QA + MLA_Q_RANK
OFF_GATE = OFF_MLA_KVA + MLA_KVA_W
IN_WIDTH = OFF_GATE + N_BRANCH * D_MODEL

N_EXPERTS = 32
TOP_K = 4
D_EXPERT = 1024
SWIGLU_LIMIT = 7.0
SWIGLU_ALPHA = 1.702
EXPERT_BLOCK = 256

kernel_name = 'hybrid_gqa_mla_moe_dit_layer'


def _rms_norm(x, g):
    xf = x.astype(jnp.float32)
    y = xf * lax.rsqrt(jnp.mean(xf * xf, axis=-1, keepdims=True) + EPS)
    return (y * g.astype(jnp.float32)).astype(x.dtype)


def _modulate(x, g, shift, scale):
    return _rms_norm(x, g) * (1 + scale) + shift


def _rope_1d(x, pos):
    d = x.shape[-1]
    inv_freq = ROPE_THETA ** (-jnp.arange(0, d, 2, dtype=jnp.float32) / d)
    ang = pos.astype(jnp.float32)[:, None] * inv_freq[None, :]
    cos = jnp.cos(ang)[None, :, None, :].astype(x.dtype)
    sin = jnp.sin(ang)[None, :, None, :].astype(x.dtype)
    x1, x2 = x[..., : d // 2], x[..., d // 2:]
    return jnp.concatenate([x1 * cos - x2 * sin, x1 * sin + x2 * cos], axis=-1)


def _axial_rope(x, row, col):
    half = x.shape[-1] // 2
    return jnp.concatenate([_rope_1d(x[..., :half], row), _rope_1d(x[..., half:], col)], axis=-1)


def _rope_tail(x, pos):
    return jnp.concatenate([x[..., :MLA_NOPE_DIM], _axial_rope(x[..., MLA_NOPE_DIM:], *pos)], axis=-1)


def _blocked_attention(q, k, v, scale):
    b, lq, kvh, g, dk = q.shape
    nb = lq // Q_BLOCK
    qb = jnp.moveaxis(q.reshape(b, nb, Q_BLOCK, kvh, g, dk), 1, 0)

    def one_block(q_blk):
        s = jnp.einsum('bqkgd,btkd->bkgqt', q_blk, k, preferred_element_type=jnp.float32) * scale
        p = jax.nn.softmax(s, axis=-1).astype(v.dtype)
        return jnp.einsum('bkgqt,btkv->bqkgv', p, v)

    o = lax.map(one_block, qb)
    return jnp.moveaxis(o, 0, 1).reshape(b, lq, kvh * g * v.shape[-1])


def _split_proj(proj):
    return (proj[..., :OFF_GQA_K], proj[..., OFF_GQA_K:OFF_GQA_V], proj[..., OFF_GQA_V:OFF_MLA_QA],
            proj[..., OFF_MLA_QA:OFF_MLA_KVA], proj[..., OFF_MLA_KVA:OFF_GATE], proj[..., OFF_GATE:])


def _gqa_q(pq, q_norm_g, pos):
    b, l, _ = pq.shape
    q = _rms_norm(pq.reshape(b, l, GQA_HEADS, GQA_HEAD_DIM), q_norm_g)
    if pos is not None:
        q = _axial_rope(q, *pos)
    return q.reshape(b, l, GQA_KV_HEADS, GQA_GROUP, GQA_HEAD_DIM)


def _gqa_kv(pk, pv, k_norm_g, pos):
    b, l, _ = pk.shape
    k = _rms_norm(pk.reshape(b, l, GQA_KV_HEADS, GQA_HEAD_DIM), k_norm_g)
    if pos is not None:
        k = _axial_rope(k, *pos)
    return k, pv.reshape(b, l, GQA_KV_HEADS, GQA_HEAD_DIM)


def _mla_q(pqa, p, pos):
    b, l, _ = pqa.shape
    q = (_rms_norm(pqa, p['mla_q_a_norm']) @ p['mla_w_qb']).reshape(b, l, MLA_HEADS, MLA_QK_DIM)
    q = _rms_norm(q, p['mla_q_norm'])
    if pos is not None:
        q = _rope_tail(q, pos)
    return q[:, :, :, None, :]


def _mla_kv(pkva, p, pos):
    b, l, _ = pkva.shape
    c_kv, k_rope = pkva[..., :MLA_KV_RANK], pkva[..., MLA_KV_RANK:]
    kv = (_rms_norm(c_kv, p['mla_kv_a_norm']) @ p['mla_w_kvb']).reshape(b, l, MLA_HEADS, MLA_NOPE_DIM + MLA_V_DIM)
    k_nope, v = kv[..., :MLA_NOPE_DIM], kv[..., MLA_NOPE_DIM:]
    k_rope = jnp.broadcast_to(k_rope[:, :, None, :], (b, l, MLA_HEADS, MLA_ROPE_DIM))
    k = _rms_norm(jnp.concatenate([k_nope, k_rope], axis=-1), p['mla_k_norm'])
    if pos is not None:
        k = _rope_tail(k, pos)
    return k, v


def _merge(o_a, o_b, gate_logits, p):
    g = jax.nn.sigmoid(gate_logits)
    y = g[..., :D_MODEL] * (o_a @ p['w_o_gqa']) + g[..., D_MODEL:] * (o_b @ p['w_o_mla'])
    return y @ p['w_out']


def _mixer(h, hc, p, pos, with_ctx_queries):
    q_a, k_a, v_a, qa_b, kva_b, gate_l = _split_proj(h @ p['w_in'])
    qc_a, kc_a_raw, vc_a_raw, qac_b, kvac_b, gate_c = _split_proj(hc @ p['w_in'])
    kc_a, vc_a = _gqa_kv(kc_a_raw, vc_a_raw, p['gqa_k_norm'], None)
    kc_b, vc_b = _mla_kv(kvac_b, p, None)
    kl_a, vl_a = _gqa_kv(k_a, v_a, p['gqa_k_norm'], pos)
    kl_b, vl_b = _mla_kv(kva_b, p, pos)
    o_a = _blocked_attention(_gqa_q(q_a, p['gqa_q_norm'], pos),
                             jnp.concatenate([kc_a, kl_a], axis=1), jnp.concatenate([vc_a, vl_a], axis=1),
                             GQA_HEAD_DIM ** -0.5)
    o_b = _blocked_attention(_mla_q(qa_b, p, pos),
                             jnp.concatenate([kc_b, kl_b], axis=1), jnp.concatenate([vc_b, vl_b], axis=1),
                             MLA_QK_DIM ** -0.5)
    y = _merge(o_a, o_b, gate_l, p)
    if with_ctx_queries:
        oc_a = _blocked_attention(_gqa_q(qc_a, p['gqa_q_norm'], None), kc_a, vc_a, GQA_HEAD_DIM ** -0.5)
        oc_b = _blocked_attention(_mla_q(qac_b, p, None), kc_b, vc_b, MLA_QK_DIM ** -0.5)
        return y, _merge(oc_a, oc_b, gate_c, p)
    return y, None


def _moe(h, p):
    b, l, d = h.shape
    t = h.reshape(-1, d)
    n_tok = t.shape[0]
    logits = (t @ p['router_w'] + p['router_b']).astype(jnp.float32)
    top_val, top_idx = lax.top_k(logits, TOP_K)
    gates = jax.nn.softmax(top_val, axis=-1)
    n_assign = n_tok * TOP_K
    e = top_idx.reshape(-1)
    tok = jnp.repeat(jnp.arange(n_tok, dtype=jnp.int32), TOP_K)
    order = jnp.argsort(e)
    e_s, tok_s, g_s = e[order], tok[order], gates.reshape(-1)[order]
    counts = jnp.bincount(e, length=N_EXPERTS)
    starts = jnp.cumsum(counts) - counts
    padded = (counts + EXPERT_BLOCK - 1) // EXPERT_BLOCK * EXPERT_BLOCK
    pad_starts = jnp.cumsum(padded) - padded
    pad_ends = pad_starts + padded
    dest = pad_starts[e_s] + (jnp.arange(n_assign, dtype=jnp.int32) - starts[e_s])
    n_blocks = -(-n_assign // EXPERT_BLOCK) + N_EXPERTS
    buf = jnp.zeros((n_blocks * EXPERT_BLOCK, d), h.dtype).at[dest].set(t[tok_s])
    block_e = jnp.minimum(jnp.searchsorted(pad_ends, jnp.arange(n_blocks) * EXPERT_BLOCK, side='right'),
                          N_EXPERTS - 1)

    def expert_block(args):
        xb, eid = args
        gu = xb @ p['expert_w1'][eid] + p['expert_b1'][eid]
        x_glu, x_lin = gu[..., :D_EXPERT], gu[..., D_EXPERT:]
        x_glu = jnp.minimum(x_glu, SWIGLU_LIMIT)
        x_lin = jnp.clip(x_lin, -SWIGLU_LIMIT, SWIGLU_LIMIT)
        act = x_glu * jax.nn.sigmoid(SWIGLU_ALPHA * x_glu) * (x_lin + 1)
        return act @ p['expert_w2'][eid] + p['expert_b2'][eid]

    y = lax.map(expert_block, (buf.reshape(n_blocks, EXPERT_BLOCK, d), block_e)).reshape(-1, d)
    y = y[dest] * g_s[:, None].astype(h.dtype)
    return jax.ops.segment_sum(y, tok_s, num_segments=n_tok).reshape(b, l, d)


def _layer(x, xc, c, c_ctx, p, pos, update_ctx):
    mod = (jax.nn.silu(c) @ p['ada_w'] + p['ada_b'])[:, None, :]
    mod_c = jax.nn.silu(c_ctx) @ p['ada_w'] + p['ada_b']
    sh1, sc1, g1, sh2, sc2, g2 = jnp.split(mod, 6, axis=-1)
    csh1, csc1, cg1, csh2, csc2, cg2 = jnp.split(mod_c, 6, axis=-1)
    h = _modulate(x, p['norm_mix'], sh1, sc1)
    hc = _modulate(xc, p['norm_mix'], csh1, csc1)
    y, yc = _mixer(h, hc, p, pos, update_ctx)
    x = x + g1 * y
    x = x + g2 * _moe(_modulate(x, p['norm_ffn'], sh2, sc2), p)
    if update_ctx:
        xc = xc + cg1 * yc
        xc = xc + cg2 * _moe(_modulate(xc, p['norm_ffn'], csh2, csc2), p)
    return x, xc


def setup_inputs(seed: int = 0) -> dict:
    key = jax.random.key(seed)
    ks = jax.random.split(key, 26)
    f32 = jnp.float32
    D = D_MODEL

    def nrm(k, shape, scale):
        return jax.random.normal(k, shape, f32) * scale

    def gain(k, shape):
        return 1.0 + 0.02 * jax.random.normal(k, shape, f32)

    return {
        'x': nrm(ks[0], (BATCH, SEQ, D), 1.0),
        'c': nrm(ks[1], (BATCH, D), 1.0),
        'ctx': nrm(ks[2], (BATCH, CTX_LEN, D), 1.0),
        'c_ctx': nrm(ks[3], (D,), 1.0),
        'ada_w': nrm(ks[4], (DEPTH, D, 6 * D), 0.5 * D ** -0.5),
        'ada_b': nrm(ks[5], (DEPTH, 6 * D), 0.02),
        'norm_mix': gain(ks[6], (DEPTH, D)),
        'norm_ffn': gain(ks[7], (DEPTH, D)),
        'w_in': nrm(ks[8], (DEPTH, D, IN_WIDTH), D ** -0.5),
        'gqa_q_norm': gain(ks[9], (DEPTH, GQA_HEAD_DIM)),
        'gqa_k_norm': gain(ks[10], (DEPTH, GQA_HEAD_DIM)),
        'mla_q_a_norm': gain(ks[11], (DEPTH, MLA_Q_RANK)),
        'mla_kv_a_norm': gain(ks[12], (DEPTH, MLA_KV_RANK)),
        'mla_w_qb': nrm(ks[13], (DEPTH, MLA_Q_RANK, MLA_HEADS * MLA_QK_DIM), MLA_Q_RANK ** -0.5),
        'mla_w_kvb': nrm(ks[14], (DEPTH, MLA_KV_RANK, MLA_HEADS * (MLA_NOPE_DIM + MLA_V_DIM)), MLA_KV_RANK ** -0.5),
        'mla_q_norm': gain(ks[15], (DEPTH, MLA_QK_DIM)),
        'mla_k_norm': gain(ks[16], (DEPTH, MLA_QK_DIM)),
        'w_o_gqa': nrm(ks[17], (DEPTH, GQA_Q_W, D), GQA_Q_W ** -0.5),
        'w_o_mla': nrm(ks[18], (DEPTH, MLA_V_W, D), MLA_V_W ** -0.5),
        'w_out': nrm(ks[19], (DEPTH, D, D), D ** -0.5),
        'router_w': nrm(ks[20], (DEPTH, D, N_EXPERTS), D ** -0.5),
        'router_b': nrm(ks[21], (DEPTH, N_EXPERTS), 0.01),
        'expert_w1': nrm(ks[22], (DEPTH, N_EXPERTS, D, 2 * D_EXPERT), D ** -0.5),
        'expert_b1': nrm(ks[23], (DEPTH, N_EXPERTS, 2 * D_EXPERT), 0.02),
        'expert_w2': nrm(ks[24], (DEPTH, N_EXPERTS, D_EXPERT, D), D_EXPERT ** -0.5),
        'expert_b2': nrm(ks[25], (DEPTH, N_EXPERTS, D), 0.02),
    }


def reference(x, c, ctx, c_ctx, ada_w, ada_b, norm_mix, norm_ffn, w_in, gqa_q_norm, gqa_k_norm,
              mla_q_a_norm, mla_kv_a_norm, mla_w_qb, mla_w_kvb, mla_q_norm, mla_k_norm,
              w_o_gqa, w_o_mla, w_out, router_w, router_b, expert_w1, expert_b1, expert_w2, expert_b2):
    seq_len = x.shape[1]
    n_rows = seq_len // GRID_W
    row = jnp.broadcast_to(jnp.arange(n_rows, dtype=jnp.int32)[:, None], (n_rows, GRID_W)).reshape(-1)
    col = jnp.broadcast_to(jnp.arange(GRID_W, dtype=jnp.int32)[None, :], (n_rows, GRID_W)).reshape(-1)
    pos = (row, col)
    xc = ctx
    for l in range(DEPTH):
        p = {
            'ada_w': ada_w[l], 'ada_b': ada_b[l], 'norm_mix': norm_mix[l], 'norm_ffn': norm_ffn[l],
            'w_in': w_in[l], 'gqa_q_norm': gqa_q_norm[l], 'gqa_k_norm': gqa_k_norm[l],
            'mla_q_a_norm': mla_q_a_norm[l], 'mla_kv_a_norm': mla_kv_a_norm[l],
            'mla_w_qb': mla_w_qb[l], 'mla_w_kvb': mla_w_kvb[l],
            'mla_q_norm': mla_q_norm[l], 'mla_k_norm': mla_k_norm[l],
            'w_o_gqa': w_o_gqa[l], 'w_o_mla': w_o_mla[l], 'w_out': w_out[l],
            'router_w': router_w[l], 'router_b': router_b[l],
            'expert_w1': expert_w1[l], 'expert_b1': expert_b1[l],
            'expert_w2': expert_w2[l], 'expert_b2': expert_b2[l],
        }
        x, xc = _layer(x, xc, c, c_ctx, p, pos, l < DEPTH - 1)
    return x
```

```python
import os
import numpy as np
from contextlib import ExitStack
import concourse.bass as bass
import concourse.mybir as mybir
from concourse.bass_utils import run_bass_kernel_spmd

F32 = mybir.dt.float32
BF16 = mybir.dt.bfloat16
ALU = mybir.AluOpType
AF = mybir.ActivationFunctionType
AX = mybir.AxisListType
U32 = mybir.dt.uint32
SPARSE = os.environ.get("MK_SPARSE", "1") == "1"
BLK = 512
NBLK = 16384 // BLK + 32
ECAP = 4096
ZROW = 32 * ECAP
NCONST = 76 + 128 + NBLK

D = 1024
SEQ = 4096
NCTX = 256
NT = SEQ + NCTX
EPS = 1e-6
NEXP = 32
STAGE = int(os.environ.get("MK_STAGE", "99"))


class Tok:
    __slots__ = ("w", "wd", "rs", "rd")

    def __init__(self):
        self.w = None
        self.wd = []
        self.rs = {}
        self.rd = []


class Tile:
    def __init__(self, ap):
        self.ap = ap
        self.tok = Tok()

    def __getitem__(self, k):
        return self.ap[k]


class Op:
    __slots__ = ("eng", "fn", "deps", "dma", "needs_inc", "seg", "val", "sem", "barred", "seq")


ENGS = ("pe", "act", "dve", "pool", "sp")
SEG = 4000
NDMASEM = 40


class Sched:
    def __init__(self):
        self.ops = {e: [] for e in ENGS}
        self.nseq = 0

    def add(self, eng, fn, reads=(), writes=(), dma=False):
        op = Op()
        op.eng, op.fn, op.dma, op.deps, op.needs_inc = eng, fn, dma, [], False
        op.seg = op.val = op.sem = None
        seen = set()

        def dep(d, raw):
            if d is None or id(d) in seen:
                return
            if d.fn is None:
                if d.eng != eng:
                    for dd in d.deps:
                        dep(dd, raw)
                return
            if not d.dma and not dma and d.eng == eng:
                if eng == "pe" or raw is None:
                    return
            seen.add(id(d))
            op.deps.append(d)
            d.needs_inc = True

        for t in reads:
            dep(t.tok.w, True)
            for d in t.tok.wd:
                dep(d, True)
        for t in writes:
            k = t.tok
            if dma:
                if k.w is not None:
                    dep(k.w, None)
            else:
                dep(k.w, None)
                for d in k.wd:
                    dep(d, None)
            for r in k.rs.values():
                dep(r, False)
            for r in k.rd:
                dep(r, False)
        for t in reads:
            if dma:
                t.tok.rd.append(op)
            else:
                t.tok.rs[eng] = op
        for t in writes:
            k = t.tok
            had_readers = bool(k.rs) or bool(k.rd)
            if dma:
                if had_readers or k.w is not None:
                    k.w, k.wd = None, [op]
                else:
                    k.wd.append(op)
            else:
                k.w, k.wd = op, []
            k.rs, k.rd = {}, []
        op.seq = self.nseq
        self.nseq += 1
        self.ops[eng].append(op)
        return op

    def barrier(self):
        lasts = []
        for e in ENGS:
            for op in reversed(self.ops[e]):
                if not op.dma and op.fn is not None:
                    lasts.append(op)
                    break
        dmas = [op for e in ENGS for op in self.ops[e] if op.dma and not getattr(op, "barred", False)]
        for op in dmas:
            op.barred = True
        for e in ENGS:
            b = self.add(e, None, [], [])
            for d in lasts:
                if d.eng != e:
                    b.deps.append(d); d.needs_inc = True
            for d in dmas:
                b.deps.append(d)

    def emit(self, nc, es, block):
        dsems = [es.enter_context(nc.semaphore("dq%d" % i)) for i in range(NDMASEM)]
        dcount = [0] * NDMASEM
        csems = {}
        pools = {"sp": list(range(0, 24)), "pool": list(range(24, 36)), "act": list(range(36, 40))}
        for e in ENGS:
            cnt = 0
            di = 0
            for op in self.ops[e]:
                if op.dma:
                    pl = pools[e]
                    op.sem = pl[di % len(pl)]
                    dcount[op.sem] += 16
                    op.val = dcount[op.sem]
                    di += 1
                elif op.needs_inc:
                    cnt += 1
                    op.seg, op.val = (cnt - 1) // SEG, (cnt - 1) % SEG + 1
                    if (e, op.seg) not in csems:
                        csems[(e, op.seg)] = es.enter_context(nc.semaphore("c_%s_%d" % (e, op.seg)))
        self.n_inst = {e: len(self.ops[e]) for e in ENGS}

        def run(e, eng):
            waited = {}
            nw = 0
            for op in self.ops[e]:
                for d in op.deps:
                    if d.dma:
                        key, val, sem = ("d", d.sem), d.val, dsems[d.sem]
                        if waited.get(key, 0) >= val:
                            continue
                        waited[key] = val
                    else:
                        key, val, sem = ("c", d.eng), (d.seg, d.val), csems[(d.eng, d.seg)]
                        if waited.get(key, (-1, 0)) >= val:
                            continue
                        waited[key] = val
                        val = d.val
                    eng.wait_ge(sem, val)
                    nw += 1
                if op.dma and op.val > 16:
                    key = ("d", op.sem)
                    if waited.get(key, 0) < op.val - 16:
                        eng.wait_ge(dsems[op.sem], op.val - 16)
                        waited[key] = op.val - 16
                if op.fn is None:
                    continue
                ins = op.fn(eng)
                if op.dma:
                    ins.then_inc(dsems[op.sem], 16)
                elif op.needs_inc:
                    ins.then_inc(csems[(e, op.seg)], 1)
            self.n_inst[e] = (len(self.ops[e]), nw)

        @block.tensor
        def _(eng):
            run("pe", eng)

        @block.scalar
        def _(eng):
            run("act", eng)

        @block.vector
        def _(eng):
            run("dve", eng)

        @block.gpsimd
        def _(eng):
            run("pool", eng)

        @block.sync
        def _(eng):
            run("sp", eng)


def build_program():
    nc = bass.Bass("TRN2", target_bir_lowering=False)
    S = Sched()
    es = ExitStack()

    def din(name, shape, dt=F32):
        return nc.dram_tensor(name, list(shape), dt, kind="ExternalInput").ap()

    def dscr(name, shape, dt=BF16):
        if STAGE in (2, 3):
            return nc.dram_tensor(name, list(shape), dt, kind="ExternalOutput").ap()
        return nc.dram_tensor(name, list(shape), dt).ap()

    x_d = din("x", [SEQ, D]); ctx_d = din("ctx", [NCTX, D]); cc_d = din("cc", [2, D])
    ada_w = din("ada_w", [D, 6 * D]); ada_b = din("ada_b", [6 * D])
    norm_mix = din("norm_mix", [D]); norm_ffn = din("norm_ffn", [D])
    w_in = din("w_in", [D, 4032])
    gq_d = din("gqa_q_norm", [128]); gk_d = din("gqa_k_norm", [128])
    gqa_d = din("mla_q_a_norm", [256]); gkva_d = din("mla_kv_a_norm", [128])
    w_qb = din("mla_w_qb", [256, 1536]); w_kvb = din("mla_w_kvb", [128, 2048])
    gmq_d = din("mla_q_norm", [192]); gmk_d = din("mla_k_norm", [192])
    w_oa = din("w_o_gqa", [D, D]); w_ob = din("w_o_mla", [D, D]); w_out = din("w_out", [D, D])
    rw_d = din("router_w", [D, NEXP]); rb_d = din("router_b", [NEXP])
    ew1 = din("expert_w1", [NEXP, D, 2 * D]); eb1 = din("expert_b1", [NEXP, 2 * D])
    ew2 = din("expert_w2", [NEXP, D, D]); eb2 = din("expert_b2", [NEXP, D])
    ident_d = din("ident", [128, 128]); sel0_d = din("sel0", [2, 128])
    cosA_d = din("cosA", [128, NT]); sinA_d = din("sinA", [128, NT])
    cosB_d = din("cosB", [64, NT]); sinB_d = din("sinB", [64, NT])
    consts_d = din("consts", [128, NCONST]) if SPARSE else None
    out_d = nc.dram_tensor("out", [SEQ, D], F32, kind="ExternalOutput").ap()

    qa_scr = dscr("qa_scr", [8, 128, SEQ]); ka_scr = dscr("ka_scr", [2, 128, NT])
    va_scr = dscr("va_scr", [2, 128, 34, 128])
    qb_scr = dscr("qb_scr", [8, 128, SEQ]); qbr_scr = dscr("qbr_scr", [8, 64, SEQ])
    kb_scr = dscr("kb_scr", [8, 128, NT]); kbr_scr = dscr("kbr_scr", [8, 64, NT])
    vb_scr = dscr("vb_scr", [8, 128, 34, 128])
    o_scr = (nc.dram_tensor("o_scr", [16, 128, SEQ], BF16, kind="ExternalOutput").ap() if STAGE in (4, 5) else dscr("o_scr", [16, 128, SEQ]))
    x1_scr = out_d if STAGE == 5 else dscr("x1_scr", [SEQ, D], F32)
    dbg_d = None
    if STAGE < 99:
        dbg_d = nc.dram_tensor("dbg", [128, 8 * NT], F32, kind="ExternalOutput").ap()

    ARENA_F = 52400
    arena = es.enter_context(nc.sbuf_tensor("arena", [128, ARENA_F], F32))
    psum = [Tile(es.enter_context(nc.psum_tensor("ps%d" % i, [128, 512], F32))[:, :]) for i in range(8)]
    block = es.enter_context(nc.Block())

    class Alloc:
        def __init__(self, base=0):
            self.off = base

        def __call__(self, shape, dt=F32, parts=128):
            n = int(np.prod(shape[1:]))
            nf = n if dt in (F32, U32) else (n + 1) // 2
            nf = (nf + 7) // 8 * 8
            a = arena[:, self.off:self.off + nf]
            self.off += nf
            assert self.off <= ARENA_F, "SBUF arena overflow %d" % self.off
            if dt != F32:
                a = a.bitcast(dt)
            a = a[:, 0:n]
            a = a[0:shape[0]]
            if len(shape) == 3:
                a = a.rearrange("p (a b) -> p a b", a=shape[1])
            elif len(shape) == 4:
                a = a.rearrange("p (a b c) -> p a b c", a=shape[1], b=shape[2])
            return Tile(a)

    def PE(fn, r, w): S.add("pe", fn, r, w)
    def ACT(fn, r, w): S.add("act", fn, r, w)
    def DVE(fn, r, w): S.add("dve", fn, r, w)
    def POOL(fn, r, w): S.add("pool", fn, r, w)
    def DMA(out, in_, r, w, q="sp", nonc=False):
        def fn(e):
            if nonc:
                with nc.allow_non_contiguous_dma(reason="tiny vector load"):
                    return e.dma_start(out=out, in_=in_)
            return e.dma_start(out=out, in_=in_)
        return S.add(q, fn, r, w, dma=True)

    def mm(out, pairs, r, w):
        n = len(pairs)

        def fn(e):
            ins = None
            for i, (l, rh) in enumerate(pairs):
                ins = e.matmul(out, l, rh, start=(i == 0), stop=(i == n - 1))
            return ins
        PE(fn, r, w)

    def ts(eng, out, in0, s1, s2, op0, op1, r, w):
        if s2 is None:
            eng(lambda e: e.tensor_scalar(out=out, in0=in0, scalar1=s1, scalar2=None, op0=op0), r, w)
        else:
            eng(lambda e: e.tensor_scalar(out=out, in0=in0, scalar1=s1, scalar2=s2, op0=op0, op1=op1), r, w)

    def tt(eng, out, in0, in1, op, r, w):
        eng(lambda e: e.tensor_tensor(out=out, in0=in0, in1=in1, op=op), r, w)

    def stt(eng, out, in0, sc, in1, op0, op1, r, w):
        eng(lambda e: e.scalar_tensor_tensor(out=out, in0=in0, scalar=sc, in1=in1, op0=op0, op1=op1), r, w)

    def act(out, in_, func, r, w, bias=None, scale=None, accum=None):
        kw = {}
        if bias is not None: kw["bias"] = bias
        if scale is not None: kw["scale"] = scale
        if accum is not None: kw["accum_out"] = accum
        ACT(lambda e: e.activation(out=out, in_=in_, func=func, **kw), r, w)

    def rstd_from(out_t, out_ap, ss_ap, ss_deps, n):
        act(out_ap, ss_ap, AF.Ln, ss_deps + [eps_c], [out_t], bias=eps_c[0:out_ap.shape[0], 0:1], scale=1.0 / n)
        act(out_ap, out_ap, AF.Exp, [out_t], [out_t], scale=-0.5)

    P = Alloc(0)
    ident = P([128, 128]); ones_f = P([128, 128]); ones_b = P([128, 128], BF16)
    modT = P([128, 48, 2]); adabT = P([128, 48]); nmT = P([128, 8]); nfT = P([128, 8])
    A1 = P([128, 8, 2]); A2 = P([128, 8])
    g1bc = P([128, D]); g2bc = P([128, D])
    gqT = P([128, 2]); gkT = P([128, 2])
    gqaT = P([128, 2]); gkvaT = P([128, 1])
    gmqT = P([128, 3]); gmkT = P([128, 3])
    ss1 = P([128, 4]); rs1 = P([128, 4])
    zero_c = P([128, 1]); eps_c = P([128, 1])
    HT_OFF = P.off
    hT = P([128, 8, NT], BF16)
    PERSIST_END = P.off

    DMA(ident.ap, ident_d, [], [ident])
    POOL(lambda e: e.memset(ones_f.ap, 1.0), [], [ones_f])
    POOL(lambda e: e.memset(ones_b.ap, 1.0), [], [ones_b])
    POOL(lambda e: e.memset(zero_c.ap, 0.0), [], [zero_c])
    POOL(lambda e: e.memset(eps_c.ap, EPS), [], [eps_c])
    DMA(nmT.ap, norm_mix.rearrange("(j p) -> p j", p=128), [], [nmT], nonc=True)
    DMA(nfT.ap, norm_ffn.rearrange("(j p) -> p j", p=128), [], [nfT], nonc=True)

    def colvec(dst_t, col, src_d, lo, hi, p0=0):
        DMA(dst_t[p0:p0 + (hi - lo), col:col + 1], src_d[lo:hi].rearrange("(p o) -> p o", o=1), [], [dst_t], nonc=True)

    for (t_, d_) in ((gqT, gq_d), (gkT, gk_d)):
        colvec(t_, 0, d_, 0, 128)
        colvec(t_, 1, d_, 32, 64, 0); colvec(t_, 1, d_, 0, 32, 32)
        colvec(t_, 1, d_, 96, 128, 64); colvec(t_, 1, d_, 64, 96, 96)
    colvec(gqaT, 0, gqa_d, 0, 128); colvec(gqaT, 1, gqa_d, 128, 256)
    colvec(gkvaT, 0, gkva_d, 0, 128)
    for (t_, d_) in ((gmqT, gmq_d), (gmkT, gmk_d)):
        colvec(t_, 0, d_, 0, 128)
        colvec(t_, 1, d_, 128, 192, 0)
        colvec(t_, 2, d_, 144, 160, 0); colvec(t_, 2, d_, 128, 144, 16)
        colvec(t_, 2, d_, 176, 192, 32); colvec(t_, 2, d_, 160, 176, 48)

    T0 = Alloc(PERSIST_END)
    ccT = T0([128, 8, 2]); scT = T0([128, 8, 2])
    wblk = [T0([128, 8, 1024]) for _ in range(2)]
    modrow = T0([2, 6 * D]); adab2 = T0([2, 6 * D]); sel0 = T0([2, 128])
    abtmp = [T0([128, D]) for _ in range(2)]; nfbc = T0([128, D])
    ab_scr = dscr("ab_scr", [2, 128, D], F32)
    for r in range(2):
        DMA(ccT[:, :, r], cc_d[r].rearrange("(j p) -> p j", p=128), [], [ccT], nonc=True)
        DMA(adab2[r:r + 1, :], ada_b.rearrange("(o n) -> o n", o=1), [], [adab2])
    DMA(sel0.ap, sel0_d, [], [sel0])
    act(scT.ap, ccT.ap, AF.Silu, [ccT], [scT])
    ada_v = ada_w.rearrange("(kc p) n -> p kc n", p=128)
    for blk in range(6):
        wb = wblk[blk % 2]
        DMA(wb.ap, ada_v[:, :, blk * 1024:(blk + 1) * 1024], [], [wb])
        for half in range(2):
            pr = psum[(blk * 2 + half) % 4]
            cols = slice(blk * 1024 + half * 512, blk * 1024 + (half + 1) * 512)
            mm(pr[0:2, :], [(scT[:, kc, :], wb[:, kc, half * 512:(half + 1) * 512]) for kc in range(8)], [scT, wb], [pr])
            tt(DVE, modrow[:, cols], pr[0:2, :], adab2[:, cols], ALU.add, [pr, adab2], [modrow])
    pst = psum[4]

    def ftm(e):
        ins = None
        for j in range(48):
            ins = e.transpose(out=pst[:, 2 * j:2 * j + 2], in_=modrow[0:2, j * 128:(j + 1) * 128], identity=ident[0:2, 0:2])
        return ins
    PE(ftm, [modrow, ident], [pst])
    DVE(lambda e: e.tensor_copy(out=modT.ap, in_=pst[:, 0:96].rearrange("p (j r) -> p j r", r=2)), [pst], [modT])
    for blk in ((2, 5, 3, 4) if SPARSE else (2, 5)):
        gdst = {2: g1bc, 5: g2bc, 3: abtmp[0], 4: abtmp[1]}[blk]
        for half in range(2):
            pg = psum[5 + half]
            cols = slice(blk * 1024 + half * 512, blk * 1024 + (half + 1) * 512)
            mm(pg[:, :], [(sel0.ap, modrow[0:2, cols])], [sel0, modrow], [pg])
            if half == 0:
                ACT(lambda e, gdst=gdst, pg=pg: e.copy(out=gdst[:, 0:512], in_=pg[:, :]), [pg], [gdst])
            else:
                DVE(lambda e, gdst=gdst, pg=pg: e.tensor_copy(out=gdst[:, 512:1024], in_=pg[:, :]), [pg], [gdst])
        if blk == 4:
            DMA(nfbc.ap, norm_ffn.partition_broadcast(128), [], [nfbc])
            stt(DVE, gdst.ap, gdst.ap, 1.0, nfbc.ap, ALU.add, ALU.mult, [gdst, nfbc], [gdst])
        if blk in (3, 4):
            DMA(ab_scr[blk - 3], gdst.ap, [gdst], [])
    for r in range(2):
        stt(DVE, A1[:, :, r], modT[:, 8:16, r], 1.0, nmT.ap, ALU.add, ALU.mult, [modT, nmT], [A1])
    stt(DVE, A2.ap, modT[:, 32:40, 0], 1.0, nfT.ap, ALU.add, ALU.mult, [modT, nfT], [A2])

    S.barrier()
    T1 = Alloc(PERSIST_END)
    xt = [T1([128, D]) for _ in range(4)]
    xn = [T1([128, D]) for _ in range(4)]
    NB = {"xt": xt, "xn": xn}

    def norm_mod_T(i, src_ap, A_ap, S_ap, dst_fn, ps_pair, extra_f32=None, src_deps=()):
        xt_, xn_ = NB["xt"][i % len(NB["xt"])], NB["xn"][i % len(NB["xn"])]
        ss_, rs_ = ss1[:, i % 4:i % 4 + 1], rs1[:, i % 4:i % 4 + 1]
        DMA(xt_.ap, src_ap, list(src_deps), [xt_])
        act(xn_.ap, xt_.ap, AF.Square, [xt_], [xn_, ss1], accum=ss_)
        rstd_from(rs1, rs_, ss_, [ss1], D)
        ACT(lambda e: e.activation(out=xn_.ap, in_=xt_.ap, func=AF.Copy, scale=rs_), [xt_, rs1], [xn_])
        for half in range(2):
            pst = ps_pair[half]

            def fn(e, half=half, pst=pst):
                ins = None
                for jj in range(4):
                    j = half * 4 + jj
                    ins = e.transpose(out=pst[:, jj * 128:(jj + 1) * 128], in_=xn_[:, j * 128:(j + 1) * 128], identity=ident.ap)
                return ins
            PE(fn, [xn_, ident], [pst])
            for jj in range(4):
                j = half * 4 + jj
                dst_ap, dst_t = dst_fn(j)
                if jj % 2 == 0:
                    ts(DVE, dst_ap, pst[:, jj * 128:(jj + 1) * 128], A_ap[:, j:j + 1], S_ap[:, j:j + 1], ALU.mult, ALU.add,
                       [pst, A1, A2, modT], [dst_t])
                else:
                    act(dst_ap, pst[:, jj * 128:(jj + 1) * 128], AF.Identity, [pst, A1, A2, modT], [dst_t],
                        bias=S_ap[:, j:j + 1], scale=A_ap[:, j:j + 1])
                if extra_f32 is not None:
                    ts(DVE, extra_f32[0][:, j, :], pst[:, jj * 128:(jj + 1) * 128], A_ap[:, j:j + 1], S_ap[:, j:j + 1],
                       ALU.mult, ALU.add, [pst, A1, A2, modT], [extra_f32[1]])
        return xt_

    hT_chunks = [Tile(hT.ap) for _ in range(9)]

    for i in range(34):
        if i < 2:
            src = ctx_d[i * 128:(i + 1) * 128, :]; r = 1
        else:
            src = x_d[(i - 2) * 128:(i - 1) * 128, :]; r = 0
        c0 = i * 128
        chunk = hT_chunks[c0 // 512]
        norm_mod_T(i, src, A1[:, :, r], modT[:, 0:8, r], lambda j, c0=c0, chunk=chunk: (hT[:, j, c0:c0 + 128], chunk),
                   (psum[(i % 4) * 2], psum[(i % 4) * 2 + 1]))

    final_deps = []

    def dbg_dump(src_tile, ncols, col0=0):
        pass

    if STAGE == 1:
        TD = Alloc(T1.off)
        for j in range(8):
            for c in range(9):
                w = min(512, NT - c * 512)
                tmp = TD([128, 512])
                ts(DVE, tmp[:, 0:w], hT[:, j, c * 512:c * 512 + w], 1.0, None, ALU.mult, None, [hT_chunks[c]], [tmp])
                final_deps.append(DMA(dbg_d[:, j * NT + c * 512:j * NT + c * 512 + w], tmp[:, 0:w], [tmp], []))
                if TD.off > ARENA_F - 600:
                    TD = Alloc(T1.off)
        return finish(nc, S, es, block, final_deps)


    def all_dmas():
        return [op for e_ in ENGS for op in S.ops[e_] if op.dma]

    w_in_v = w_in.rearrange("(kc p) n -> p kc n", p=128)
    chunks = [(c, c * 512, min(512, NT - c * 512)) for c in range(9)]

    def qstore(dst_h, src_t, c, c0, w, parts=128):
        if c == 0:
            return DMA(dst_h[:, 0:256], src_t[0:parts, 256:512], [src_t], [])
        return DMA(dst_h[:, c0 - 256:c0 - 256 + w], src_t[0:parts, 0:w], [src_t], [])

    S.barrier()
    T2 = Alloc(PERSIST_END)
    wqa = T2([128, 8, 1536], BF16); wrot = T2([128, 8, 1280], BF16)
    cosT = [T2([128, 512]) for _ in range(2)]; sinT = [T2([128, 512]) for _ in range(2)]
    sqb = [T2([128, 512], BF16) for _ in range(2)]; rsd = [T2([128, 512]) for _ in range(2)]
    t1b = [T2([128, 512]) for _ in range(2)]; t2b = [T2([128, 512]) for _ in range(2)]
    qob = [T2([128, 512], BF16) for _ in range(3)]
    vta = [T2([128, 4, 256], BF16) for _ in range(2)]
    DMA(wqa.ap, w_in_v[:, :, 0:1536], [], [wqa], q="pool")
    for kc in range(8):
        sv = wqa[:, kc, 0:1280].rearrange("p (h d) -> p h d", d=128)
        dv = wrot[:, kc, :].rearrange("p (h d) -> p h d", d=128)
        for (dlo, slo, neg) in ((0, 32, True), (32, 0, False), (64, 96, True), (96, 64, False)):
            if neg:
                ts(DVE, dv[:, :, dlo:dlo + 32], sv[:, :, slo:slo + 32], -1.0, None, ALU.mult, None, [wqa], [wrot])
            else:
                ACT(lambda e, dv=dv, sv=sv, dlo=dlo, slo=slo: e.copy(out=dv[:, :, dlo:dlo + 32], in_=sv[:, :, slo:slo + 32]), [wqa], [wrot])
    it = 0
    for (c, c0, w) in chunks:
        hc_ = hT_chunks[c]
        cs_, sn_ = cosT[c % 2], sinT[c % 2]
        DMA(cs_[:, 0:w], cosA_d[:, c0:c0 + w], [], [cs_])
        DMA(sn_[:, 0:w], sinA_d[:, c0:c0 + w], [], [sn_])
        for hh in range(10):
            raw, rot, ssp = psum[hh % 2], psum[2 + hh % 2], psum[4 + hh % 2]
            cols = slice(hh * 128, (hh + 1) * 128)
            mm(raw[:, 0:w], [(wqa[:, kc, cols], hT[:, kc, c0:c0 + w]) for kc in range(8)], [wqa, hc_], [raw])
            mm(rot[:, 0:w], [(wrot[:, kc, cols], hT[:, kc, c0:c0 + w]) for kc in range(8)], [wrot, hc_], [rot])
            sq_, rs_, t1_, t2_, qo_ = sqb[it % 2], rsd[it % 2], t1b[it % 2], t2b[it % 2], qob[it % 3]
            it += 1
            act(sq_[:, 0:w], raw[:, 0:w], AF.Square, [raw], [sq_])
            mm(ssp[:, 0:w], [(ones_b.ap, sq_[:, 0:w])], [ones_b, sq_], [ssp])
            rstd_from(rs_, rs_[:, 0:w], ssp[:, 0:w], [ssp], 128)
            g_ = gqT if hh < 8 else gkT
            stt(DVE, t1_[:, 0:w], raw[:, 0:w], g_[:, 0:1], cs_[:, 0:w], ALU.mult, ALU.mult, [raw, g_, cs_, sq_], [t1_])
            stt(DVE, t2_[:, 0:w], rot[:, 0:w], g_[:, 1:2], sn_[:, 0:w], ALU.mult, ALU.mult, [rot, g_, sn_], [t2_])
            tt(POOL, t1_[:, 0:w], t1_[:, 0:w], t2_[:, 0:w], ALU.add, [t1_, t2_], [t1_])
            tt(DVE, qo_[:, 0:w], t1_[:, 0:w], rs_[:, 0:w], ALU.mult, [t1_, rs_], [qo_])
            if hh < 8:
                qstore(qa_scr[hh], qo_, c, c0, w)
            else:
                DMA(ka_scr[hh - 8, :, c0:c0 + w], qo_[:, 0:w], [qo_], [])
        vt_ = vta[c % 2]
        nsub = w // 128
        for sub in range(nsub):
            pv = psum[6 + sub % 2]
            mm(pv[:, 0:256], [(hT[:, kc, c0 + sub * 128:c0 + (sub + 1) * 128], wqa[:, kc, 1280:1536]) for kc in range(8)],
               [wqa, hc_], [pv])
            ACT(lambda e, vt_=vt_, pv=pv, sub=sub: e.copy(out=vt_[:, sub, :], in_=pv[:, 0:256]), [pv], [vt_])
        for g in range(2):
            DMA(va_scr[g, :, c * 4:c * 4 + nsub, :], vt_[:, 0:nsub, g * 128:(g + 1) * 128], [vt_], [])

    if STAGE == 2:
        return finish(nc, S, es, block, all_dmas())

    S.barrier()
    T3 = Alloc(PERSIST_END)
    wm = T3([128, 8, 448], BF16); wkrot = T3([128, 8, 64], BF16)
    wqb_t = T3([128, 2, 1536], BF16); wqb_rot = T3([128, 2, 8, 64], BF16)
    wkvb_t = T3([128, 2048], BF16); wv_t = T3([128, 1024], BF16)
    cosBt = [T3([128, 512]) for _ in range(2)]; sinBt = [T3([128, 512]) for _ in range(2)]
    sqa = T3([128, 2, 512], BF16); qan = T3([128, 2, 512], BF16); ckvn = T3([128, 512], BF16)
    sqk = T3([128, 512], BF16); krsq = T3([128, 512], BF16); krr = T3([128, 512]); krt2 = T3([128, 512])
    rs3 = [T3([128, 512]) for _ in range(4)]
    sqn = [T3([128, 512], BF16) for _ in range(2)]; sqr = [T3([128, 512], BF16) for _ in range(2)]
    t13 = [T3([128, 512]) for _ in range(2)]; t23 = [T3([128, 512]) for _ in range(2)]
    o3 = [T3([128, 512], BF16) for _ in range(8)]
    vtb = [T3([128, 4, 1024], BF16) for _ in range(2)]
    DMA(wm.ap, w_in_v[:, :, 1536:1984], [], [wm], q="pool")
    DMA(wqb_t.ap, w_qb.rearrange("(c p) n -> p c n", p=128), [], [wqb_t], q="pool")
    DMA(wkvb_t.ap, w_kvb, [], [wkvb_t], q="pool")
    POOL(lambda e: e.tensor_copy(out=wv_t.ap.rearrange("p (h d) -> p h d", d=128),
                                 in_=wkvb_t.ap.rearrange("p (h t d) -> p h t d", t=2, d=128)[:, :, 1, :]), [wkvb_t], [wv_t])
    for (dlo, slo, neg) in ((0, 16, True), (16, 0, False), (32, 48, True), (48, 32, False)):
        for kc in range(8):
            if neg:
                ts(DVE, wkrot[:, kc, dlo:dlo + 16], wm[:, kc, 384 + slo:384 + slo + 16], -1.0, None, ALU.mult, None, [wm], [wkrot])
            else:
                ACT(lambda e, kc=kc, dlo=dlo, slo=slo: e.copy(out=wkrot[:, kc, dlo:dlo + 16], in_=wm[:, kc, 384 + slo:384 + slo + 16]), [wm], [wkrot])
        for c2 in range(2):
            sv = wqb_t[:, c2, :].rearrange("p (h d) -> p h d", d=192)
            if neg:
                ts(DVE, wqb_rot[:, c2, :, dlo:dlo + 16], sv[:, :, 128 + slo:128 + slo + 16], -1.0, None, ALU.mult, None, [wqb_t], [wqb_rot])
            else:
                ACT(lambda e, c2=c2, sv=sv, dlo=dlo, slo=slo: e.copy(out=wqb_rot[:, c2, :, dlo:dlo + 16], in_=sv[:, :, 128 + slo:128 + slo + 16]), [wqb_t], [wqb_rot])
    it = 0
    for (c, c0, w) in chunks:
        hc_ = hT_chunks[c]
        W = slice(0, w)
        cs_, sn_ = cosBt[c % 2], sinBt[c % 2]
        DMA(cs_[0:64, W], cosB_d[:, c0:c0 + w], [], [cs_])
        DMA(sn_[0:64, W], sinB_d[:, c0:c0 + w], [], [sn_])
        for c2 in range(2):
            mm(psum[c2][:, W], [(wm[:, kc, c2 * 128:(c2 + 1) * 128], hT[:, kc, c0:c0 + w]) for kc in range(8)], [wm, hc_], [psum[c2]])
            act(sqa[:, c2, W], psum[c2][:, W], AF.Square, [psum[c2]], [sqa])
        mm(psum[3][:, W], [(ones_b.ap, sqa[:, 0, W]), (ones_b.ap, sqa[:, 1, W])], [ones_b, sqa], [psum[3]])
        r_ = rs3[0]
        rstd_from(r_, r_[:, W], psum[3][:, W], [psum[3]], 256)
        for c2 in range(2):
            stt(DVE, qan[:, c2, W], psum[c2][:, W], gqaT[:, c2:c2 + 1], r_[:, W], ALU.mult, ALU.mult, [psum[c2], gqaT, r_, sqa], [qan])
        mm(psum[4][:, W], [(wm[:, kc, 256:384], hT[:, kc, c0:c0 + w]) for kc in range(8)], [wm, hc_], [psum[4]])
        act(sqk[:, W], psum[4][:, W], AF.Square, [psum[4]], [sqk])
        mm(psum[5][:, W], [(ones_b.ap, sqk[:, W])], [ones_b, sqk], [psum[5]])
        r_ = rs3[1]
        rstd_from(r_, r_[:, W], psum[5][:, W], [psum[5]], 128)
        stt(DVE, ckvn[:, W], psum[4][:, W], gkvaT[:, 0:1], r_[:, W], ALU.mult, ALU.mult, [psum[4], gkvaT, r_, sqk], [ckvn])
        mm(psum[6][0:64, W], [(wm[:, kc, 384:448], hT[:, kc, c0:c0 + w]) for kc in range(8)], [wm, hc_], [psum[6]])
        mm(psum[7][0:64, W], [(wkrot[:, kc, :], hT[:, kc, c0:c0 + w]) for kc in range(8)], [wkrot, hc_], [psum[7]])
        act(krsq[0:64, W], psum[6][0:64, W], AF.Square, [psum[6]], [krsq])
        stt(DVE, krr[0:64, W], psum[6][0:64, W], gmkT[0:64, 1:2], cs_[0:64, W], ALU.mult, ALU.mult, [psum[6], gmkT, cs_, krsq], [krr])
        stt(DVE, krt2[0:64, W], psum[7][0:64, W], gmkT[0:64, 2:3], sn_[0:64, W], ALU.mult, ALU.mult, [psum[7], gmkT, sn_], [krt2])
        tt(POOL, krr[0:64, W], krr[0:64, W], krt2[0:64, W], ALU.add, [krr, krt2], [krr])
        vt_ = vtb[c % 2]
        nsub = w // 128
        for sub in range(nsub):
            for half in range(2):
                pv = psum[6 + half]
                mm(pv[:, :], [(ckvn[:, sub * 128:(sub + 1) * 128], wv_t[:, half * 512:(half + 1) * 512])], [ckvn, wv_t], [pv])
                ACT(lambda e, vt_=vt_, pv=pv, sub=sub, half=half: e.copy(out=vt_[:, sub, half * 512:(half + 1) * 512], in_=pv[:, :]), [pv], [vt_])
        for h in range(8):
            DMA(vb_scr[h, :, c * 4:c * 4 + nsub, :], vt_[:, 0:nsub, h * 128:(h + 1) * 128], [vt_], [])
        for h in range(8):
            par = h % 2
            pn, pr, prr, pss = psum[par], psum[2 + par], psum[4 + par], psum[6 + par]
            sqn_, sqr_, t1_, t2_ = sqn[par], sqr[par], t13[par], t23[par]
            rq_ = rs3[2 + par]
            mm(pn[:, W], [(wqb_t[:, c2, h * 192:h * 192 + 128], qan[:, c2, W]) for c2 in range(2)], [wqb_t, qan], [pn])
            mm(pr[0:64, W], [(wqb_t[:, c2, h * 192 + 128:h * 192 + 192], qan[:, c2, W]) for c2 in range(2)], [wqb_t, qan], [pr])
            mm(prr[0:64, W], [(wqb_rot[:, c2, h, :], qan[:, c2, W]) for c2 in range(2)], [wqb_rot, qan], [prr])
            act(sqn_[:, W], pn[:, W], AF.Square, [pn], [sqn_])
            act(sqr_[0:64, W], pr[0:64, W], AF.Square, [pr], [sqr_])
            mm(pss[:, W], [(ones_b.ap, sqn_[:, W]), (ones_b[0:64, :], sqr_[0:64, W])], [ones_b, sqn_, sqr_], [pss])
            rstd_from(rq_, rq_[:, W], pss[:, W], [pss], 192)
            oq, oqr = o3[par * 4], o3[par * 4 + 1]
            stt(DVE, oq[:, W], pn[:, W], gmqT[:, 0:1], rq_[:, W], ALU.mult, ALU.mult, [pn, gmqT, rq_, sqn_], [oq])
            qstore(qb_scr[h], oq, c, c0, w)
            stt(DVE, t1_[0:64, W], pr[0:64, W], gmqT[0:64, 1:2], cs_[0:64, W], ALU.mult, ALU.mult, [pr, gmqT, cs_, sqr_], [t1_])
            stt(DVE, t2_[0:64, W], prr[0:64, W], gmqT[0:64, 2:3], sn_[0:64, W], ALU.mult, ALU.mult, [prr, gmqT, sn_], [t2_])
            tt(POOL, t1_[0:64, W], t1_[0:64, W], t2_[0:64, W], ALU.add, [t1_, t2_], [t1_])
            tt(DVE, oqr[0:64, W], t1_[0:64, W], rq_[0:64, W], ALU.mult, [t1_, rq_], [oqr])
            qstore(qbr_scr[h], oqr, c, c0, w, parts=64)
        for h in range(8):
            par = h % 2
            pk, psk = psum[h % 4], psum[4 + h % 4]
            sqn_, rk_ = sqn[par], rs3[2 + par]
            ok, okr = o3[par * 4 + 2], o3[par * 4 + 3]
            mm(pk[:, W], [(wkvb_t[:, h * 256:h * 256 + 128], ckvn[:, W])], [wkvb_t, ckvn], [pk])
            act(sqn_[:, W], pk[:, W], AF.Square, [pk], [sqn_])
            mm(psk[:, W], [(ones_b.ap, sqn_[:, W]), (ones_b[0:64, :], krsq[0:64, W])], [ones_b, sqn_, krsq], [psk])
            rstd_from(rk_, rk_[:, W], psk[:, W], [psk], 192)
            stt(DVE, ok[:, W], pk[:, W], gmkT[:, 0:1], rk_[:, W], ALU.mult, ALU.mult, [pk, gmkT, rk_, sqn_], [ok])
            DMA(kb_scr[h, :, c0:c0 + w], ok[:, W], [ok], [])
            tt(POOL, okr[0:64, W], krr[0:64, W], rk_[0:64, W], ALU.mult, [krr, rk_], [okr])
            DMA(kbr_scr[h, :, c0:c0 + w], okr[0:64, W], [okr], [])

    if STAGE == 3:
        return finish(nc, S, es, block, all_dmas())

    scr_tok = Tile(None)
    bar = S.add("sp", None, [], [scr_tok])
    for e_ in ENGS:
        for op in S.ops[e_]:
            if op.dma and op is not bar:
                bar.deps.append(op); op.needs_inc = True

    S.barrier()
    T4 = Alloc(PERSIST_END)
    qTb = [T4([128, SEQ], BF16) for _ in range(2)]; qrb = [T4([128, SEQ], BF16) for _ in range(2)]
    kTb = [T4([128, NT], BF16) for _ in range(2)]; krb = [T4([128, NT], BF16) for _ in range(2)]
    Vb = [T4([128, 34, 128], BF16) for _ in range(2)]
    pT = [T4([128, 512], BF16) for _ in range(4)]
    rinv = [T4([128, 512]) for _ in range(2)]
    obuf = [T4([128, 512], BF16) for _ in range(2)]
    o_stores = []
    LAG = 2
    zfill_ops = []
    if SPARSE:
        xs_d = dscr("xs_scr", [NBLK * BLK, D], BF16)
        zt4 = T4([128, 4096], BF16)
        POOL(lambda e: e.memset(zt4.ap, 0.0), [], [zt4])
        for zi in range(NBLK * BLK // 512):
            zfill_ops.append(DMA(xs_d[zi * 512:(zi + 1) * 512, :].rearrange("(p r) d -> p (r d)", p=128), zt4.ap, [zt4], [], q="pool"))
    for t_ in qrb + krb:
        POOL(lambda e, t_=t_: e.memset(t_[64:128, :], 0.0), [], [t_])
    for hd in range(16):
        b = hd % 2
        mla = hd >= 8
        h = hd - 8 if mla else hd
        qT_, kT_, V_, qr_, kr_ = qTb[b], kTb[b], Vb[b], qrb[b], krb[b]
        if not mla:
            g = h // 4
            DMA(qT_.ap, qa_scr[h], [scr_tok], [qT_])
            if h % 4 == 0:
                kg, vg = Tile(None), Tile(None)
                kT_g, V_g = kTb[g % 2], Vb[g % 2]
                DMA(kT_g.ap, ka_scr[g], [scr_tok], [kT_g])
                DMA(V_g.ap, va_scr[g], [scr_tok], [V_g])
            kT_, V_ = kT_g, V_g
            scale = 128 ** -0.5
        else:
            DMA(qT_.ap, qb_scr[h], [scr_tok], [qT_])
            DMA(qr_[0:64, :], qbr_scr[h], [scr_tok], [qr_])
            DMA(kT_.ap, kb_scr[h], [scr_tok], [kT_])
            DMA(kr_[0:64, :], kbr_scr[h], [scr_tok], [kr_])
            DMA(V_.ap, vb_scr[h], [scr_tok], [V_])
            scale = 192 ** -0.5
        for qi in range(8):
            o_ps = psum[4 + qi % 2]
            sum_ps = psum[6 + qi % 2]
            Q = slice(qi * 512, (qi + 1) * 512)
            for step in range(34 + LAG):
                if step < 34:
                    ti = step
                    s_ps = psum[ti % 4]
                    Kc = slice(ti * 128, (ti + 1) * 128)
                    pairs = [(kT_[:, Kc], qT_[:, Q])]
                    rds = [kT_, qT_]
                    if mla:
                        pairs.append((kr_[:, Kc], qr_[:, Q]))
                        rds += [kr_, qr_]
                    mm(s_ps[:, :], pairs, rds, [s_ps])
                    p_ = pT[ti % 4]
                    act(p_.ap, s_ps[:, :], AF.Exp, [s_ps], [p_], scale=scale)
                if step >= LAG:
                    ti = step - LAG
                    p_ = pT[ti % 4]
                    PE(lambda e, o_ps=o_ps, V_=V_, ti=ti, p_=p_: e.matmul(o_ps[:, :], V_[:, ti, :], p_.ap, start=(ti == 0), stop=(ti == 33)),
                       [V_, p_], [o_ps])
                    PE(lambda e, sum_ps=sum_ps, ti=ti, p_=p_: e.matmul(sum_ps[:, :], ones_b.ap, p_.ap, start=(ti == 0), stop=(ti == 33)),
                       [ones_b, p_], [sum_ps])
            ri, ob = rinv[qi % 2], obuf[qi % 2]
            DVE(lambda e, ri=ri, sum_ps=sum_ps: e.reciprocal(out=ri.ap, in_=sum_ps[:, :]), [sum_ps], [ri])
            tt(DVE, ob.ap, o_ps[:, :], ri.ap, ALU.mult, [o_ps, ri], [ob])
            o_stores.append(DMA(o_scr[hd, :, Q], ob.ap, [ob], []))

    o_tok = Tile(None)
    bar2 = S.add("sp", None, [], [o_tok])
    for op in o_stores:
        bar2.deps.append(op); op.needs_inc = True

    if STAGE == 4:
        return finish(nc, S, es, block, o_stores)

    S.barrier()
    T5 = Alloc(PERSIST_END)
    wg = T5([128, 8, 2048], BF16); woa_t = T5([128, 8, D], BF16); wob_t = T5([128, 8, D], BF16); wout_t = T5([128, 8, D], BF16)
    DMA(wg.ap, w_in_v[:, :, 1984:4032], [], [wg], q="pool")
    DMA(woa_t.ap, w_oa.rearrange("(kc p) n -> p kc n", p=128), [], [woa_t], q="pool")
    DMA(wob_t.ap, w_ob.rearrange("(kc p) n -> p kc n", p=128), [], [wob_t], q="pool")
    DMA(wout_t.ap, w_out.rearrange("(kc p) n -> p kc n", p=128), [], [wout_t], q="pool")
    oTt = [T5([128, 16, 512], BF16) for _ in range(1)]
    yT = [T5([128, 8, 512], BF16) for _ in range(1)]
    sgb = [T5([128, 512]) for _ in range(4)]
    xt5 = [T5([128, D]) for _ in range(1)]
    x1t = [T5([128, D]) for _ in range(1)]
    x1_stores = []
    for qi in range(8):
        Q = slice(qi * 512, (qi + 1) * 512)
        HQ = slice(256 + qi * 512, 256 + (qi + 1) * 512)
        hc_a, hc_b = hT_chunks[(256 + qi * 512) // 512], hT_chunks[(256 + qi * 512 + 511) // 512]
        oT_ = oTt[0]
        for hq in range(4):
            DMA(oT_[:, hq * 4:(hq + 1) * 4, :], o_scr[hq * 4:(hq + 1) * 4, :, Q].rearrange("h d t -> d h t"), [o_tok], [oT_])
        yT_ = yT[0]
        for m in range(8):
            pga, pgb, pA, pB = psum[m % 2], psum[2 + m % 2], psum[4], psum[5]
            M = slice(m * 128, (m + 1) * 128)
            mm(pga[:, :], [(wg[:, kc, m * 128:(m + 1) * 128], hT[:, kc, HQ]) for kc in range(8)], [wg, hc_a, hc_b], [pga])
            mm(pgb[:, :], [(wg[:, kc, D + m * 128:D + (m + 1) * 128], hT[:, kc, HQ]) for kc in range(8)], [wg, hc_a, hc_b], [pgb])
            mm(pA[:, :], [(woa_t[:, hh, M], oT_[:, hh, :]) for hh in range(8)], [woa_t, oT_], [pA])
            mm(pB[:, :], [(wob_t[:, hh, M], oT_[:, 8 + hh, :]) for hh in range(8)], [wob_t, oT_], [pB])
            sa, sb = sgb[(m % 2) * 2], sgb[(m % 2) * 2 + 1]
            act(sa.ap, pga[:, :], AF.Sigmoid, [pga], [sa])
            act(sb.ap, pgb[:, :], AF.Sigmoid, [pgb], [sb])
            tt(DVE, sa.ap, sa.ap, pA[:, :], ALU.mult, [sa, pA], [sa])
            tt(DVE, sb.ap, sb.ap, pB[:, :], ALU.mult, [sb, pB], [sb])
            tt(POOL, yT_[:, m, :], sa.ap, sb.ap, ALU.add, [sa, sb], [yT_])
        for sub in range(4):
            tok0 = qi * 512 + sub * 128
            xt_, x1_ = xt5[0], x1t[0]
            DMA(xt_.ap, x_d[tok0:tok0 + 128, :], [], [xt_])
            for n in range(2):
                po = psum[6 + n]
                mm(po[:, :], [(yT_[:, m, sub * 128:(sub + 1) * 128], wout_t[:, m, n * 512:(n + 1) * 512]) for m in range(8)], [yT_, wout_t], [po])
                tt(DVE, x1_[:, n * 512:(n + 1) * 512], po[:, :], g1bc[:, n * 512:(n + 1) * 512], ALU.mult, [po, g1bc], [x1_])
            tt(POOL, x1_.ap, x1_.ap, xt_.ap, ALU.add, [x1_, xt_], [x1_])
            x1_stores.append(DMA(x1_scr[tok0:tok0 + 128, :], x1_.ap, [x1_], []))
    x1_tok = Tile(None)
    bar3 = S.add("sp", None, [], [x1_tok])
    for op in x1_stores:
        bar3.deps.append(op); op.needs_inc = True
    if STAGE == 5:
        return finish(nc, S, es, block, x1_stores)


    S.barrier()
    if SPARSE:
        return sparse_moe(locals())

    T6 = Alloc(HT_OFF)
    rw_t = T6([128, 8, NEXP], BF16); rb_row = T6([1, NEXP], BF16); b1T = T6([128, 16, NEXP])
    b2rows = [T6([1, D], BF16) for _ in range(2)]
    gates = T6([128, 8, NEXP]); lgt = [T6([128, NEXP]) for _ in range(2)]; ext = [T6([128, NEXP]) for _ in range(2)]
    mk = [T6([128, NEXP]) for _ in range(2)]
    top8 = [T6([128, 8]) for _ in range(2)]; sm6 = [T6([128, 2]) for _ in range(2)]
    T6_MID = T6.off
    b1raw = T6([32, 2 * D])
    if not os.environ.get("MK_SKIP_SETUP"):
        DMA(rw_t.ap, rw_d.rearrange("(kc p) e -> p kc e", p=128), [], [rw_t], q="pool")
        DMA(rb_row.ap, rb_d.rearrange("(o e) -> o e", o=1), [], [rb_row], q="pool")
        DMA(b1raw.ap, eb1, [], [b1raw])
    for half in range(0 if os.environ.get("MK_SKIP_B1") else 2):
        pt = psum[half]

        def fnb(e, half=half, pt=pt):
            ins = None
            for f in range(8):
                fc = half * 8 + f
                ins = e.transpose(out=pt[:, f * 32:(f + 1) * 32], in_=b1raw[0:32, fc * 128:(fc + 1) * 128], identity=ident[0:32, 0:32])
            return ins
        PE(fnb, [b1raw, ident], [pt])
        DVE(lambda e, half=half, pt=pt: e.tensor_copy(out=b1T[:, half * 8:(half + 1) * 8, :], in_=pt[:, 0:256].rearrange("p (f e) -> p f e", e=NEXP)), [pt], [b1T])
    ts(DVE, b1T[:, 8:16, :], b1T[:, 8:16, :], 1.0, None, ALU.add, None, [b1T], [b1T])
    S.barrier()
    T6 = Alloc(T6_MID)
    h2T = T6([128, 8, 1024], BF16); h2f = T6([128, 8, 128]); acc = T6([128, 8, D])
    w1b = [T6([128, 8, 2 * D], BF16) for _ in range(2)]; w2t = T6([128, 8, D], BF16)
    actT = [T6([128, 8, 512], BF16) for _ in range(2)]
    ta = [T6([128, 512]) for _ in range(2)]; tsg = [T6([128, 512]) for _ in range(2)]; tl = [T6([128, 512]) for _ in range(2)]
    NB["xt"] = [T6([128, D]) for _ in range(2)]; NB["xn"] = [T6([128, D]) for _ in range(2)]
    h2chunks = [Tile(None) for _ in range(2)]
    final_stores = []
    nm_i = 0
    for g in range(int(os.environ.get("MK_NG", "4"))):
        for sub in range(8):
            tok0 = g * 1024 + sub * 128
            hch = h2chunks[sub // 4]
            if os.environ.get("MK_SKIP_NM"):
                continue
            norm_mod_T(nm_i, x1_scr[tok0:tok0 + 128, :], A2.ap, modT[:, 24:32, 0],
                       lambda j, sub=sub, hch=hch: (h2T[:, j, sub * 128:(sub + 1) * 128], hch),
                       (psum[(nm_i % 2) * 2], psum[(nm_i % 2) * 2 + 1]), src_deps=[x1_tok])
            nm_i += 1
            if os.environ.get("MK_SKIP_RT"):
                continue
            lg_ps = psum[4 + sub % 2]
            mm(lg_ps[:, 0:NEXP], [(h2T[:, kc, sub * 128:(sub + 1) * 128], rw_t[:, kc, :]) for kc in range(8)] + [(ones_b[0:1, :], rb_row[0:1, :])],
               [hch, rw_t, ones_b, rb_row], [lg_ps])
            lg, ex, mk_, t8, sm = lgt[sub % 2], ext[sub % 2], mk[sub % 2], top8[sub % 2], sm6[sub % 2]
            DVE(lambda e, lg=lg, lg_ps=lg_ps: e.tensor_copy(out=lg.ap, in_=lg_ps[:, 0:NEXP]), [lg_ps], [lg])
            DVE(lambda e, t8=t8, lg=lg: e.max(out=t8.ap, in_=lg.ap), [lg], [t8])
            ts(DVE, mk_.ap, lg.ap, t8[:, 3:4], None, ALU.is_ge, None, [lg, t8], [mk_])
            ts(DVE, sm[:, 0:1], t8[:, 0:1], -1.0, None, ALU.mult, None, [t8], [sm])
            act(ex.ap, lg.ap, AF.Exp, [lg, sm], [ex], bias=sm[:, 0:1])
            tt(DVE, ex.ap, ex.ap, mk_.ap, ALU.mult, [ex, mk_], [ex])
            DVE(lambda e, sm=sm, ex=ex: e.reduce_sum(out=sm[:, 1:2], in_=ex.ap, axis=AX.X), [ex], [sm])
            DVE(lambda e, sm=sm: e.reciprocal(out=sm[:, 1:2], in_=sm[:, 1:2]), [sm], [sm])
            ts(DVE, gates[:, sub, :], ex.ap, sm[:, 1:2], None, ALU.mult, None, [ex, sm], [gates])
        for ex_i in range(int(os.environ.get("MK_NE", "32"))):
            w1_ = w1b[ex_i % 2]
            b2r = b2rows[ex_i % 2]
            if not (os.environ.get("MK_NOW") and (g > 0 or ex_i > 1)):
                DMA(w1_.ap, ew1[ex_i].rearrange("(kc p) n -> p kc n", p=128), [], [w1_], q="pool")
                DMA(w2t.ap, ew2[ex_i].rearrange("(kc p) n -> p kc n", p=128), [], [w2t], q="pool")
            DMA(b2r.ap, eb2[ex_i:ex_i + 1, :], [], [b2r], q="pool")
            for rt in range(2):
                hch = h2chunks[rt]
                C = slice(rt * 512, (rt + 1) * 512)
                aT = actT[rt % 2]
                for fc in range(8):
                    pg, pl = psum[fc % 2], psum[2 + fc % 2]
                    a_, s_, l_ = ta[fc % 2], tsg[fc % 2], tl[fc % 2]
                    mm(pg[:, :], [(w1_[:, kc, fc * 128:(fc + 1) * 128], h2T[:, kc, C]) for kc in range(8)], [w1_, hch], [pg])
                    mm(pl[:, :], [(w1_[:, kc, D + fc * 128:D + (fc + 1) * 128], h2T[:, kc, C]) for kc in range(8)], [w1_, hch], [pl])
                    ts(DVE, a_.ap, pg[:, :], b1T[:, fc, ex_i:ex_i + 1], 7.0, ALU.add, ALU.min, [pg, b1T], [a_])
                    act(s_.ap, a_.ap, AF.Sigmoid, [a_], [s_], scale=1.702)
                    act(l_.ap, pl[:, :], AF.Identity, [pl, b1T], [l_], bias=b1T[:, 8 + fc, ex_i:ex_i + 1])
                    ts(DVE, l_.ap, l_.ap, -6.0, 8.0, ALU.max, ALU.min, [l_], [l_])
                    tt(DVE, s_.ap, a_.ap, s_.ap, ALU.mult, [a_, s_], [s_])
                    tt(POOL, aT[:, fc, :], s_.ap, l_.ap, ALU.mult, [s_, l_], [aT])
                for s4 in range(4):
                    sub = rt * 4 + s4
                    for n in range(2):
                        py = psum[4 + (s4 % 2) * 2 + n]
                        N = slice(n * 512, (n + 1) * 512)
                        mm(py[:, :], [(aT[:, fc, s4 * 128:(s4 + 1) * 128], w2t[:, fc, N]) for fc in range(8)] + [(ones_b[0:1, :], b2r[0:1, N])],
                           [aT, w2t, ones_b, b2r], [py])
                        if ex_i == 0:
                            ts(DVE, acc[:, sub, N], py[:, :], gates[:, sub, ex_i:ex_i + 1], None, ALU.mult, None, [py, gates], [acc])
                        else:
                            stt(DVE, acc[:, sub, N], py[:, :], gates[:, sub, ex_i:ex_i + 1], acc[:, sub, N], ALU.mult, ALU.add, [py, gates, acc], [acc])
        for sub in range(8):
            tok0 = g * 1024 + sub * 128
            xt_ = NB["xt"][sub % 2]
            DMA(xt_.ap, x1_scr[tok0:tok0 + 128, :], [x1_tok], [xt_])
            tt(DVE, acc[:, sub, :], acc[:, sub, :], g2bc.ap, ALU.mult, [acc, g2bc], [acc])
            tt(POOL, xt_.ap, xt_.ap, acc[:, sub, :], ALU.add, [xt_, acc], [xt_])
            final_stores.append(DMA(out_d[tok0:tok0 + 128, :], xt_.ap, [xt_], []))
    return finish(nc, S, es, block, final_stores)


def sparse_moe(L):
    g = dict(L)
    nc, S, es, block, Alloc, psum = g["nc"], g["S"], g["es"], g["block"], g["Alloc"], g["psum"]
    DMA, PE, ACT, DVE, POOL, mm, ts, tt, stt, act = (g[k] for k in ("DMA", "PE", "ACT", "DVE", "POOL", "mm", "ts", "tt", "stt", "act"))
    din, dscr, norm_mod_T, NB, finish = g["din"], g["dscr"], g["norm_mod_T"], g["NB"], finish_
    ident, ones_f, ones_b, modT, A2, g2bc, rs1 = g["ident"], g["ones_f"], g["ones_b"], g["modT"], g["A2"], g["g2bc"], g["rs1"]
    x1_scr, x1_tok, out_d, ab_scr = g["x1_scr"], g["x1_tok"], g["out_d"], g["ab_scr"]
    rw_d, rb_d, eb1, eb2, ew1, ew2 = g["rw_d"], g["rb_d"], g["eb1"], g["eb2"], g["ew1"], g["ew2"]
    IOA = bass.IndirectOffsetOnAxis

    NROWS = NBLK * BLK
    xs_d = g["xs_d"]
    ys_d = dscr("ys_scr", [NROWS, D], BF16)
    consts_d = g["consts_d"]
    ew1f = ew1.rearrange("e k n -> (e k) n"); ew2f = ew2.rearrange("e k n -> (e k) n")

    def IDMA(gather, dram, idx_ap, sb_ap, r, w):
        def fn(e):
            if gather:
                return e.indirect_dma_start(out=sb_ap, out_offset=None, in_=dram, in_offset=IOA(ap=idx_ap, axis=0))
            return e.indirect_dma_start(out=dram, out_offset=IOA(ap=idx_ap, axis=0), in_=sb_ap, in_offset=None)
        return S.add("pool", fn, r, w, dma=True)

    T6 = Alloc(g["HT_OFF"])
    cst = T6([128, NCONST])
    iota_r, pk = cst[:, 0:32], cst[:, 64:72]
    ustr_f = cst[:, 76:204]
    ustr_b = T6([128, 128], BF16); ident_b = T6([128, 128], BF16)
    rw_t = T6([128, 8, NEXP], BF16); rb_row = T6([1, NEXP], BF16)
    b1raw = T6([32, 2 * D]); b2_t = T6([32, D])
    A2bc = T6([128, D]); S2bc = T6([128, D])
    tot = T6([128, NEXP]); gates_all = T6([128, 32, NEXP]); gk_all = T6([128, 32, 4]); slots_u = T6([128, 32, 4], U32)
    ebv = T6([128, NBLK]); ohcol = T6([32, NBLK]); w_u = T6([128, 8, NBLK], U32)
    T6_MID = T6.off
    DMA(cst.ap, consts_d, [], [cst])
    DMA(rw_t.ap, rw_d.rearrange("(kc p) e -> p kc e", p=128), [], [rw_t], q="pool")
    DMA(rb_row.ap, rb_d.rearrange("(o e) -> o e", o=1), [], [rb_row], q="pool")
    DMA(b1raw.ap, eb1, [], [b1raw]); DMA(b2_t.ap, eb2, [], [b2_t])
    DMA(A2bc.ap, ab_scr[1], [], [A2bc]); DMA(S2bc.ap, ab_scr[0], [], [S2bc])
    ts(DVE, b1raw[:, D:2 * D], b1raw[:, D:2 * D], 1.0, None, ALU.add, None, [b1raw], [b1raw])
    DVE(lambda e: e.tensor_copy(out=ustr_b.ap, in_=ustr_f), [cst], [ustr_b])
    DVE(lambda e: e.tensor_copy(out=ident_b.ap, in_=ident.ap), [ident], [ident_b])
    POOL(lambda e: e.memset(tot.ap, 0.0), [], [tot])

    TA = Alloc(T6_MID)
    h2Tt = [TA([128, 8, 128], BF16) for _ in range(4)]
    h2tok = [TA([128, D], BF16) for _ in range(32)]
    tmpf = [TA([128, D]) for _ in range(3)]
    NB["xt"] = [TA([128, D]) for _ in range(4)]; NB["xn"] = [TA([128, D]) for _ in range(3)]
    lg_all = TA([128, 32, NEXP]); t8_all = TA([128, 32, 8]); posw = TA([128, 32, NEXP])
    ext = [TA([128, NEXP]) for _ in range(4)]; mkt = [TA([128, NEXP]) for _ in range(4)]
    mkb = [TA([128, NEXP], BF16) for _ in range(4)]; slotm = [TA([128, NEXP]) for _ in range(2)]
    sm6 = [TA([128, 2]) for _ in range(4)]
    oht = [TA([128, NEXP]) for _ in range(2)]; t32 = [TA([128, NEXP]) for _ in range(4)]
    slf = [TA([128, 4]) for _ in range(2)]
    zfill = g["zfill_ops"]
    for i in range(32):
        tok0 = i * 128
        hT_ = h2Tt[i % 4]
        xt_ = norm_mod_T(i, x1_scr[tok0:tok0 + 128, :], A2.ap, modT[:, 24:32, 0],
                         lambda j, hT_=hT_: (hT_[:, j, :], hT_), (psum[(i % 2) * 2], psum[(i % 2) * 2 + 1]), src_deps=[x1_tok])
        rs_ = rs1[:, i % 4:i % 4 + 1]
        tf, hk = tmpf[i % 3], h2tok[i]
        stt(DVE, tf.ap, xt_.ap, rs_, A2bc.ap, ALU.mult, ALU.mult, [xt_, rs1, A2bc], [tf])
        tt(POOL, hk.ap, tf.ap, S2bc.ap, ALU.add, [tf, S2bc], [hk])
        lg_ps = psum[4 + i % 2]
        mm(lg_ps[:, 0:NEXP], [(hT_[:, kc, :], rw_t[:, kc, :]) for kc in range(8)] + [(ones_b[0:1, :], rb_row[0:1, :])],
           [hT_, rw_t, ones_b, rb_row], [lg_ps])
        ex, mk_, sm, mb = ext[i % 4], mkt[i % 4], sm6[i % 4], mkb[i % 4]
        lg_i, t8_i = lg_all[:, i, :], t8_all[:, i, :]
        DVE(lambda e, lg_i=lg_i, lg_ps=lg_ps: e.tensor_copy(out=lg_i, in_=lg_ps[:, 0:NEXP]), [lg_ps], [lg_all])
        DVE(lambda e, t8_i=t8_i, lg_i=lg_i: e.max(out=t8_i, in_=lg_i), [lg_all], [t8_all])
        ts(DVE, mk_.ap, lg_i, t8_all[:, i, 3:4], None, ALU.is_ge, None, [lg_all, t8_all], [mk_])
        ts(DVE, sm[:, 0:1], t8_all[:, i, 0:1], -1.0, None, ALU.mult, None, [t8_all], [sm])
        act(ex.ap, lg_i, AF.Exp, [lg_all, sm], [ex], bias=sm[:, 0:1])
        tt(DVE, ex.ap, ex.ap, mk_.ap, ALU.mult, [ex, mk_], [ex])
        DVE(lambda e, sm=sm, ex=ex: e.reduce_sum(out=sm[:, 1:2], in_=ex.ap, axis=AX.X), [ex], [sm])
        DVE(lambda e, sm=sm: e.reciprocal(out=sm[:, 1:2], in_=sm[:, 1:2]), [sm], [sm])
        ts(DVE, gates_all[:, i, :], ex.ap, sm[:, 1:2], None, ALU.mult, None, [ex, sm], [gates_all])
        DVE(lambda e, mb=mb, mk_=mk_: e.tensor_copy(out=mb.ap, in_=mk_.ap), [mk_], [mb])
        pos_ps, cs_ps = psum[6], psum[7]
        mm(pos_ps[:, 0:NEXP], [(ustr_b.ap, mb.ap)], [ustr_b, mb], [pos_ps])
        mm(cs_ps[:, 0:NEXP], [(ones_b.ap, mb.ap)], [ones_b, mb], [cs_ps])
        tt(DVE, posw[:, i, :], pos_ps[:, 0:NEXP], tot.ap, ALU.add, [pos_ps, tot], [posw])
        tt(DVE, tot.ap, tot.ap, cs_ps[:, 0:NEXP], ALU.add, [tot, cs_ps], [tot])

    tq, tm, pcv, pstart = (TA([128, NEXP]) for _ in range(4))
    pp = [TA([128, NEXP]) for _ in range(2)]
    cmpt = [TA([128, NEXP]) for _ in range(2)]
    wf = TA([128, 8, NBLK])
    ts(DVE, tq.ap, tot.ap, 0.0, None, ALU.is_gt, None, [tot], [tq])
    for m_ in range(1, ECAP // BLK):
        ts(DVE, tm.ap, tot.ap, float(m_ * BLK), None, ALU.is_gt, None, [tot], [tm])
        tt(DVE, tq.ap, tq.ap, tm.ap, ALU.add, [tq, tm], [tq])
    ts(DVE, pcv.ap, tq.ap, float(BLK), None, ALU.mult, None, [tq], [pcv])
    DVE(lambda e: e.tensor_copy(out=pp[0].ap, in_=pcv.ap), [pcv], [pp[0]])
    cur = 0
    for sh in (1, 2, 4, 8, 16):
        a_, b_ = pp[cur], pp[1 - cur]
        DVE(lambda e, a_=a_, b_=b_: e.tensor_copy(out=b_.ap, in_=a_.ap), [a_], [b_])
        tt(DVE, b_[:, sh:NEXP], a_[:, sh:NEXP], a_[:, 0:NEXP - sh], ALU.add, [a_], [b_])
        cur = 1 - cur
    pend = pp[cur]
    tt(DVE, pstart.ap, pend.ap, pcv.ap, ALU.subtract, [pend, pcv], [pstart])
    for b in range(NBLK):
        c_ = cmpt[b % 2]
        ts(DVE, c_.ap, pend.ap, float(b * BLK), None, ALU.is_le, None, [pend], [c_])
        DVE(lambda e, c_=c_, b=b: e.reduce_sum(out=ebv[:, b:b + 1], in_=c_.ap, axis=AX.X), [c_], [ebv])
    ts(DVE, ebv.ap, ebv.ap, float(NEXP - 1), None, ALU.min, None, [ebv], [ebv])
    for kc in range(8):
        ts(DVE, wf[:, kc, :], ebv.ap, float(D), pk[:, kc:kc + 1], ALU.mult, ALU.add, [ebv, cst], [wf])
        DVE(lambda e, kc=kc: e.tensor_copy(out=w_u[:, kc, :], in_=wf[:, kc, :]), [wf], [w_u])
    ts(DVE, ohcol.ap, ebv[0:32, :], cst[0:32, 72:73], None, ALU.is_equal, None, [ebv, cst], [ohcol])

    zb = S.add("pool", None, [], [])
    for op in zfill:
        zb.deps.append(op)
    scat = []
    for i in range(32):
        sl, sf, hk = slotm[i % 2], slf[i % 2], h2tok[i]
        tt(DVE, sl.ap, posw[:, i, :], pstart.ap, ALU.add, [posw, pstart], [sl])
        for k in range(4):
            oh, ta_, tb_ = oht[k % 2], t32[(k % 2) * 2], t32[(k % 2) * 2 + 1]
            ts(DVE, oh.ap, lg_all[:, i, :], t8_all[:, i, k:k + 1], None, ALU.is_equal, None, [lg_all, t8_all], [oh])
            tt(DVE, ta_.ap, oh.ap, sl.ap, ALU.mult, [oh, sl], [ta_])
            DVE(lambda e, sf=sf, ta_=ta_, k=k: e.reduce_sum(out=sf[:, k:k + 1], in_=ta_.ap, axis=AX.X), [ta_], [sf])
            tt(DVE, tb_.ap, oh.ap, gates_all[:, i, :], ALU.mult, [oh, gates_all], [tb_])
            DVE(lambda e, tb_=tb_, k=k, i=i: e.reduce_sum(out=gk_all[:, i, k:k + 1], in_=tb_.ap, axis=AX.X), [tb_], [gk_all])
        DVE(lambda e, sf=sf, i=i: e.tensor_copy(out=slots_u[:, i, :], in_=sf.ap), [sf], [slots_u])
        for k in range(4):
            scat.append(IDMA(False, xs_d, slots_u[:, i, k:k + 1], hk.ap, [hk, slots_u], []))
    xs_tok = Tile(None)
    bs = S.add("sp", None, [], [xs_tok])
    for op in scat:
        bs.deps.append(op)
    S.barrier()

    TB = Alloc(T6_MID)
    w1b = [TB([128, 8, 2 * D], BF16) for _ in range(2)]; w2b = [TB([128, 8, D], BF16) for _ in range(2)]
    xrows = [TB([128, 4, D], BF16) for _ in range(2)]; xTb = [TB([128, 8, BLK], BF16) for _ in range(2)]
    actT = [TB([128, 8, BLK], BF16) for _ in range(2)]
    ta = [TB([128, 512]) for _ in range(2)]; tsg = [TB([128, 512]) for _ in range(2)]; tl = [TB([128, 512]) for _ in range(2)]
    ysb = [TB([128, D], BF16) for _ in range(2)]
    b1c = [TB([128, 16]) for _ in range(2)]
    ysc = []
    def loads_xw1(b):
        xr, w1_ = xrows[b % 2], w1b[b % 2]
        DMA(xr.ap, xs_d[b * BLK:(b + 1) * BLK, :].rearrange("(j p) d -> p j d", p=128), [xs_tok], [xr])
        for kc in range(8):
            IDMA(True, ew1f, w_u[:, kc, b:b + 1], w1_[:, kc, :], [w_u], [w1_])

    def loads_w2(b):
        w2_ = w2b[b % 2]
        for kc in range(8):
            IDMA(True, ew2f, w_u[:, kc, b:b + 1], w2_[:, kc, :], [w_u], [w2_])

    def stage_pre(b):
        xr, xT_, bc = xrows[b % 2], xTb[b % 2], b1c[b % 2]
        pb = psum[4 + b % 2]

        def fb(e, pb=pb, b=b):
            ins = None
            for fc in range(16):
                ins = e.matmul(pb[:, fc:fc + 1], b1raw[0:32, fc * 128:(fc + 1) * 128], ohcol[0:32, b:b + 1], start=True, stop=True)
            return ins
        PE(fb, [b1raw, ohcol], [pb])
        DVE(lambda e, bc=bc, pb=pb: e.tensor_copy(out=bc.ap, in_=pb[:, 0:16]), [pb], [bc])
        for j in range(4):
            for half in range(2):
                pt = psum[6 + half]
                ptb = pt.ap.bitcast(BF16)

                def ft(e, ptb=ptb, xr=xr, j=j, half=half):
                    ins = None
                    for q in range(4):
                        kc = half * 4 + q
                        ins = e.transpose(out=ptb[:, q * 128:(q + 1) * 128], in_=xr[:, j, kc * 128:(kc + 1) * 128], identity=ident_b.ap)
                    return ins
                PE(ft, [xr, ident_b], [pt])
                src = ptb[:, 0:512].rearrange("p (q r) -> p q r", q=4)
                dst = xT_[:, half * 4:(half + 1) * 4, j * 128:(j + 1) * 128]
                if half == 0:
                    ACT(lambda e, dst=dst, src=src: e.copy(out=dst, in_=src), [pt], [xT_])
                else:
                    DVE(lambda e, dst=dst, src=src: e.tensor_copy(out=dst, in_=src), [pt], [xT_])

    def stage_w1(b):
        xT_, w1_, aT, bc = xTb[b % 2], w1b[b % 2], actT[b % 2], b1c[b % 2]
        for fc in range(8):
            pg, pl = psum[fc % 2], psum[2 + fc % 2]
            a_, s_, l_ = ta[fc % 2], tsg[fc % 2], tl[fc % 2]
            mm(pg[:, :], [(w1_[:, kc, fc * 128:(fc + 1) * 128], xT_[:, kc, :]) for kc in range(8)], [w1_, xT_], [pg])
            mm(pl[:, :], [(w1_[:, kc, D + fc * 128:D + (fc + 1) * 128], xT_[:, kc, :]) for kc in range(8)], [w1_, xT_], [pl])
            ts(DVE, a_.ap, pg[:, :], bc[:, fc:fc + 1], 7.0, ALU.add, ALU.min, [pg, bc], [a_])
            act(s_.ap, a_.ap, AF.Sigmoid, [a_], [s_], scale=1.702)
            act(l_.ap, pl[:, :], AF.Identity, [pl, bc], [l_], bias=bc[:, 8 + fc:9 + fc])
            ts(DVE, l_.ap, l_.ap, -6.0, 8.0, ALU.max, ALU.min, [l_], [l_])
            tt(DVE, s_.ap, a_.ap, s_.ap, ALU.mult, [a_, s_], [s_])
            tt(POOL, aT[:, fc, :], s_.ap, l_.ap, ALU.mult, [s_, l_], [aT])

    def stage_w2(b):
        w2_, aT = w2b[b % 2], actT[b % 2]
        for j in range(4):
            yb = ysb[j % 2]
            for n in range(2):
                py = psum[4 + (j % 2) * 2 + n]
                N = slice(n * 512, (n + 1) * 512)
                mm(py[:, :], [(aT[:, fc, j * 128:(j + 1) * 128], w2_[:, fc, N]) for fc in range(8)], [aT, w2_], [py])
                if n == 0:
                    ACT(lambda e, yb=yb, py=py, N=N: e.copy(out=yb[:, N], in_=py[:, :]), [py], [yb])
                else:
                    DVE(lambda e, yb=yb, py=py, N=N: e.tensor_copy(out=yb[:, N], in_=py[:, :]), [py], [yb])
            ysc.append(DMA(ys_d[b * BLK + j * 128:b * BLK + (j + 1) * 128, :], yb.ap, [yb], []))

    loads_xw1(0); loads_w2(0)
    stage_pre(0)
    for b in range(NBLK):
        if b + 1 < NBLK:
            loads_xw1(b + 1)
        stage_w1(b)
        if b >= 1:
            stage_w2(b - 1)
        if b + 1 < NBLK:
            loads_w2(b + 1)
            stage_pre(b + 1)
    stage_w2(NBLK - 1)
    ys_tok = Tile(None)
    bs2 = S.add("pool", None, [], [ys_tok])
    for op in ysc:
        bs2.deps.append(op)
    S.barrier()

    TC = Alloc(T6_MID)
    yk = [TC([128, D], BF16) for _ in range(8)]
    accc = [TC([128, D]) for _ in range(2)]
    gT = [TC([32, 128]) for _ in range(2)]
    xt6 = [TC([128, D]) for _ in range(2)]
    final_stores = []
    for i in range(32):
        tok0 = i * 128
        ac, gt, xt_ = accc[i % 2], gT[i % 2], xt6[i % 2]
        for k in range(4):
            IDMA(True, ys_d, slots_u[:, i, k:k + 1], yk[(i % 2) * 4 + k].ap, [ys_tok, slots_u], [yk[(i % 2) * 4 + k]])
        DMA(xt_.ap, x1_scr[tok0:tok0 + 128, :], [x1_tok], [xt_])
        pgt = psum[i % 2]
        PE(lambda e, pgt=pgt, i=i: e.transpose(out=pgt[0:32, 0:128], in_=gates_all[:, i, :], identity=ident.ap), [gates_all, ident], [pgt])
        DVE(lambda e, gt=gt, pgt=pgt: e.tensor_copy(out=gt.ap, in_=pgt[0:32, 0:128]), [pgt], [gt])
        for n in range(2):
            pbias = psum[2 + (i % 2) * 2 + n]
            N = slice(n * 512, (n + 1) * 512)
            mm(pbias[:, :], [(gt[0:32, :], b2_t[0:32, N])], [gt, b2_t], [pbias])
            stt(DVE, ac[:, N], yk[(i % 2) * 4][:, N], gk_all[:, i, 0:1], pbias[:, :], ALU.mult, ALU.add, [yk[(i % 2) * 4], gk_all, pbias], [ac])
        for k in range(1, 4):
            y_ = yk[(i % 2) * 4 + k]
            stt(DVE, ac.ap, y_.ap, gk_all[:, i, k:k + 1], ac.ap, ALU.mult, ALU.add, [y_, gk_all, ac], [ac])
        tt(DVE, ac.ap, ac.ap, g2bc.ap, ALU.mult, [ac, g2bc], [ac])
        tt(DVE, xt_.ap, xt_.ap, ac.ap, ALU.add, [xt_, ac], [xt_])
        final_stores.append(DMA(out_d[tok0:tok0 + 128, :], xt_.ap, [xt_], []))
    return finish(nc, S, es, block, final_stores)


def finish_(nc, S, es, block, final_deps):
    return finish(nc, S, es, block, final_deps)


def finish(nc, S, es, block, final_deps):
    fin = S.add("sp", None, [], [])
    for d in final_deps:
        fin.deps.append(d)
        d.needs_inc = True
    S.emit(nc, es, block)
    es.close()
    return nc, S


_CONSTS = None


def rope_tables():
    theta = 10000.0
    n_rows = SEQ // 64
    row = np.repeat(np.arange(n_rows), 64).astype(np.float32)
    col = np.tile(np.arange(64), n_rows).astype(np.float32)

    def tab(half):
        nf = half // 2
        inv = (theta ** (-(np.arange(0, half, 2, dtype=np.float32)) / half)).astype(np.float32)
        cos = np.ones((2 * half, NT), np.float32)
        sin = np.zeros((2 * half, NT), np.float32)
        for blk, pos in enumerate((row, col)):
            ang = (pos[None, :] * inv[:, None]).astype(np.float32)
            c, s = np.cos(ang).astype(np.float32), np.sin(ang).astype(np.float32)
            base = blk * half
            cos[base:base + nf, NCTX:] = c
            cos[base + nf:base + half, NCTX:] = c
            sin[base:base + nf, NCTX:] = s
            sin[base + nf:base + half, NCTX:] = s
        return cos, sin
    cA, sA = tab(64)
    cB, sB = tab(32)
    return cA, sA, cB, sB


def kernel(**inputs):
    cA, sA, cB, sB = rope_tables()
    nc, S = build_program()
    g = lambda k: np.ascontiguousarray(np.asarray(inputs[k], dtype=np.float32)[0])
    shared = {
        "ada_w": g("ada_w"), "ada_b": g("ada_b"), "norm_mix": g("norm_mix"), "norm_ffn": g("norm_ffn"),
        "w_in": g("w_in"), "gqa_q_norm": g("gqa_q_norm"), "gqa_k_norm": g("gqa_k_norm"),
        "mla_q_a_norm": g("mla_q_a_norm"), "mla_kv_a_norm": g("mla_kv_a_norm"),
        "mla_w_qb": g("mla_w_qb"), "mla_w_kvb": g("mla_w_kvb"), "mla_q_norm": g("mla_q_norm"),
        "mla_k_norm": g("mla_k_norm"), "w_o_gqa": g("w_o_gqa"), "w_o_mla": g("w_o_mla"), "w_out": g("w_out"),
        "router_w": g("router_w"), "router_b": g("router_b"), "expert_w1": g("expert_w1"),
        "expert_b1": g("expert_b1"), "expert_w2": g("expert_w2"), "expert_b2": g("expert_b2"),
        "ident": np.eye(128, dtype=np.float32), "sel0": np.stack([np.ones(128, np.float32), np.zeros(128, np.float32)]), "cosA": cA, "sinA": sA, "cosB": cB, "sinB": sB,
    }
    if SPARSE:
        cst = np.zeros((128, NCONST), np.float32)
        pidx = np.arange(128, dtype=np.float32)
        cst[:, 0:32] = np.arange(32, dtype=np.float32)[None, :]
        cst[:, 32:64] = (np.arange(32, dtype=np.float32) * ECAP)[None, :]
        cst[:, 64:72] = pidx[:, None] + 128.0 * np.arange(8, dtype=np.float32)[None, :]
        cst[:, 72:76] = pidx[:, None] + 128.0 * np.arange(4, dtype=np.float32)[None, :]
        cst[:, 76:204] = (pidx[:, None] < pidx[None, :]).astype(np.float32)
        cst[:, 204:204 + NBLK] = (np.arange(NBLK, dtype=np.float32) * BLK)[None, :]
        shared["consts"] = cst
    x = np.asarray(inputs["x"], np.float32); c = np.asarray(inputs["c"], np.float32)
    ctx = np.asarray(inputs["ctx"], np.float32); c_ctx = np.asarray(inputs["c_ctx"], np.float32)
    ncores = int(os.environ.get("MK_CORES", "8"))
    in_maps = []
    for b in range(ncores):
        m = dict(shared)
        m["x"] = np.ascontiguousarray(x[b]); m["ctx"] = np.ascontiguousarray(ctx[b])
        m["cc"] = np.ascontiguousarray(np.stack([c[b], c_ctx], 0))
        in_maps.append(m)
    res = run_bass_kernel_spmd(nc, in_maps, core_ids=list(range(ncores)))
    if STAGE < 99:
        return res.results
    if ncores < 8:
        return [r["out"] for r in res.results]
    return np.stack([r["out"] for r in res.results], 0).astype(np.float32)
```

```python
import os
import numpy as np
from contextlib import ExitStack
import concourse.bass as bass
import concourse.mybir as mybir
from concourse.bass_utils import run_bass_kernel_spmd

F32 = mybir.dt.float32
BF16 = mybir.dt.bfloat16
ALU = mybir.AluOpType
AF = mybir.ActivationFunctionType
AX = mybir.AxisListType
U32 = mybir.dt.uint32
SPARSE = os.environ.get("MK_SPARSE", "1") == "1"
BLK = 512
NBLK = 16384 // BLK + 32
ECAP = 4096
ZROW = 32 * ECAP
NCONST = 76 + 128 + NBLK

D = 1024
SEQ = 4096
NCTX = 256
NT = SEQ + NCTX
EPS = 1e-6
NEXP = 32
STAGE = int(os.environ.get("MK_STAGE", "99"))


class Tok:
    __slots__ = ("w", "wd", "rs", "rd")

    def __init__(self):
        self.w = None
        self.wd = []
        self.rs = {}
        self.rd = []


class Tile:
    def __init__(self, ap):
        self.ap = ap
        self.tok = Tok()

    def __getitem__(self, k):
        return self.ap[k]


class Op:
    __slots__ = ("eng", "fn", "deps", "dma", "needs_inc", "seg", "val", "sem", "barred", "seq")


ENGS = ("pe", "act", "dve", "pool", "sp")
SEG = 4000
NDMASEM = 40


class Sched:
    def __init__(self):
        self.ops = {e: [] for e in ENGS}
        self.nseq = 0

    def add(self, eng, fn, reads=(), writes=(), dma=False):
        op = Op()
        op.eng, op.fn, op.dma, op.deps, op.needs_inc = eng, fn, dma, [], False
        op.seg = op.val = op.sem = None
        seen = set()

        def dep(d, raw):
            if d is None or id(d) in seen:
                return
            if d.fn is None:
                if d.eng != eng:
                    for dd in d.deps:
                        dep(dd, raw)
                return
            if not d.dma and not dma and d.eng == eng:
                if eng == "pe" or raw is None:
                    return
            seen.add(id(d))
            op.deps.append(d)
            d.needs_inc = True

        for t in reads:
            dep(t.tok.w, True)
            for d in t.tok.wd:
                dep(d, True)
        for t in writes:
            k = t.tok
            if dma:
                if k.w is not None:
                    dep(k.w, None)
            else:
                dep(k.w, None)
                for d in k.wd:
                    dep(d, None)
            for r in k.rs.values():
                dep(r, False)
            for r in k.rd:
                dep(r, False)
        for t in reads:
            if dma:
                t.tok.rd.append(op)
            else:
                t.tok.rs[eng] = op
        for t in writes:
            k = t.tok
            had_readers = bool(k.rs) or bool(k.rd)
            if dma:
                if had_readers or k.w is not None:
                    k.w, k.wd = None, [op]
                else:
                    k.wd.append(op)
            else:
                k.w, k.wd = op, []
            k.rs, k.rd = {}, []
        op.seq = self.nseq
        self.nseq += 1
        self.ops[eng].append(op)
        return op

    def barrier(self):
        lasts = []
        for e in ENGS:
            for op in reversed(self.ops[e]):
                if not op.dma and op.fn is not None:
                    lasts.append(op)
                    break
        dmas = [op for e in ENGS for op in self.ops[e] if op.dma and not getattr(op, "barred", False)]
        for op in dmas:
            op.barred = True
        for e in ENGS:
            b = self.add(e, None, [], [])
            for d in lasts:
                if d.eng != e:
                    b.deps.append(d); d.needs_inc = True
            for d in dmas:
                b.deps.append(d)

    def emit(self, nc, es, block):
        dsems = [es.enter_context(nc.semaphore("dq%d" % i)) for i in range(NDMASEM)]
        dcount = [0] * NDMASEM
        csems = {}
        pools = {"sp": list(range(0, 24)), "pool": list(range(24, 36)), "act": list(range(36, 40))}
        for e in ENGS:
            cnt = 0
            di = 0
            for op in self.ops[e]:
                if op.dma:
                    pl = pools[e]
                    op.sem = pl[di % len(pl)]
                    dcount[op.sem] += 16
                    op.val = dcount[op.sem]
                    di += 1
                elif op.needs_inc:
                    cnt += 1
                    op.seg, op.val = (cnt - 1) // SEG, (cnt - 1) % SEG + 1
                    if (e, op.seg) not in csems:
                        csems[(e, op.seg)] = es.enter_context(nc.semaphore("c_%s_%d" % (e, op.seg)))
        self.n_inst = {e: len(self.ops[e]) for e in ENGS}

        def run(e, eng):
            waited = {}
            nw = 0
            for op in self.ops[e]:
                for d in op.deps:
                    if d.dma:
                        key, val, sem = ("d", d.sem), d.val, dsems[d.sem]
                        if waited.get(key, 0) >= val:
                            continue
                        waited[key] = val
                    else:
                        key, val, sem = ("c", d.eng), (d.seg, d.val), csems[(d.eng, d.seg)]
                        if waited.get(key, (-1, 0)) >= val:
                            continue
                        waited[key] = val
                        val = d.val
                    eng.wait_ge(sem, val)
                    nw += 1
                if op.dma and op.val > 16:
                    key = ("d", op.sem)
                    if waited.get(key, 0) < op.val - 16:
                        eng.wait_ge(dsems[op.sem], op.val - 16)
                        waited[key] = op.val - 16
                if op.fn is None:
                    continue
                ins = op.fn(eng)
                if op.dma:
                    ins.then_inc(dsems[op.sem], 16)
                elif op.needs_inc:
                    ins.then_inc(csems[(e, op.seg)], 1)
            self.n_inst[e] = (len(self.ops[e]), nw)

        @block.tensor
        def _(eng):
            run("pe", eng)

        @block.scalar
        def _(eng):
            run("act", eng)

        @block.vector
        def _(eng):
            run("dve", eng)

        @block.gpsimd
        def _(eng):
            run("pool", eng)

        @block.sync
        def _(eng):
            run("sp", eng)


def build_program():
    nc = bass.Bass("TRN2", target_bir_lowering=False)
    S = Sched()
    es = ExitStack()

    def din(name, shape, dt=F32):
        return nc.dram_tensor(name, list(shape), dt, kind="ExternalInput").ap()

    def dscr(name, shape, dt=BF16):
        if STAGE in (2, 3):
            return nc.dram_tensor(name, list(shape), dt, kind="ExternalOutput").ap()
        return nc.dram_tensor(name, list(shape), dt).ap()

    x_d = din("x", [SEQ, D]); ctx_d = din("ctx", [NCTX, D]); cc_d = din("cc", [2, D])
    ada_w = din("ada_w", [D, 6 * D]); ada_b = din("ada_b", [6 * D])
    norm_mix = din("norm_mix", [D]); norm_ffn = din("norm_ffn", [D])
    w_in = din("w_in", [D, 4032])
    gq_d = din("gqa_q_norm", [128]); gk_d = din("gqa_k_norm", [128])
    gqa_d = din("mla_q_a_norm", [256]); gkva_d = din("mla_kv_a_norm", [128])
    w_qb = din("mla_w_qb", [256, 1536]); w_kvb = din("mla_w_kvb", [128, 2048])
    gmq_d = din("mla_q_norm", [192]); gmk_d = din("mla_k_norm", [192])
    w_oa = din("w_o_gqa", [D, D]); w_ob = din("w_o_mla", [D, D]); w_out = din("w_out", [D, D])
    rw_d = din("router_w", [D, NEXP]); rb_d = din("router_b", [NEXP])
    ew1 = din("expert_w1", [NEXP, D, 2 * D]); eb1 = din("expert_b1", [NEXP, 2 * D])
    ew2 = din("expert_w2", [NEXP, D, D]); eb2 = din("expert_b2", [NEXP, D])
    ident_d = din("ident", [128, 128]); sel0_d = din("sel0", [2, 128])
    cosA_d = din("cosA", [128, NT]); sinA_d = din("sinA", [128, NT])
    cosB_d = din("cosB", [64, NT]); sinB_d = din("sinB", [64, NT])
    consts_d = din("consts", [128, NCONST]) if SPARSE else None
    out_d = nc.dram_tensor("out", [SEQ, D], F32, kind="ExternalOutput").ap()

    qa_scr = dscr("qa_scr", [8, 128, SEQ]); ka_scr = dscr("ka_scr", [2, 128, NT])
    va_scr = dscr("va_scr", [2, 128, 34, 128])
    qb_scr = dscr("qb_scr", [8, 128, SEQ]); qbr_scr = dscr("qbr_scr", [8, 64, SEQ])
    kb_scr = dscr("kb_scr", [8, 128, NT]); kbr_scr = dscr("kbr_scr", [8, 64, NT])
    vb_scr = dscr("vb_scr", [8, 128, 34, 128])
    o_scr = (nc.dram_tensor("o_scr", [16, 128, SEQ], BF16, kind="ExternalOutput").ap() if STAGE in (4, 5) else dscr("o_scr", [16, 128, SEQ]))
    x1_scr = out_d if STAGE == 5 else dscr("x1_scr", [SEQ, D], F32)
    dbg_d = None
    if STAGE < 99:
        dbg_d = nc.dram_tensor("dbg", [128, 8 * NT], F32, kind="ExternalOutput").ap()

    ARENA_F = 52400
    arena = es.enter_context(nc.sbuf_tensor("arena", [128, ARENA_F], F32))
    psum = [Tile(es.enter_context(nc.psum_tensor("ps%d" % i, [128, 512], F32))[:, :]) for i in range(8)]
    block = es.enter_context(nc.Block())

    class Alloc:
        def __init__(self, base=0):
            self.off = base

        def __call__(self, shape, dt=F32, parts=128):
            n = int(np.prod(shape[1:]))
            nf = n if dt in (F32, U32) else (n + 1) // 2
            nf = (nf + 7) // 8 * 8
            a = arena[:, self.off:self.off + nf]
            self.off += nf
            assert self.off <= ARENA_F, "SBUF arena overflow %d" % self.off
            if dt != F32:
                a = a.bitcast(dt)
            a = a[:, 0:n]
            a = a[0:shape[0]]
            if len(shape) == 3:
                a = a.rearrange("p (a b) -> p a b", a=shape[1])
            elif len(shape) == 4:
                a = a.rearrange("p (a b c) -> p a b c", a=shape[1], b=shape[2])
            return Tile(a)

    def PE(fn, r, w): S.add("pe", fn, r, w)
    def ACT(fn, r, w): S.add("act", fn, r, w)
    def DVE(fn, r, w): S.add("dve", fn, r, w)
    def POOL(fn, r, w): S.add("pool", fn, r, w)
    def DMA(out, in_, r, w, q="sp", nonc=False):
        def fn(e):
            if nonc:
                with nc.allow_non_contiguous_dma(reason="tiny vector load"):
                    return e.dma_start(out=out, in_=in_)
            return e.dma_start(out=out, in_=in_)
        return S.add(q, fn, r, w, dma=True)

    def mm(out, pairs, r, w):
        n = len(pairs)

        def fn(e):
            ins = None
            for i, (l, rh) in enumerate(pairs):
                ins = e.matmul(out, l, rh, start=(i == 0), stop=(i == n - 1))
            return ins
        PE(fn, r, w)

    def ts(eng, out, in0, s1, s2, op0, op1, r, w):
        if s2 is None:
            eng(lambda e: e.tensor_scalar(out=out, in0=in0, scalar1=s1, scalar2=None, op0=op0), r, w)
        else:
            eng(lambda e: e.tensor_scalar(out=out, in0=in0, scalar1=s1, scalar2=s2, op0=op0, op1=op1), r, w)

    def tt(eng, out, in0, in1, op, r, w):
        eng(lambda e: e.tensor_tensor(out=out, in0=in0, in1=in1, op=op), r, w)

    def stt(eng, out, in0, sc, in1, op0, op1, r, w):
        eng(lambda e: e.scalar_tensor_tensor(out=out, in0=in0, scalar=sc, in1=in1, op0=op0, op1=op1), r, w)

    def act(out, in_, func, r, w, bias=None, scale=None, accum=None):
        kw = {}
        if bias is not None: kw["bias"] = bias
        if scale is not None: kw["scale"] = scale
        if accum is not None: kw["accum_out"] = accum
        ACT(lambda e: e.activation(out=out, in_=in_, func=func, **kw), r, w)

    def rstd_from(out_t, out_ap, ss_ap, ss_deps, n):
        act(out_ap, ss_ap, AF.Ln, ss_deps + [eps_c], [out_t], bias=eps_c[0:out_ap.shape[0], 0:1], scale=1.0 / n)
        act(out_ap, out_ap, AF.Exp, [out_t], [out_t], scale=-0.5)

    P = Alloc(0)
    ident = P([128, 128]); ones_f = P([128, 128]); ones_b = P([128, 128], BF16)
    modT = P([128, 48, 2]); adabT = P([128, 48]); nmT = P([128, 8]); nfT = P([128, 8])
    A1 = P([128, 8, 2]); A2 = P([128, 8])
    g1bc = P([128, D]); g2bc = P([128, D])
    gqT = P([128, 2]); gkT = P([128, 2])
    gqaT = P([128, 2]); gkvaT = P([128, 1])
    gmqT = P([128, 3]); gmkT = P([128, 3])
    ss1 = P([128, 4]); rs1 = P([128, 4])
    zero_c = P([128, 1]); eps_c = P([128, 1])
    HT_OFF = P.off
    hT = P([128, 8, NT], BF16)
    PERSIST_END = P.off

    DMA(ident.ap, ident_d, [], [ident])
    POOL(lambda e: e.memset(ones_f.ap, 1.0), [], [ones_f])
    POOL(lambda e: e.memset(ones_b.ap, 1.0), [], [ones_b])
    POOL(lambda e: e.memset(zero_c.ap, 0.0), [], [zero_c])
    POOL(lambda e: e.memset(eps_c.ap, EPS), [], [eps_c])
    DMA(nmT.ap, norm_mix.rearrange("(j p) -> p j", p=128), [], [nmT], nonc=True)
    DMA(nfT.ap, norm_ffn.rearrange("(j p) -> p j", p=128), [], [nfT], nonc=True)

    def colvec(dst_t, col, src_d, lo, hi, p0=0):
        DMA(dst_t[p0:p0 + (hi - lo), col:col + 1], src_d[lo:hi].rearrange("(p o) -> p o", o=1), [], [dst_t], nonc=True)

    for (t_, d_) in ((gqT, gq_d), (gkT, gk_d)):
        colvec(t_, 0, d_, 0, 128)
        colvec(t_, 1, d_, 32, 64, 0); colvec(t_, 1, d_, 0, 32, 32)
        colvec(t_, 1, d_, 96, 128, 64); colvec(t_, 1, d_, 64, 96, 96)
    colvec(gqaT, 0, gqa_d, 0, 128); colvec(gqaT, 1, gqa_d, 128, 256)
    colvec(gkvaT, 0, gkva_d, 0, 128)
    for (t_, d_) in ((gmqT, gmq_d), (gmkT, gmk_d)):
        colvec(t_, 0, d_, 0, 128)
        colvec(t_, 1, d_, 128, 192, 0)
        colvec(t_, 2, d_, 144, 160, 0); colvec(t_, 2, d_, 128, 144, 16)
        colvec(t_, 2, d_, 176, 192, 32); colvec(t_, 2, d_, 160, 176, 48)

    T0 = Alloc(PERSIST_END)
    ccT = T0([128, 8, 2]); scT = T0([128, 8, 2])
    wblk = [T0([128, 8, 1024]) for _ in range(2)]
    modrow = T0([2, 6 * D]); adab2 = T0([2, 6 * D]); sel0 = T0([2, 128])
    abtmp = [T0([128, D]) for _ in range(2)]; nfbc = T0([128, D])
    ab_scr = dscr("ab_scr", [2, 128, D], F32)
    for r in range(2):
        DMA(ccT[:, :, r], cc_d[r].rearrange("(j p) -> p j", p=128), [], [ccT], nonc=True)
        DMA(adab2[r:r + 1, :], ada_b.rearrange("(o n) -> o n", o=1), [], [adab2])
    DMA(sel0.ap, sel0_d, [], [sel0])
    act(scT.ap, ccT.ap, AF.Silu, [ccT], [scT])
    ada_v = ada_w.rearrange("(kc p) n -> p kc n", p=128)
    for blk in range(6):
        wb = wblk[blk % 2]
        DMA(wb.ap, ada_v[:, :, blk * 1024:(blk + 1) * 1024], [], [wb])
        for half in range(2):
            pr = psum[(blk * 2 + half) % 4]
            cols = slice(blk * 1024 + half * 512, blk * 1024 + (half + 1) * 512)
            mm(pr[0:2, :], [(scT[:, kc, :], wb[:, kc, half * 512:(half + 1) * 512]) for kc in range(8)], [scT, wb], [pr])
            tt(DVE, modrow[:, cols], pr[0:2, :], adab2[:, cols], ALU.add, [pr, adab2], [modrow])
    pst = psum[4]

    def ftm(e):
        ins = None
        for j in range(48):
            ins = e.transpose(out=pst[:, 2 * j:2 * j + 2], in_=modrow[0:2, j * 128:(j + 1) * 128], identity=ident[0:2, 0:2])
        return ins
    PE(ftm, [modrow, ident], [pst])
    DVE(lambda e: e.tensor_copy(out=modT.ap, in_=pst[:, 0:96].rearrange("p (j r) -> p j r", r=2)), [pst], [modT])
    for blk in ((2, 5, 3, 4) if SPARSE else (2, 5)):
        gdst = {2: g1bc, 5: g2bc, 3: abtmp[0], 4: abtmp[1]}[blk]
        for half in range(2):
            pg = psum[5 + half]
            cols = slice(blk * 1024 + half * 512, blk * 1024 + (half + 1) * 512)
            mm(pg[:, :], [(sel0.ap, modrow[0:2, cols])], [sel0, modrow], [pg])
            if half == 0:
                ACT(lambda e, gdst=gdst, pg=pg: e.copy(out=gdst[:, 0:512], in_=pg[:, :]), [pg], [gdst])
            else:
                DVE(lambda e, gdst=gdst, pg=pg: e.tensor_copy(out=gdst[:, 512:1024], in_=pg[:, :]), [pg], [gdst])
        if blk == 4:
            DMA(nfbc.ap, norm_ffn.partition_broadcast(128), [], [nfbc])
            stt(DVE, gdst.ap, gdst.ap, 1.0, nfbc.ap, ALU.add, ALU.mult, [gdst, nfbc], [gdst])
        if blk in (3, 4):
            DMA(ab_scr[blk - 3], gdst.ap, [gdst], [])
    for r in range(2):
        stt(DVE, A1[:, :, r], modT[:, 8:16, r], 1.0, nmT.ap, ALU.add, ALU.mult, [modT, nmT], [A1])
    stt(DVE, A2.ap, modT[:, 32:40, 0], 1.0, nfT.ap, ALU.add, ALU.mult, [modT, nfT], [A2])

    S.barrier()
    T1 = Alloc(PERSIST_END)
    xt = [T1([128, D]) for _ in range(4)]
    xn = [T1([128, D]) for _ in range(4)]
    NB = {"xt": xt, "xn": xn}

    def nm_A(i, src_ap, src_deps=()):
        xt_, xn_ = NB["xt"][i % len(NB["xt"])], NB["xn"][i % len(NB["xn"])]
        ss_, rs_ = ss1[:, i % 4:i % 4 + 1], rs1[:, i % 4:i % 4 + 1]
        DMA(xt_.ap, src_ap, list(src_deps), [xt_])
        act(xn_.ap, xt_.ap, AF.Square, [xt_], [xn_, ss1], accum=ss_)
        return (xt_, xn_, ss_, rs_)

    def nm_B(cx):
        xt_, xn_, ss_, rs_ = cx
        rstd_from(rs1, rs_, ss_, [ss1], D)
        ACT(lambda e: e.activation(out=xn_.ap, in_=xt_.ap, func=AF.Copy, scale=rs_), [xt_, rs1], [xn_])

    def nm_C(cx, A_ap, S_ap, dst_fn, ps_pair):
        xt_, xn_, ss_, rs_ = cx
        for half in range(2):
            pst = ps_pair[half]

            def fn(e, half=half, pst=pst):
                ins = None
                for jj in range(4):
                    j = half * 4 + jj
                    ins = e.transpose(out=pst[:, jj * 128:(jj + 1) * 128], in_=xn_[:, j * 128:(j + 1) * 128], identity=ident.ap)
                return ins
            PE(fn, [xn_, ident], [pst])
            for jj in range(4):
                j = half * 4 + jj
                dst_ap, dst_t = dst_fn(j)
                if jj % 2 == 0:
                    ts(DVE, dst_ap, pst[:, jj * 128:(jj + 1) * 128], A_ap[:, j:j + 1], S_ap[:, j:j + 1], ALU.mult, ALU.add,
                       [pst, A1, A2, modT], [dst_t])
                else:
                    act(dst_ap, pst[:, jj * 128:(jj + 1) * 128], AF.Identity, [pst, A1, A2, modT], [dst_t],
                        bias=S_ap[:, j:j + 1], scale=A_ap[:, j:j + 1])

    def norm_mod_T(i, src_ap, A_ap, S_ap, dst_fn, ps_pair, extra_f32=None, src_deps=()):
        cx = nm_A(i, src_ap, src_deps)
        nm_B(cx)
        nm_C(cx, A_ap, S_ap, dst_fn, ps_pair)
        return cx[0]

    hT_chunks = [Tile(hT.ap) for _ in range(9)]

    p1ctx = {}
    for step in range(34 + 2):
        if step < 34:
            i = step
            src = ctx_d[i * 128:(i + 1) * 128, :] if i < 2 else x_d[(i - 2) * 128:(i - 1) * 128, :]
            p1ctx[i] = nm_A(i, src)
        if 0 <= step - 1 < 34:
            nm_B(p1ctx[step - 1])
        if 0 <= step - 2 < 34:
            i = step - 2
            r = 1 if i < 2 else 0
            c0 = i * 128
            chunk = hT_chunks[c0 // 512]
            nm_C(p1ctx[i], A1[:, :, r], modT[:, 0:8, r], lambda j, c0=c0, chunk=chunk: (hT[:, j, c0:c0 + 128], chunk),
                 (psum[(i % 4) * 2], psum[(i % 4) * 2 + 1]))

    final_deps = []

    def dbg_dump(src_tile, ncols, col0=0):
        pass

    if STAGE == 1:
        TD = Alloc(T1.off)
        for j in range(8):
            for c in range(9):
                w = min(512, NT - c * 512)
                tmp = TD([128, 512])
                ts(DVE, tmp[:, 0:w], hT[:, j, c * 512:c * 512 + w], 1.0, None, ALU.mult, None, [hT_chunks[c]], [tmp])
                final_deps.append(DMA(dbg_d[:, j * NT + c * 512:j * NT + c * 512 + w], tmp[:, 0:w], [tmp], []))
                if TD.off > ARENA_F - 600:
                    TD = Alloc(T1.off)
        return finish(nc, S, es, block, final_deps)


    def all_dmas():
        return [op for e_ in ENGS for op in S.ops[e_] if op.dma]

    w_in_v = w_in.rearrange("(kc p) n -> p kc n", p=128)
    chunks = [(c, c * 512, min(512, NT - c * 512)) for c in range(9)]

    def qstore(dst_h, src_t, c, c0, w, parts=128):
        if c == 0:
            return DMA(dst_h[:, 0:256], src_t[0:parts, 256:512], [src_t], [])
        return DMA(dst_h[:, c0 - 256:c0 - 256 + w], src_t[0:parts, 0:w], [src_t], [])

    S.barrier()
    T2 = Alloc(PERSIST_END)
    wqa = T2([128, 8, 1536], BF16); wrot = T2([128, 8, 1280], BF16)
    cosT = [T2([128, 512]) for _ in range(2)]; sinT = [T2([128, 512]) for _ in range(2)]
    sqb = [T2([128, 512], BF16) for _ in range(2)]; rsd = [T2([128, 512]) for _ in range(2)]
    t1b = [T2([128, 512]) for _ in range(2)]; t2b = [T2([128, 512]) for _ in range(2)]
    qob = [T2([128, 512], BF16) for _ in range(3)]
    vta = [T2([128, 4, 256], BF16) for _ in range(2)]
    DMA(wqa.ap, w_in_v[:, :, 0:1536], [], [wqa], q="pool")
    for kc in range(8):
        sv = wqa[:, kc, 0:1280].rearrange("p (h d) -> p h d", d=128)
        dv = wrot[:, kc, :].rearrange("p (h d) -> p h d", d=128)
        for (dlo, slo, neg) in ((0, 32, True), (32, 0, False), (64, 96, True), (96, 64, False)):
            if neg:
                ts(DVE, dv[:, :, dlo:dlo + 32], sv[:, :, slo:slo + 32], -1.0, None, ALU.mult, None, [wqa], [wrot])
            else:
                ACT(lambda e, dv=dv, sv=sv, dlo=dlo, slo=slo: e.copy(out=dv[:, :, dlo:dlo + 32], in_=sv[:, :, slo:slo + 32]), [wqa], [wrot])
    it = 0
    for (c, c0, w) in chunks:
        hc_ = hT_chunks[c]
        cs_, sn_ = cosT[c % 2], sinT[c % 2]
        DMA(cs_[:, 0:w], cosA_d[:, c0:c0 + w], [], [cs_])
        DMA(sn_[:, 0:w], sinA_d[:, c0:c0 + w], [], [sn_])
        for hh in range(10):
            raw, rot, ssp = psum[hh % 2], psum[2 + hh % 2], psum[4 + hh % 2]
            cols = slice(hh * 128, (hh + 1) * 128)
            mm(raw[:, 0:w], [(wqa[:, kc, cols], hT[:, kc, c0:c0 + w]) for kc in range(8)], [wqa, hc_], [raw])
            mm(rot[:, 0:w], [(wrot[:, kc, cols], hT[:, kc, c0:c0 + w]) for kc in range(8)], [wrot, hc_], [rot])
            sq_, rs_, t1_, t2_, qo_ = sqb[it % 2], rsd[it % 2], t1b[it % 2], t2b[it % 2], qob[it % 3]
            it += 1
            act(sq_[:, 0:w], raw[:, 0:w], AF.Square, [raw], [sq_])
            mm(ssp[:, 0:w], [(ones_b.ap, sq_[:, 0:w])], [ones_b, sq_], [ssp])
            rstd_from(rs_, rs_[:, 0:w], ssp[:, 0:w], [ssp], 128)
            g_ = gqT if hh < 8 else gkT
            stt(DVE, t1_[:, 0:w], raw[:, 0:w], g_[:, 0:1], cs_[:, 0:w], ALU.mult, ALU.mult, [raw, g_, cs_, sq_], [t1_])
            stt(DVE, t2_[:, 0:w], rot[:, 0:w], g_[:, 1:2], sn_[:, 0:w], ALU.mult, ALU.mult, [rot, g_, sn_], [t2_])
            tt(POOL, t1_[:, 0:w], t1_[:, 0:w], t2_[:, 0:w], ALU.add, [t1_, t2_], [t1_])
            tt(DVE, qo_[:, 0:w], t1_[:, 0:w], rs_[:, 0:w], ALU.mult, [t1_, rs_], [qo_])
            if hh < 8:
                qstore(qa_scr[hh], qo_, c, c0, w)
            else:
                DMA(ka_scr[hh - 8, :, c0:c0 + w], qo_[:, 0:w], [qo_], [])
        vt_ = vta[c % 2]
        nsub = w // 128
        for sub in range(nsub):
            pv = psum[6 + sub % 2]
            mm(pv[:, 0:256], [(hT[:, kc, c0 + sub * 128:c0 + (sub + 1) * 128], wqa[:, kc, 1280:1536]) for kc in range(8)],
               [wqa, hc_], [pv])
            ACT(lambda e, vt_=vt_, pv=pv, sub=sub: e.copy(out=vt_[:, sub, :], in_=pv[:, 0:256]), [pv], [vt_])
        for g in range(2):
            DMA(va_scr[g, :, c * 4:c * 4 + nsub, :], vt_[:, 0:nsub, g * 128:(g + 1) * 128], [vt_], [])

    if STAGE == 2:
        return finish(nc, S, es, block, all_dmas())

    S.barrier()
    T3 = Alloc(PERSIST_END)
    wm = T3([128, 8, 448], BF16); wkrot = T3([128, 8, 64], BF16)
    wqb_t = T3([128, 2, 1536], BF16); wqb_rot = T3([128, 2, 8, 64], BF16)
    wkvb_t = T3([128, 2048], BF16); wv_t = T3([128, 1024], BF16)
    cosBt = [T3([128, 512]) for _ in range(2)]; sinBt = [T3([128, 512]) for _ in range(2)]
    sqa = T3([128, 2, 512], BF16); qan = T3([128, 2, 512], BF16); ckvn = T3([128, 512], BF16)
    sqk = T3([128, 512], BF16); krsq = T3([128, 512], BF16); krr = T3([128, 512]); krt2 = T3([128, 512])
    rs3 = [T3([128, 512]) for _ in range(4)]
    sqn = [T3([128, 512], BF16) for _ in range(2)]; sqr = [T3([128, 512], BF16) for _ in range(2)]
    t13 = [T3([128, 512]) for _ in range(2)]; t23 = [T3([128, 512]) for _ in range(2)]
    o3 = [T3([128, 512], BF16) for _ in range(8)]
    vtb = [T3([128, 4, 1024], BF16) for _ in range(2)]
    DMA(wm.ap, w_in_v[:, :, 1536:1984], [], [wm], q="pool")
    DMA(wqb_t.ap, w_qb.rearrange("(c p) n -> p c n", p=128), [], [wqb_t], q="pool")
    DMA(wkvb_t.ap, w_kvb, [], [wkvb_t], q="pool")
    POOL(lambda e: e.tensor_copy(out=wv_t.ap.rearrange("p (h d) -> p h d", d=128),
                                 in_=wkvb_t.ap.rearrange("p (h t d) -> p h t d", t=2, d=128)[:, :, 1, :]), [wkvb_t], [wv_t])
    for (dlo, slo, neg) in ((0, 16, True), (16, 0, False), (32, 48, True), (48, 32, False)):
        for kc in range(8):
            if neg:
                ts(DVE, wkrot[:, kc, dlo:dlo + 16], wm[:, kc, 384 + slo:384 + slo + 16], -1.0, None, ALU.mult, None, [wm], [wkrot])
            else:
                ACT(lambda e, kc=kc, dlo=dlo, slo=slo: e.copy(out=wkrot[:, kc, dlo:dlo + 16], in_=wm[:, kc, 384 + slo:384 + slo + 16]), [wm], [wkrot])
        for c2 in range(2):
            sv = wqb_t[:, c2, :].rearrange("p (h d) -> p h d", d=192)
            if neg:
                ts(DVE, wqb_rot[:, c2, :, dlo:dlo + 16], sv[:, :, 128 + slo:128 + slo + 16], -1.0, None, ALU.mult, None, [wqb_t], [wqb_rot])
            else:
                ACT(lambda e, c2=c2, sv=sv, dlo=dlo, slo=slo: e.copy(out=wqb_rot[:, c2, :, dlo:dlo + 16], in_=sv[:, :, 128 + slo:128 + slo + 16]), [wqb_t], [wqb_rot])
    it = 0
    for (c, c0, w) in chunks:
        hc_ = hT_chunks[c]
        W = slice(0, w)
        cs_, sn_ = cosBt[c % 2], sinBt[c % 2]
        DMA(cs_[0:64, W], cosB_d[:, c0:c0 + w], [], [cs_])
        DMA(sn_[0:64, W], sinB_d[:, c0:c0 + w], [], [sn_])
        for c2 in range(2):
            mm(psum[c2][:, W], [(wm[:, kc, c2 * 128:(c2 + 1) * 128], hT[:, kc, c0:c0 + w]) for kc in range(8)], [wm, hc_], [psum[c2]])
            act(sqa[:, c2, W], psum[c2][:, W], AF.Square, [psum[c2]], [sqa])
        mm(psum[3][:, W], [(ones_b.ap, sqa[:, 0, W]), (ones_b.ap, sqa[:, 1, W])], [ones_b, sqa], [psum[3]])
        r_ = rs3[0]
        rstd_from(r_, r_[:, W], psum[3][:, W], [psum[3]], 256)
        for c2 in range(2):
            stt(DVE, qan[:, c2, W], psum[c2][:, W], gqaT[:, c2:c2 + 1], r_[:, W], ALU.mult, ALU.mult, [psum[c2], gqaT, r_, sqa], [qan])
        mm(psum[4][:, W], [(wm[:, kc, 256:384], hT[:, kc, c0:c0 + w]) for kc in range(8)], [wm, hc_], [psum[4]])
        act(sqk[:, W], psum[4][:, W], AF.Square, [psum[4]], [sqk])
        mm(psum[5][:, W], [(ones_b.ap, sqk[:, W])], [ones_b, sqk], [psum[5]])
        r_ = rs3[1]
        rstd_from(r_, r_[:, W], psum[5][:, W], [psum[5]], 128)
        stt(DVE, ckvn[:, W], psum[4][:, W], gkvaT[:, 0:1], r_[:, W], ALU.mult, ALU.mult, [psum[4], gkvaT, r_, sqk], [ckvn])
        mm(psum[6][0:64, W], [(wm[:, kc, 384:448], hT[:, kc, c0:c0 + w]) for kc in range(8)], [wm, hc_], [psum[6]])
        mm(psum[7][0:64, W], [(wkrot[:, kc, :], hT[:, kc, c0:c0 + w]) for kc in range(8)], [wkrot, hc_], [psum[7]])
        act(krsq[0:64, W], psum[6][0:64, W], AF.Square, [psum[6]], [krsq])
        stt(DVE, krr[0:64, W], psum[6][0:64, W], gmkT[0:64, 1:2], cs_[0:64, W], ALU.mult, ALU.mult, [psum[6], gmkT, cs_, krsq], [krr])
        stt(DVE, krt2[0:64, W], psum[7][0:64, W], gmkT[0:64, 2:3], sn_[0:64, W], ALU.mult, ALU.mult, [psum[7], gmkT, sn_], [krt2])
        tt(POOL, krr[0:64, W], krr[0:64, W], krt2[0:64, W], ALU.add, [krr, krt2], [krr])
        vt_ = vtb[c % 2]
        nsub = w // 128
        for sub in range(nsub):
            for half in range(2):
                pv = psum[6 + half]
                mm(pv[:, :], [(ckvn[:, sub * 128:(sub + 1) * 128], wv_t[:, half * 512:(half + 1) * 512])], [ckvn, wv_t], [pv])
                ACT(lambda e, vt_=vt_, pv=pv, sub=sub, half=half: e.copy(out=vt_[:, sub, half * 512:(half + 1) * 512], in_=pv[:, :]), [pv], [vt_])
        for h in range(8):
            DMA(vb_scr[h, :, c * 4:c * 4 + nsub, :], vt_[:, 0:nsub, h * 128:(h + 1) * 128], [vt_], [])
        for h in range(8):
            par = h % 2
            pn, pr, prr, pss = psum[par], psum[2 + par], psum[4 + par], psum[6 + par]
            sqn_, sqr_, t1_, t2_ = sqn[par], sqr[par], t13[par], t23[par]
            rq_ = rs3[2 + par]
            mm(pn[:, W], [(wqb_t[:, c2, h * 192:h * 192 + 128], qan[:, c2, W]) for c2 in range(2)], [wqb_t, qan], [pn])
            mm(pr[0:64, W], [(wqb_t[:, c2, h * 192 + 128:h * 192 + 192], qan[:, c2, W]) for c2 in range(2)], [wqb_t, qan], [pr])
            mm(prr[0:64, W], [(wqb_rot[:, c2, h, :], qan[:, c2, W]) for c2 in range(2)], [wqb_rot, qan], [prr])
            act(sqn_[:, W], pn[:, W], AF.Square, [pn], [sqn_])
            act(sqr_[0:64, W], pr[0:64, W], AF.Square, [pr], [sqr_])
            mm(pss[:, W], [(ones_b.ap, sqn_[:, W]), (ones_b[0:64, :], sqr_[0:64, W])], [ones_b, sqn_, sqr_], [pss])
            rstd_from(rq_, rq_[:, W], pss[:, W], [pss], 192)
            oq, oqr = o3[par * 4], o3[par * 4 + 1]
            stt(DVE, oq[:, W], pn[:, W], gmqT[:, 0:1], rq_[:, W], ALU.mult, ALU.mult, [pn, gmqT, rq_, sqn_], [oq])
            qstore(qb_scr[h], oq, c, c0, w)
            stt(DVE, t1_[0:64, W], pr[0:64, W], gmqT[0:64, 1:2], cs_[0:64, W], ALU.mult, ALU.mult, [pr, gmqT, cs_, sqr_], [t1_])
            stt(DVE, t2_[0:64, W], prr[0:64, W], gmqT[0:64, 2:3], sn_[0:64, W], ALU.mult, ALU.mult, [prr, gmqT, sn_], [t2_])
            tt(POOL, t1_[0:64, W], t1_[0:64, W], t2_[0:64, W], ALU.add, [t1_, t2_], [t1_])
            tt(DVE, oqr[0:64, W], t1_[0:64, W], rq_[0:64, W], ALU.mult, [t1_, rq_], [oqr])
            qstore(qbr_scr[h], oqr, c, c0, w, parts=64)
        for h in range(8):
            par = h % 2
            pk, psk = psum[h % 4], psum[4 + h % 4]
            sqn_, rk_ = sqn[par], rs3[2 + par]
            ok, okr = o3[par * 4 + 2], o3[par * 4 + 3]
            mm(pk[:, W], [(wkvb_t[:, h * 256:h * 256 + 128], ckvn[:, W])], [wkvb_t, ckvn], [pk])
            act(sqn_[:, W], pk[:, W], AF.Square, [pk], [sqn_])
            mm(psk[:, W], [(ones_b.ap, sqn_[:, W]), (ones_b[0:64, :], krsq[0:64, W])], [ones_b, sqn_, krsq], [psk])
            rstd_from(rk_, rk_[:, W], psk[:, W], [psk], 192)
            stt(DVE, ok[:, W], pk[:, W], gmkT[:, 0:1], rk_[:, W], ALU.mult, ALU.mult, [pk, gmkT, rk_, sqn_], [ok])
            DMA(kb_scr[h, :, c0:c0 + w], ok[:, W], [ok], [])
            tt(POOL, okr[0:64, W], krr[0:64, W], rk_[0:64, W], ALU.mult, [krr, rk_], [okr])
            DMA(kbr_scr[h, :, c0:c0 + w], okr[0:64, W], [okr], [])

    if STAGE == 3:
        return finish(nc, S, es, block, all_dmas())

    scr_tok = Tile(None)
    bar = S.add("sp", None, [], [scr_tok])
    for e_ in ENGS:
        for op in S.ops[e_]:
            if op.dma and op is not bar:
                bar.deps.append(op); op.needs_inc = True

    S.barrier()
    T4 = Alloc(PERSIST_END)
    qTb = [T4([128, SEQ], BF16) for _ in range(2)]; qrb = [T4([128, SEQ], BF16) for _ in range(2)]
    kTb = [T4([128, NT], BF16) for _ in range(2)]; krb = [T4([128, NT], BF16) for _ in range(2)]
    Vb = [T4([128, 34, 128], BF16) for _ in range(2)]
    pT = [T4([128, 512], BF16) for _ in range(4)]
    rinv = [T4([128, 512]) for _ in range(2)]
    obuf = [T4([128, 512], BF16) for _ in range(2)]
    o_stores = []
    LAG = 2
    zfill_ops = []
    if SPARSE:
        xs_d = dscr("xs_scr", [NBLK * BLK, D], BF16)
        zt4 = T4([128, 4096], BF16)
        POOL(lambda e: e.memset(zt4.ap, 0.0), [], [zt4])
        for zi in range(NBLK * BLK // 512):
            zfill_ops.append(DMA(xs_d[zi * 512:(zi + 1) * 512, :].rearrange("(p r) d -> p (r d)", p=128), zt4.ap, [zt4], [], q="pool"))
    for t_ in qrb + krb:
        POOL(lambda e, t_=t_: e.memset(t_[64:128, :], 0.0), [], [t_])
    for hd in range(16):
        b = hd % 2
        mla = hd >= 8
        h = hd - 8 if mla else hd
        qT_, kT_, V_, qr_, kr_ = qTb[b], kTb[b], Vb[b], qrb[b], krb[b]
        if not mla:
            g = h // 4
            DMA(qT_.ap, qa_scr[h], [scr_tok], [qT_])
            if h % 4 == 0:
                kg, vg = Tile(None), Tile(None)
                kT_g, V_g = kTb[g % 2], Vb[g % 2]
                DMA(kT_g.ap, ka_scr[g], [scr_tok], [kT_g])
                DMA(V_g.ap, va_scr[g], [scr_tok], [V_g])
            kT_, V_ = kT_g, V_g
            scale = 128 ** -0.5
        else:
            DMA(qT_.ap, qb_scr[h], [scr_tok], [qT_])
            DMA(qr_[0:64, :], qbr_scr[h], [scr_tok], [qr_])
            DMA(kT_.ap, kb_scr[h], [scr_tok], [kT_])
            DMA(kr_[0:64, :], kbr_scr[h], [scr_tok], [kr_])
            DMA(V_.ap, vb_scr[h], [scr_tok], [V_])
            scale = 192 ** -0.5
        for qi in range(8):
            o_ps = psum[4 + qi % 2]
            sum_ps = psum[6 + qi % 2]
            Q = slice(qi * 512, (qi + 1) * 512)
            for step in range(34 + LAG):
                if step < 34:
                    ti = step
                    s_ps = psum[ti % 4]
                    Kc = slice(ti * 128, (ti + 1) * 128)
                    pairs = [(kT_[:, Kc], qT_[:, Q])]
                    rds = [kT_, qT_]
                    if mla:
                        pairs.append((kr_[:, Kc], qr_[:, Q]))
                        rds += [kr_, qr_]
                    mm(s_ps[:, :], pairs, rds, [s_ps])
                    p_ = pT[ti % 4]
                    act(p_.ap, s_ps[:, :], AF.Exp, [s_ps], [p_], scale=scale)
                if step >= LAG:
                    ti = step - LAG
                    p_ = pT[ti % 4]
                    PE(lambda e, o_ps=o_ps, V_=V_, ti=ti, p_=p_: e.matmul(o_ps[:, :], V_[:, ti, :], p_.ap, start=(ti == 0), stop=(ti == 33)),
                       [V_, p_], [o_ps])
                    PE(lambda e, sum_ps=sum_ps, ti=ti, p_=p_: e.matmul(sum_ps[:, :], ones_b.ap, p_.ap, start=(ti == 0), stop=(ti == 33)),
                       [ones_b, p_], [sum_ps])
            ri, ob = rinv[qi % 2], obuf[qi % 2]
            DVE(lambda e, ri=ri, sum_ps=sum_ps: e.reciprocal(out=ri.ap, in_=sum_ps[:, :]), [sum_ps], [ri])
            tt(DVE, ob.ap, o_ps[:, :], ri.ap, ALU.mult, [o_ps, ri], [ob])
            o_stores.append(DMA(o_scr[hd, :, Q], ob.ap, [ob], []))

    o_tok = Tile(None)
    bar2 = S.add("sp", None, [], [o_tok])
    for op in o_stores:
        bar2.deps.append(op); op.needs_inc = True

    if STAGE == 4:
        return finish(nc, S, es, block, o_stores)

    S.barrier()
    T5 = Alloc(PERSIST_END)
    wg = T5([128, 8, 2048], BF16); woa_t = T5([128, 8, D], BF16); wob_t = T5([128, 8, D], BF16); wout_t = T5([128, 8, D], BF16)
    DMA(wg.ap, w_in_v[:, :, 1984:4032], [], [wg], q="pool")
    DMA(woa_t.ap, w_oa.rearrange("(kc p) n -> p kc n", p=128), [], [woa_t], q="pool")
    DMA(wob_t.ap, w_ob.rearrange("(kc p) n -> p kc n", p=128), [], [wob_t], q="pool")
    DMA(wout_t.ap, w_out.rearrange("(kc p) n -> p kc n", p=128), [], [wout_t], q="pool")
    oTt = [T5([128, 16, 512], BF16) for _ in range(1)]
    yT = [T5([128, 8, 512], BF16) for _ in range(1)]
    sgb = [T5([128, 512]) for _ in range(4)]
    xt5 = [T5([128, D]) for _ in range(1)]
    x1t = [T5([128, D]) for _ in range(1)]
    x1_stores = []
    for qi in range(8):
        Q = slice(qi * 512, (qi + 1) * 512)
        HQ = slice(256 + qi * 512, 256 + (qi + 1) * 512)
        hc_a, hc_b = hT_chunks[(256 + qi * 512) // 512], hT_chunks[(256 + qi * 512 + 511) // 512]
        oT_ = oTt[0]
        for hq in range(4):
            DMA(oT_[:, hq * 4:(hq + 1) * 4, :], o_scr[hq * 4:(hq + 1) * 4, :, Q].rearrange("h d t -> d h t"), [o_tok], [oT_])
        yT_ = yT[0]
        for m in range(8):
            pga, pgb, pA, pB = psum[m % 2], psum[2 + m % 2], psum[4], psum[5]
            M = slice(m * 128, (m + 1) * 128)
            mm(pga[:, :], [(wg[:, kc, m * 128:(m + 1) * 128], hT[:, kc, HQ]) for kc in range(8)], [wg, hc_a, hc_b], [pga])
            mm(pgb[:, :], [(wg[:, kc, D + m * 128:D + (m + 1) * 128], hT[:, kc, HQ]) for kc in range(8)], [wg, hc_a, hc_b], [pgb])
            mm(pA[:, :], [(woa_t[:, hh, M], oT_[:, hh, :]) for hh in range(8)], [woa_t, oT_], [pA])
            mm(pB[:, :], [(wob_t[:, hh, M], oT_[:, 8 + hh, :]) for hh in range(8)], [wob_t, oT_], [pB])
            sa, sb = sgb[(m % 2) * 2], sgb[(m % 2) * 2 + 1]
            act(sa.ap, pga[:, :], AF.Sigmoid, [pga], [sa])
            act(sb.ap, pgb[:, :], AF.Sigmoid, [pgb], [sb])
            tt(DVE, sa.ap, sa.ap, pA[:, :], ALU.mult, [sa, pA], [sa])
            tt(DVE, sb.ap, sb.ap, pB[:, :], ALU.mult, [sb, pB], [sb])
            tt(POOL, yT_[:, m, :], sa.ap, sb.ap, ALU.add, [sa, sb], [yT_])
        for sub in range(4):
            tok0 = qi * 512 + sub * 128
            xt_, x1_ = xt5[0], x1t[0]
            DMA(xt_.ap, x_d[tok0:tok0 + 128, :], [], [xt_])
            for n in range(2):
                po = psum[6 + n]
                mm(po[:, :], [(yT_[:, m, sub * 128:(sub + 1) * 128], wout_t[:, m, n * 512:(n + 1) * 512]) for m in range(8)], [yT_, wout_t], [po])
                tt(DVE, x1_[:, n * 512:(n + 1) * 512], po[:, :], g1bc[:, n * 512:(n + 1) * 512], ALU.mult, [po, g1bc], [x1_])
            tt(POOL, x1_.ap, x1_.ap, xt_.ap, ALU.add, [x1_, xt_], [x1_])
            x1_stores.append(DMA(x1_scr[tok0:tok0 + 128, :], x1_.ap, [x1_], []))
    x1_tok = Tile(None)
    bar3 = S.add("sp", None, [], [x1_tok])
    for op in x1_stores:
        bar3.deps.append(op); op.needs_inc = True
    if STAGE == 5:
        return finish(nc, S, es, block, x1_stores)


    S.barrier()
    if SPARSE:
        return sparse_moe(locals())

    T6 = Alloc(HT_OFF)
    rw_t = T6([128, 8, NEXP], BF16); rb_row = T6([1, NEXP], BF16); b1T = T6([128, 16, NEXP])
    b2rows = [T6([1, D], BF16) for _ in range(2)]
    gates = T6([128, 8, NEXP]); lgt = [T6([128, NEXP]) for _ in range(2)]; ext = [T6([128, NEXP]) for _ in range(2)]
    mk = [T6([128, NEXP]) for _ in range(2)]
    top8 = [T6([128, 8]) for _ in range(2)]; sm6 = [T6([128, 2]) for _ in range(2)]
    T6_MID = T6.off
    b1raw = T6([32, 2 * D])
    if not os.environ.get("MK_SKIP_SETUP"):
        DMA(rw_t.ap, rw_d.rearrange("(kc p) e -> p kc e", p=128), [], [rw_t], q="pool")
        DMA(rb_row.ap, rb_d.rearrange("(o e) -> o e", o=1), [], [rb_row], q="pool")
        DMA(b1raw.ap, eb1, [], [b1raw])
    for half in range(0 if os.environ.get("MK_SKIP_B1") else 2):
        pt = psum[half]

        def fnb(e, half=half, pt=pt):
            ins = None
            for f in range(8):
                fc = half * 8 + f
                ins = e.transpose(out=pt[:, f * 32:(f + 1) * 32], in_=b1raw[0:32, fc * 128:(fc + 1) * 128], identity=ident[0:32, 0:32])
            return ins
        PE(fnb, [b1raw, ident], [pt])
        DVE(lambda e, half=half, pt=pt: e.tensor_copy(out=b1T[:, half * 8:(half + 1) * 8, :], in_=pt[:, 0:256].rearrange("p (f e) -> p f e", e=NEXP)), [pt], [b1T])
    ts(DVE, b1T[:, 8:16, :], b1T[:, 8:16, :], 1.0, None, ALU.add, None, [b1T], [b1T])
    S.barrier()
    T6 = Alloc(T6_MID)
    h2T = T6([128, 8, 1024], BF16); h2f = T6([128, 8, 128]); acc = T6([128, 8, D])
    w1b = [T6([128, 8, 2 * D], BF16) for _ in range(2)]; w2t = T6([128, 8, D], BF16)
    actT = [T6([128, 8, 512], BF16) for _ in range(2)]
    ta = [T6([128, 512]) for _ in range(2)]; tsg = [T6([128, 512]) for _ in range(2)]; tl = [T6([128, 512]) for _ in range(2)]
    NB["xt"] = [T6([128, D]) for _ in range(2)]; NB["xn"] = [T6([128, D]) for _ in range(2)]
    h2chunks = [Tile(None) for _ in range(2)]
    final_stores = []
    nm_i = 0
    for g in range(int(os.environ.get("MK_NG", "4"))):
        for sub in range(8):
            tok0 = g * 1024 + sub * 128
            hch = h2chunks[sub // 4]
            if os.environ.get("MK_SKIP_NM"):
                continue
            norm_mod_T(nm_i, x1_scr[tok0:tok0 + 128, :], A2.ap, modT[:, 24:32, 0],
                       lambda j, sub=sub, hch=hch: (h2T[:, j, sub * 128:(sub + 1) * 128], hch),
                       (psum[(nm_i % 2) * 2], psum[(nm_i % 2) * 2 + 1]), src_deps=[x1_tok])
            nm_i += 1
            if os.environ.get("MK_SKIP_RT"):
                continue
            lg_ps = psum[4 + sub % 2]
            mm(lg_ps[:, 0:NEXP], [(h2T[:, kc, sub * 128:(sub + 1) * 128], rw_t[:, kc, :]) for kc in range(8)] + [(ones_b[0:1, :], rb_row[0:1, :])],
               [hch, rw_t, ones_b, rb_row], [lg_ps])
            lg, ex, mk_, t8, sm = lgt[sub % 2], ext[sub % 2], mk[sub % 2], top8[sub % 2], sm6[sub % 2]
            DVE(lambda e, lg=lg, lg_ps=lg_ps: e.tensor_copy(out=lg.ap, in_=lg_ps[:, 0:NEXP]), [lg_ps], [lg])
            DVE(lambda e, t8=t8, lg=lg: e.max(out=t8.ap, in_=lg.ap), [lg], [t8])
            ts(DVE, mk_.ap, lg.ap, t8[:, 3:4], None, ALU.is_ge, None, [lg, t8], [mk_])
            ts(DVE, sm[:, 0:1], t8[:, 0:1], -1.0, None, ALU.mult, None, [t8], [sm])
            act(ex.ap, lg.ap, AF.Exp, [lg, sm], [ex], bias=sm[:, 0:1])
            tt(DVE, ex.ap, ex.ap, mk_.ap, ALU.mult, [ex, mk_], [ex])
            DVE(lambda e, sm=sm, ex=ex: e.reduce_sum(out=sm[:, 1:2], in_=ex.ap, axis=AX.X), [ex], [sm])
            DVE(lambda e, sm=sm: e.reciprocal(out=sm[:, 1:2], in_=sm[:, 1:2]), [sm], [sm])
            ts(DVE, gates[:, sub, :], ex.ap, sm[:, 1:2], None, ALU.mult, None, [ex, sm], [gates])
        for ex_i in range(int(os.environ.get("MK_NE", "32"))):
            w1_ = w1b[ex_i % 2]
            b2r = b2rows[ex_i % 2]
            if not (os.environ.get("MK_NOW") and (g > 0 or ex_i > 1)):
                DMA(w1_.ap, ew1[ex_i].rearrange("(kc p) n -> p kc n", p=128), [], [w1_], q="pool")
                DMA(w2t.ap, ew2[ex_i].rearrange("(kc p) n -> p kc n", p=128), [], [w2t], q="pool")
            DMA(b2r.ap, eb2[ex_i:ex_i + 1, :], [], [b2r], q="pool")
            for rt in range(2):
                hch = h2chunks[rt]
                C = slice(rt * 512, (rt + 1) * 512)
                aT = actT[rt % 2]
                for fc in range(8):
                    pg, pl = psum[fc % 2], psum[2 + fc % 2]
                    a_, s_, l_ = ta[fc % 2], tsg[fc % 2], tl[fc % 2]
                    mm(pg[:, :], [(w1_[:, kc, fc * 128:(fc + 1) * 128], h2T[:, kc, C]) for kc in range(8)], [w1_, hch], [pg])
                    mm(pl[:, :], [(w1_[:, kc, D + fc * 128:D + (fc + 1) * 128], h2T[:, kc, C]) for kc in range(8)], [w1_, hch], [pl])
                    ts(DVE, a_.ap, pg[:, :], b1T[:, fc, ex_i:ex_i + 1], 7.0, ALU.add, ALU.min, [pg, b1T], [a_])
                    act(s_.ap, a_.ap, AF.Sigmoid, [a_], [s_], scale=1.702)
                    act(l_.ap, pl[:, :], AF.Identity, [pl, b1T], [l_], bias=b1T[:, 8 + fc, ex_i:ex_i + 1])
                    ts(DVE, l_.ap, l_.ap, -6.0, 8.0, ALU.max, ALU.min, [l_], [l_])
                    tt(DVE, s_.ap, a_.ap, s_.ap, ALU.mult, [a_, s_], [s_])
                    tt(POOL, aT[:, fc, :], s_.ap, l_.ap, ALU.mult, [s_, l_], [aT])
                for s4 in range(4):
                    sub = rt * 4 + s4
                    for n in range(2):
                        py = psum[4 + (s4 % 2) * 2 + n]
                        N = slice(n * 512, (n + 1) * 512)
                        mm(py[:, :], [(aT[:, fc, s4 * 128:(s4 + 1) * 128], w2t[:, fc, N]) for fc in range(8)] + [(ones_b[0:1, :], b2r[0:1, N])],
                           [aT, w2t, ones_b, b2r], [py])
                        if ex_i == 0:
                            ts(DVE, acc[:, sub, N], py[:, :], gates[:, sub, ex_i:ex_i + 1], None, ALU.mult, None, [py, gates], [acc])
                        else:
                            stt(DVE, acc[:, sub, N], py[:, :], gates[:, sub, ex_i:ex_i + 1], acc[:, sub, N], ALU.mult, ALU.add, [py, gates, acc], [acc])
        for sub in range(8):
            tok0 = g * 1024 + sub * 128
            xt_ = NB["xt"][sub % 2]
            DMA(xt_.ap, x1_scr[tok0:tok0 + 128, :], [x1_tok], [xt_])
            tt(DVE, acc[:, sub, :], acc[:, sub, :], g2bc.ap, ALU.mult, [acc, g2bc], [acc])
            tt(POOL, xt_.ap, xt_.ap, acc[:, sub, :], ALU.add, [xt_, acc], [xt_])
            final_stores.append(DMA(out_d[tok0:tok0 + 128, :], xt_.ap, [xt_], []))
    return finish(nc, S, es, block, final_stores)


def sparse_moe(L):
    g = dict(L)
    nc, S, es, block, Alloc, psum = g["nc"], g["S"], g["es"], g["block"], g["Alloc"], g["psum"]
    DMA, PE, ACT, DVE, POOL, mm, ts, tt, stt, act = (g[k] for k in ("DMA", "PE", "ACT", "DVE", "POOL", "mm", "ts", "tt", "stt", "act"))
    din, dscr, norm_mod_T, NB, finish = g["din"], g["dscr"], g["norm_mod_T"], g["NB"], finish_
    ident, ones_f, ones_b, modT, A2, g2bc, rs1 = g["ident"], g["ones_f"], g["ones_b"], g["modT"], g["A2"], g["g2bc"], g["rs1"]
    x1_scr, x1_tok, out_d, ab_scr = g["x1_scr"], g["x1_tok"], g["out_d"], g["ab_scr"]
    rw_d, rb_d, eb1, eb2, ew1, ew2 = g["rw_d"], g["rb_d"], g["eb1"], g["eb2"], g["ew1"], g["ew2"]
    IOA = bass.IndirectOffsetOnAxis

    NROWS = NBLK * BLK
    xs_d = g["xs_d"]
    ys_d = dscr("ys_scr", [NROWS, D], BF16)
    consts_d = g["consts_d"]
    ew1f = ew1.rearrange("e k n -> (e k) n"); ew2f = ew2.rearrange("e k n -> (e k) n")

    def IDMA(gather, dram, idx_ap, sb_ap, r, w):
        def fn(e):
            if gather:
                return e.indirect_dma_start(out=sb_ap, out_offset=None, in_=dram, in_offset=IOA(ap=idx_ap, axis=0))
            return e.indirect_dma_start(out=dram, out_offset=IOA(ap=idx_ap, axis=0), in_=sb_ap, in_offset=None)
        return S.add("pool", fn, r, w, dma=True)

    T6 = Alloc(g["HT_OFF"])
    cst = T6([128, NCONST])
    iota_r, pk = cst[:, 0:32], cst[:, 64:72]
    ustr_f = cst[:, 76:204]
    ustr_b = T6([128, 128], BF16); ident_b = T6([128, 128], BF16)
    rw_t = T6([128, 8, NEXP], BF16); rb_row = T6([1, NEXP], BF16)
    b1raw = T6([32, 2 * D]); b2_t = T6([32, D])
    A2bc = T6([128, D]); S2bc = T6([128, D])
    tot = T6([128, NEXP]); gates_all = T6([128, 32, NEXP]); gk_all = T6([128, 32, 4]); slots_u = T6([128, 32, 4], U32)
    ebv = T6([128, NBLK]); ohcol = T6([32, NBLK]); w_u = T6([128, 8, NBLK], U32)
    T6_MID = T6.off
    DMA(cst.ap, consts_d, [], [cst])
    DMA(rw_t.ap, rw_d.rearrange("(kc p) e -> p kc e", p=128), [], [rw_t], q="pool")
    DMA(rb_row.ap, rb_d.rearrange("(o e) -> o e", o=1), [], [rb_row], q="pool")
    DMA(b1raw.ap, eb1, [], [b1raw]); DMA(b2_t.ap, eb2, [], [b2_t])
    DMA(A2bc.ap, ab_scr[1], [], [A2bc]); DMA(S2bc.ap, ab_scr[0], [], [S2bc])
    ts(DVE, b1raw[:, D:2 * D], b1raw[:, D:2 * D], 1.0, None, ALU.add, None, [b1raw], [b1raw])
    DVE(lambda e: e.tensor_copy(out=ustr_b.ap, in_=ustr_f), [cst], [ustr_b])
    DVE(lambda e: e.tensor_copy(out=ident_b.ap, in_=ident.ap), [ident], [ident_b])
    POOL(lambda e: e.memset(tot.ap, 0.0), [], [tot])

    TA = Alloc(T6_MID)
    h2Tt = [TA([128, 8, 128], BF16) for _ in range(4)]
    h2tok = [TA([128, D], BF16) for _ in range(32)]
    tmpf = [TA([128, D]) for _ in range(3)]
    NB["xt"] = [TA([128, D]) for _ in range(4)]; NB["xn"] = [TA([128, D]) for _ in range(3)]
    lg_all = TA([128, 32, NEXP]); t8_all = TA([128, 32, 8]); posw = TA([128, 32, NEXP])
    ext = [TA([128, NEXP]) for _ in range(4)]; mkt = [TA([128, NEXP]) for _ in range(4)]
    mkb = [TA([128, NEXP], BF16) for _ in range(4)]; slotm = [TA([128, NEXP]) for _ in range(2)]
    sm6 = [TA([128, 2]) for _ in range(4)]
    oht = [TA([128, NEXP]) for _ in range(2)]; t32 = [TA([128, NEXP]) for _ in range(4)]
    slf = [TA([128, 4]) for _ in range(2)]
    zfill = g["zfill_ops"]
    for i in range(32):
        tok0 = i * 128
        hT_ = h2Tt[i % 4]
        xt_ = norm_mod_T(i, x1_scr[tok0:tok0 + 128, :], A2.ap, modT[:, 24:32, 0],
                         lambda j, hT_=hT_: (hT_[:, j, :], hT_), (psum[(i % 2) * 2], psum[(i % 2) * 2 + 1]), src_deps=[x1_tok])
        rs_ = rs1[:, i % 4:i % 4 + 1]
        tf, hk = tmpf[i % 3], h2tok[i]
        stt(DVE, tf.ap, xt_.ap, rs_, A2bc.ap, ALU.mult, ALU.mult, [xt_, rs1, A2bc], [tf])
        tt(POOL, hk.ap, tf.ap, S2bc.ap, ALU.add, [tf, S2bc], [hk])
        lg_ps = psum[4 + i % 2]
        mm(lg_ps[:, 0:NEXP], [(hT_[:, kc, :], rw_t[:, kc, :]) for kc in range(8)] + [(ones_b[0:1, :], rb_row[0:1, :])],
           [hT_, rw_t, ones_b, rb_row], [lg_ps])
        ex, mk_, sm, mb = ext[i % 4], mkt[i % 4], sm6[i % 4], mkb[i % 4]
        lg_i, t8_i = lg_all[:, i, :], t8_all[:, i, :]
        DVE(lambda e, lg_i=lg_i, lg_ps=lg_ps: e.tensor_copy(out=lg_i, in_=lg_ps[:, 0:NEXP]), [lg_ps], [lg_all])
        DVE(lambda e, t8_i=t8_i, lg_i=lg_i: e.max(out=t8_i, in_=lg_i), [lg_all], [t8_all])
        ts(DVE, mk_.ap, lg_i, t8_all[:, i, 3:4], None, ALU.is_ge, None, [lg_all, t8_all], [mk_])
        ts(DVE, sm[:, 0:1], t8_all[:, i, 0:1], -1.0, None, ALU.mult, None, [t8_all], [sm])
        act(ex.ap, lg_i, AF.Exp, [lg_all, sm], [ex], bias=sm[:, 0:1])
        tt(DVE, ex.ap, ex.ap, mk_.ap, ALU.mult, [ex, mk_], [ex])
        DVE(lambda e, sm=sm, ex=ex: e.reduce_sum(out=sm[:, 1:2], in_=ex.ap, axis=AX.X), [ex], [sm])
        DVE(lambda e, sm=sm: e.reciprocal(out=sm[:, 1:2], in_=sm[:, 1:2]), [sm], [sm])
        ts(DVE, gates_all[:, i, :], ex.ap, sm[:, 1:2], None, ALU.mult, None, [ex, sm], [gates_all])
        DVE(lambda e, mb=mb, mk_=mk_: e.tensor_copy(out=mb.ap, in_=mk_.ap), [mk_], [mb])
        pos_ps, cs_ps = psum[6], psum[7]
        mm(pos_ps[:, 0:NEXP], [(ustr_b.ap, mb.ap)], [ustr_b, mb], [pos_ps])
        mm(cs_ps[:, 0:NEXP], [(ones_b.ap, mb.ap)], [ones_b, mb], [cs_ps])
        tt(DVE, posw[:, i, :], pos_ps[:, 0:NEXP], tot.ap, ALU.add, [pos_ps, tot], [posw])
        tt(DVE, tot.ap, tot.ap, cs_ps[:, 0:NEXP], ALU.add, [tot, cs_ps], [tot])

    tq, tm, pcv, pstart = (TA([128, NEXP]) for _ in range(4))
    pp = [TA([128, NEXP]) for _ in range(2)]
    cmpt = [TA([128, NEXP]) for _ in range(2)]
    wf = TA([128, 8, NBLK])
    ts(DVE, tq.ap, tot.ap, 0.0, None, ALU.is_gt, None, [tot], [tq])
    for m_ in range(1, ECAP // BLK):
        ts(DVE, tm.ap, tot.ap, float(m_ * BLK), None, ALU.is_gt, None, [tot], [tm])
        tt(DVE, tq.ap, tq.ap, tm.ap, ALU.add, [tq, tm], [tq])
    ts(DVE, pcv.ap, tq.ap, float(BLK), None, ALU.mult, None, [tq], [pcv])
    DVE(lambda e: e.tensor_copy(out=pp[0].ap, in_=pcv.ap), [pcv], [pp[0]])
    cur = 0
    for sh in (1, 2, 4, 8, 16):
        a_, b_ = pp[cur], pp[1 - cur]
        DVE(lambda e, a_=a_, b_=b_: e.tensor_copy(out=b_.ap, in_=a_.ap), [a_], [b_])
        tt(DVE, b_[:, sh:NEXP], a_[:, sh:NEXP], a_[:, 0:NEXP - sh], ALU.add, [a_], [b_])
        cur = 1 - cur
    pend = pp[cur]
    tt(DVE, pstart.ap, pend.ap, pcv.ap, ALU.subtract, [pend, pcv], [pstart])
    for b in range(NBLK):
        c_ = cmpt[b % 2]
        ts(DVE, c_.ap, pend.ap, float(b * BLK), None, ALU.is_le, None, [pend], [c_])
        DVE(lambda e, c_=c_, b=b: e.reduce_sum(out=ebv[:, b:b + 1], in_=c_.ap, axis=AX.X), [c_], [ebv])
    ts(DVE, ebv.ap, ebv.ap, float(NEXP - 1), None, ALU.min, None, [ebv], [ebv])
    for kc in range(8):
        ts(DVE, wf[:, kc, :], ebv.ap, float(D), pk[:, kc:kc + 1], ALU.mult, ALU.add, [ebv, cst], [wf])
        DVE(lambda e, kc=kc: e.tensor_copy(out=w_u[:, kc, :], in_=wf[:, kc, :]), [wf], [w_u])
    ts(DVE, ohcol.ap, ebv[0:32, :], cst[0:32, 72:73], None, ALU.is_equal, None, [ebv, cst], [ohcol])

    zb = S.add("pool", None, [], [])
    for op in zfill:
        zb.deps.append(op)
    scat = []
    for i in range(32):
        sl, sf, hk = slotm[i % 2], slf[i % 2], h2tok[i]
        tt(DVE, sl.ap, posw[:, i, :], pstart.ap, ALU.add, [posw, pstart], [sl])
        for k in range(4):
            oh, ta_, tb_ = oht[k % 2], t32[(k % 2) * 2], t32[(k % 2) * 2 + 1]
            ts(DVE, oh.ap, lg_all[:, i, :], t8_all[:, i, k:k + 1], None, ALU.is_equal, None, [lg_all, t8_all], [oh])
            tt(DVE, ta_.ap, oh.ap, sl.ap, ALU.mult, [oh, sl], [ta_])
            DVE(lambda e, sf=sf, ta_=ta_, k=k: e.reduce_sum(out=sf[:, k:k + 1], in_=ta_.ap, axis=AX.X), [ta_], [sf])
            tt(DVE, tb_.ap, oh.ap, gates_all[:, i, :], ALU.mult, [oh, gates_all], [tb_])
            DVE(lambda e, tb_=tb_, k=k, i=i: e.reduce_sum(out=gk_all[:, i, k:k + 1], in_=tb_.ap, axis=AX.X), [tb_], [gk_all])
        DVE(lambda e, sf=sf, i=i: e.tensor_copy(out=slots_u[:, i, :], in_=sf.ap), [sf], [slots_u])
        for k in range(4):
            scat.append(IDMA(False, xs_d, slots_u[:, i, k:k + 1], hk.ap, [hk, slots_u], []))
    xs_tok = Tile(None)
    bs = S.add("sp", None, [], [xs_tok])
    for op in scat:
        bs.deps.append(op)
    S.barrier()

    TB = Alloc(T6_MID)
    w1b = [TB([128, 8, 2 * D], BF16) for _ in range(2)]; w2b = [TB([128, 8, D], BF16) for _ in range(2)]
    xrows = [TB([128, 4, D], BF16) for _ in range(2)]; xTb = [TB([128, 8, BLK], BF16) for _ in range(2)]
    actT = [TB([128, 8, BLK], BF16) for _ in range(2)]
    ta = [TB([128, 512]) for _ in range(2)]; tsg = [TB([128, 512]) for _ in range(2)]; tl = [TB([128, 512]) for _ in range(2)]
    ysb = [TB([128, D], BF16) for _ in range(2)]
    b1c = [TB([128, 16]) for _ in range(2)]
    ysc = []
    def loads_xw1(b):
        xr, w1_ = xrows[b % 2], w1b[b % 2]
        DMA(xr.ap, xs_d[b * BLK:(b + 1) * BLK, :].rearrange("(j p) d -> p j d", p=128), [xs_tok], [xr])
        for kc in range(8):
            IDMA(True, ew1f, w_u[:, kc, b:b + 1], w1_[:, kc, :], [w_u], [w1_])

    def loads_w2(b):
        w2_ = w2b[b % 2]
        for kc in range(8):
            IDMA(True, ew2f, w_u[:, kc, b:b + 1], w2_[:, kc, :], [w_u], [w2_])

    def stage_pre(b):
        xr, xT_, bc = xrows[b % 2], xTb[b % 2], b1c[b % 2]
        pb = psum[4 + b % 2]

        def fb(e, pb=pb, b=b):
            ins = None
            for fc in range(16):
                ins = e.matmul(pb[:, fc:fc + 1], b1raw[0:32, fc * 128:(fc + 1) * 128], ohcol[0:32, b:b + 1], start=True, stop=True)
            return ins
        PE(fb, [b1raw, ohcol], [pb])
        DVE(lambda e, bc=bc, pb=pb: e.tensor_copy(out=bc.ap, in_=pb[:, 0:16]), [pb], [bc])
        for j in range(4):
            for half in range(2):
                pt = psum[6 + half]
                ptb = pt.ap.bitcast(BF16)

                def ft(e, ptb=ptb, xr=xr, j=j, half=half):
                    ins = None
                    for q in range(4):
                        kc = half * 4 + q
                        ins = e.transpose(out=ptb[:, q * 128:(q + 1) * 128], in_=xr[:, j, kc * 128:(kc + 1) * 128], identity=ident_b.ap)
                    return ins
                PE(ft, [xr, ident_b], [pt])
                src = ptb[:, 0:512].rearrange("p (q r) -> p q r", q=4)
                dst = xT_[:, half * 4:(half + 1) * 4, j * 128:(j + 1) * 128]
                if half == 0:
                    ACT(lambda e, dst=dst, src=src: e.copy(out=dst, in_=src), [pt], [xT_])
                else:
                    DVE(lambda e, dst=dst, src=src: e.tensor_copy(out=dst, in_=src), [pt], [xT_])

    def stage_w1(b):
        xT_, w1_, aT, bc = xTb[b % 2], w1b[b % 2], actT[b % 2], b1c[b % 2]
        for fc in range(8):
            pg, pl = psum[fc % 2], psum[2 + fc % 2]
            a_, s_, l_ = ta[fc % 2], tsg[fc % 2], tl[fc % 2]
            mm(pg[:, :], [(w1_[:, kc, fc * 128:(fc + 1) * 128], xT_[:, kc, :]) for kc in range(8)], [w1_, xT_], [pg])
            mm(pl[:, :], [(w1_[:, kc, D + fc * 128:D + (fc + 1) * 128], xT_[:, kc, :]) for kc in range(8)], [w1_, xT_], [pl])
            ts(DVE, a_.ap, pg[:, :], bc[:, fc:fc + 1], 7.0, ALU.add, ALU.min, [pg, bc], [a_])
            act(s_.ap, a_.ap, AF.Sigmoid, [a_], [s_], scale=1.702)
            act(l_.ap, pl[:, :], AF.Identity, [pl, bc], [l_], bias=bc[:, 8 + fc:9 + fc])
            ts(DVE, l_.ap, l_.ap, -6.0, 8.0, ALU.max, ALU.min, [l_], [l_])
            tt(DVE, s_.ap, a_.ap, s_.ap, ALU.mult, [a_, s_], [s_])
            tt(POOL, aT[:, fc, :], s_.ap, l_.ap, ALU.mult, [s_, l_], [aT])

    def stage_w2(b):
        w2_, aT = w2b[b % 2], actT[b % 2]
        for j in range(4):
            yb = ysb[j % 2]
            for n in range(2):
                py = psum[4 + (j % 2) * 2 + n]
                N = slice(n * 512, (n + 1) * 512)
                mm(py[:, :], [(aT[:, fc, j * 128:(j + 1) * 128], w2_[:, fc, N]) for fc in range(8)], [aT, w2_], [py])
                if n == 0:
                    ACT(lambda e, yb=yb, py=py, N=N: e.copy(out=yb[:, N], in_=py[:, :]), [py], [yb])
                else:
                    DVE(lambda e, yb=yb, py=py, N=N: e.tensor_copy(out=yb[:, N], in_=py[:, :]), [py], [yb])
            ysc.append(DMA(ys_d[b * BLK + j * 128:b * BLK + (j + 1) * 128, :], yb.ap, [yb], []))

    loads_xw1(0); loads_w2(0)
    stage_pre(0)
    for b in range(NBLK):
        if b + 1 < NBLK:
            loads_xw1(b + 1)
        stage_w1(b)
        if b >= 1:
            stage_w2(b - 1)
        if b + 1 < NBLK:
            loads_w2(b + 1)
            stage_pre(b + 1)
    stage_w2(NBLK - 1)
    ys_tok = Tile(None)
    bs2 = S.add("pool", None, [], [ys_tok])
    for op in ysc:
        bs2.deps.append(op)
    S.barrier()

    TC = Alloc(T6_MID)
    yk = [TC([128, D], BF16) for _ in range(8)]
    accc = [TC([128, D]) for _ in range(2)]
    gT = [TC([32, 128]) for _ in range(2)]
    xt6 = [TC([128, D]) for _ in range(2)]
    final_stores = []
    for i in range(32):
        tok0 = i * 128
        ac, gt, xt_ = accc[i % 2], gT[i % 2], xt6[i % 2]
        for k in range(4):
            IDMA(True, ys_d, slots_u[:, i, k:k + 1], yk[(i % 2) * 4 + k].ap, [ys_tok, slots_u], [yk[(i % 2) * 4 + k]])
        DMA(xt_.ap, x1_scr[tok0:tok0 + 128, :], [x1_tok], [xt_])
        pgt = psum[i % 2]
        PE(lambda e, pgt=pgt, i=i: e.transpose(out=pgt[0:32, 0:128], in_=gates_all[:, i, :], identity=ident.ap), [gates_all, ident], [pgt])
        DVE(lambda e, gt=gt, pgt=pgt: e.tensor_copy(out=gt.ap, in_=pgt[0:32, 0:128]), [pgt], [gt])
        for n in range(2):
            pbias = psum[2 + (i % 2) * 2 + n]
            N = slice(n * 512, (n + 1) * 512)
            mm(pbias[:, :], [(gt[0:32, :], b2_t[0:32, N])], [gt, b2_t], [pbias])
            stt(DVE, ac[:, N], yk[(i % 2) * 4][:, N], gk_all[:, i, 0:1], pbias[:, :], ALU.mult, ALU.add, [yk[(i % 2) * 4], gk_all, pbias], [ac])
        for k in range(1, 4):
            y_ = yk[(i % 2) * 4 + k]
            stt(DVE, ac.ap, y_.ap, gk_all[:, i, k:k + 1], ac.ap, ALU.mult, ALU.add, [y_, gk_all, ac], [ac])
        tt(DVE, ac.ap, ac.ap, g2bc.ap, ALU.mult, [ac, g2bc], [ac])
        tt(DVE, xt_.ap, xt_.ap, ac.ap, ALU.add, [xt_, ac], [xt_])
        final_stores.append(DMA(out_d[tok0:tok0 + 128, :], xt_.ap, [xt_], []))
    return finish(nc, S, es, block, final_stores)


def finish_(nc, S, es, block, final_deps):
    return finish(nc, S, es, block, final_deps)


def finish(nc, S, es, block, final_deps):
    fin = S.add("sp", None, [], [])
    for d in final_deps:
        fin.deps.append(d)
        d.needs_inc = True
    S.emit(nc, es, block)
    es.close()
    return nc, S


_CONSTS = None


def rope_tables():
    theta = 10000.0
    n_rows = SEQ // 64
    row = np.repeat(np.arange(n_rows), 64).astype(np.float32)
    col = np.tile(np.arange(64), n_rows).astype(np.float32)

    def tab(half):
        nf = half // 2
        inv = (theta ** (-(np.arange(0, half, 2, dtype=np.float32)) / half)).astype(np.float32)
        cos = np.ones((2 * half, NT), np.float32)
        sin = np.zeros((2 * half, NT), np.float32)
        for blk, pos in enumerate((row, col)):
            ang = (pos[None, :] * inv[:, None]).astype(np.float32)
            c, s = np.cos(ang).astype(np.float32), np.sin(ang).astype(np.float32)
            base = blk * half
            cos[base:base + nf, NCTX:] = c
            cos[base + nf:base + half, NCTX:] = c
            sin[base:base + nf, NCTX:] = s
            sin[base + nf:base + half, NCTX:] = s
        return cos, sin
    cA, sA = tab(64)
    cB, sB = tab(32)
    return cA, sA, cB, sB


def kernel(**inputs):
    cA, sA, cB, sB = rope_tables()
    nc, S = build_program()
    g = lambda k: np.ascontiguousarray(np.asarray(inputs[k], dtype=np.float32)[0])
    shared = {
        "ada_w": g("ada_w"), "ada_b": g("ada_b"), "norm_mix": g("norm_mix"), "norm_ffn": g("norm_ffn"),
        "w_in": g("w_in"), "gqa_q_norm": g("gqa_q_norm"), "gqa_k_norm": g("gqa_k_norm"),
        "mla_q_a_norm": g("mla_q_a_norm"), "mla_kv_a_norm": g("mla_kv_a_norm"),
        "mla_w_qb": g("mla_w_qb"), "mla_w_kvb": g("mla_w_kvb"), "mla_q_norm": g("mla_q_norm"),
        "mla_k_norm": g("mla_k_norm"), "w_o_gqa": g("w_o_gqa"), "w_o_mla": g("w_o_mla"), "w_out": g("w_out"),
        "router_w": g("router_w"), "router_b": g("router_b"), "expert_w1": g("expert_w1"),
        "expert_b1": g("expert_b1"), "expert_w2": g("expert_w2"), "expert_b2": g("expert_b2"),
        "ident": np.eye(128, dtype=np.float32), "sel0": np.stack([np.ones(128, np.float32), np.zeros(128, np.float32)]), "cosA": cA, "sinA": sA, "cosB": cB, "sinB": sB,
    }
    if SPARSE:
        cst = np.zeros((128, NCONST), np.float32)
        pidx = np.arange(128, dtype=np.float32)
        cst[:, 0:32] = np.arange(32, dtype=np.float32)[None, :]
        cst[:, 32:64] = (np.arange(32, dtype=np.float32) * ECAP)[None, :]
        cst[:, 64:72] = pidx[:, None] + 128.0 * np.arange(8, dtype=np.float32)[None, :]
        cst[:, 72:76] = pidx[:, None] + 128.0 * np.arange(4, dtype=np.float32)[None, :]
        cst[:, 76:204] = (pidx[:, None] < pidx[None, :]).astype(np.float32)
        cst[:, 204:204 + NBLK] = (np.arange(NBLK, dtype=np.float32) * BLK)[None, :]
        shared["consts"] = cst
    x = np.asarray(inputs["x"], np.float32); c = np.asarray(inputs["c"], np.float32)
    ctx = np.asarray(inputs["ctx"], np.float32); c_ctx = np.asarray(inputs["c_ctx"], np.float32)
    ncores = int(os.environ.get("MK_CORES", "8"))
    in_maps = []
    for b in range(ncores):
        m = dict(shared)
        m["x"] = np.ascontiguousarray(x[b]); m["ctx"] = np.ascontiguousarray(ctx[b])
        m["cc"] = np.ascontiguousarray(np.stack([c[b], c_ctx], 0))
        in_maps.append(m)
    res = run_bass_kernel_spmd(nc, in_maps, core_ids=list(range(ncores)))
    if STAGE < 99:
        return res.results
    if ncores < 8:
        return [r["out"] for r in res.results]
    return np.stack([r["out"] for r in res.results], 0).astype(np.float32)
```

```python
import os
import numpy as np
from contextlib import ExitStack
import concourse.bass as bass
import concourse.mybir as mybir
from concourse.bass_utils import run_bass_kernel_spmd

F32 = mybir.dt.float32
BF16 = mybir.dt.bfloat16
ALU = mybir.AluOpType
AF = mybir.ActivationFunctionType
AX = mybir.AxisListType
U32 = mybir.dt.uint32
SPARSE = os.environ.get("MK_SPARSE", "1") == "1"
BLK = 512
NBLK = 16384 // BLK + 32
ECAP = 4096
ZROW = 32 * ECAP
NCONST = 76 + 128 + NBLK

D = 1024
SEQ = 4096
NCTX = 256
NT = SEQ + NCTX
EPS = 1e-6
NEXP = 32
STAGE = int(os.environ.get("MK_STAGE", "99"))


class Tok:
    __slots__ = ("w", "wd", "rs", "rd")

    def __init__(self):
        self.w = None
        self.wd = []
        self.rs = {}
        self.rd = []


class Tile:
    def __init__(self, ap):
        self.ap = ap
        self.tok = Tok()

    def __getitem__(self, k):
        return self.ap[k]


class Op:
    __slots__ = ("eng", "fn", "deps", "dma", "needs_inc", "seg", "val", "sem", "barred", "seq")


ENGS = ("pe", "act", "dve", "pool", "sp")
SEG = 4000
NDMASEM = 40


class Sched:
    def __init__(self):
        self.ops = {e: [] for e in ENGS}
        self.nseq = 0

    def add(self, eng, fn, reads=(), writes=(), dma=False):
        op = Op()
        op.eng, op.fn, op.dma, op.deps, op.needs_inc = eng, fn, dma, [], False
        op.seg = op.val = op.sem = None
        seen = set()

        def dep(d, raw):
            if d is None or id(d) in seen:
                return
            if d.fn is None:
                if d.eng != eng:
                    for dd in d.deps:
                        dep(dd, raw)
                return
            if not d.dma and not dma and d.eng == eng:
                if eng == "pe" or raw is None:
                    return
            seen.add(id(d))
            op.deps.append(d)
            d.needs_inc = True

        for t in reads:
            dep(t.tok.w, True)
            for d in t.tok.wd:
                dep(d, True)
        for t in writes:
            k = t.tok
            if dma:
                if k.w is not None:
                    dep(k.w, None)
            else:
                dep(k.w, None)
                for d in k.wd:
                    dep(d, None)
            for r in k.rs.values():
                dep(r, False)
            for r in k.rd:
                dep(r, False)
        for t in reads:
            if dma:
                t.tok.rd.append(op)
            else:
                t.tok.rs[eng] = op
        for t in writes:
            k = t.tok
            had_readers = bool(k.rs) or bool(k.rd)
            if dma:
                if had_readers or k.w is not None:
                    k.w, k.wd = None, [op]
                else:
                    k.wd.append(op)
            else:
                k.w, k.wd = op, []
            k.rs, k.rd = {}, []
        op.seq = self.nseq
        self.nseq += 1
        self.ops[eng].append(op)
        return op

    def barrier(self):
        lasts = []
        for e in ENGS:
            for op in reversed(self.ops[e]):
                if not op.dma and op.fn is not None:
                    lasts.append(op)
                    break
        dmas = [op for e in ENGS for op in self.ops[e] if op.dma and not getattr(op, "barred", False)]
        for op in dmas:
            op.barred = True
        for e in ENGS:
            b = self.add(e, None, [], [])
            for d in lasts:
                if d.eng != e:
                    b.deps.append(d); d.needs_inc = True
            for d in dmas:
                b.deps.append(d)

    def emit(self, nc, es, block):
        dsems = [es.enter_context(nc.semaphore("dq%d" % i)) for i in range(NDMASEM)]
        dcount = [0] * NDMASEM
        csems = {}
        pools = {"sp": list(range(0, 24)), "pool": list(range(24, 36)), "act": list(range(36, 40))}
        for e in ENGS:
            cnt = 0
            di = 0
            for op in self.ops[e]:
                if op.dma:
                    pl = pools[e]
                    op.sem = pl[di % len(pl)]
                    dcount[op.sem] += 16
                    op.val = dcount[op.sem]
                    di += 1
                elif op.needs_inc:
                    cnt += 1
                    op.seg, op.val = (cnt - 1) // SEG, (cnt - 1) % SEG + 1
                    if (e, op.seg) not in csems:
                        csems[(e, op.seg)] = es.enter_context(nc.semaphore("c_%s_%d" % (e, op.seg)))
        self.n_inst = {e: len(self.ops[e]) for e in ENGS}

        def run(e, eng):
            waited = {}
            nw = 0
            for op in self.ops[e]:
                for d in op.deps:
                    if d.dma:
                        key, val, sem = ("d", d.sem), d.val, dsems[d.sem]
                        if waited.get(key, 0) >= val:
                            continue
                        waited[key] = val
                    else:
                        key, val, sem = ("c", d.eng), (d.seg, d.val), csems[(d.eng, d.seg)]
                        if waited.get(key, (-1, 0)) >= val:
                            continue
                        waited[key] = val
                        val = d.val
                    eng.wait_ge(sem, val)
                    nw += 1
                if op.dma and op.val > 16:
                    key = ("d", op.sem)
                    if waited.get(key, 0) < op.val - 16:
                        eng.wait_ge(dsems[op.sem], op.val - 16)
                        waited[key] = op.val - 16
                if op.fn is None:
                    continue
                ins = op.fn(eng)
                if op.dma:
                    ins.then_inc(dsems[op.sem], 16)
                elif op.needs_inc:
                    ins.then_inc(csems[(e, op.seg)], 1)
            self.n_inst[e] = (len(self.ops[e]), nw)

        @block.tensor
        def _(eng):
            run("pe", eng)

        @block.scalar
        def _(eng):
            run("act", eng)

        @block.vector
        def _(eng):
            run("dve", eng)

        @block.gpsimd
        def _(eng):
            run("pool", eng)

        @block.sync
        def _(eng):
            run("sp", eng)


def build_program():
    nc = bass.Bass("TRN2", target_bir_lowering=False)
    S = Sched()
    es = ExitStack()

    def din(name, shape, dt=F32):
        return nc.dram_tensor(name, list(shape), dt, kind="ExternalInput").ap()

    def dscr(name, shape, dt=BF16):
        if STAGE in (2, 3):
            return nc.dram_tensor(name, list(shape), dt, kind="ExternalOutput").ap()
        return nc.dram_tensor(name, list(shape), dt).ap()

    x_d = din("x", [SEQ, D]); ctx_d = din("ctx", [NCTX, D]); cc_d = din("cc", [2, D])
    ada_w = din("ada_w", [D, 6 * D]); ada_b = din("ada_b", [6 * D])
    norm_mix = din("norm_mix", [D]); norm_ffn = din("norm_ffn", [D])
    w_in = din("w_in", [D, 4032])
    gq_d = din("gqa_q_norm", [128]); gk_d = din("gqa_k_norm", [128])
    gqa_d = din("mla_q_a_norm", [256]); gkva_d = din("mla_kv_a_norm", [128])
    w_qb = din("mla_w_qb", [256, 1536]); w_kvb = din("mla_w_kvb", [128, 2048])
    gmq_d = din("mla_q_norm", [192]); gmk_d = din("mla_k_norm", [192])
    w_oa = din("w_o_gqa", [D, D]); w_ob = din("w_o_mla", [D, D]); w_out = din("w_out", [D, D])
    rw_d = din("router_w", [D, NEXP]); rb_d = din("router_b", [NEXP])
    ew1 = din("expert_w1", [NEXP, D, 2 * D]); eb1 = din("expert_b1", [NEXP, 2 * D])
    ew2 = din("expert_w2", [NEXP, D, D]); eb2 = din("expert_b2", [NEXP, D])
    ident_d = din("ident", [128, 128]); sel0_d = din("sel0", [2, 128])
    cosA_d = din("cosA", [128, NT]); sinA_d = din("sinA", [128, NT])
    cosB_d = din("cosB", [64, NT]); sinB_d = din("sinB", [64, NT])
    consts_d = din("consts", [128, NCONST]) if SPARSE else None
    out_d = nc.dram_tensor("out", [SEQ, D], F32, kind="ExternalOutput").ap()

    qa_scr = dscr("qa_scr", [8, 128, SEQ]); ka_scr = dscr("ka_scr", [2, 128, NT])
    va_scr = dscr("va_scr", [2, 128, 34, 128])
    qb_scr = dscr("qb_scr", [8, 128, SEQ]); qbr_scr = dscr("qbr_scr", [8, 64, SEQ])
    kb_scr = dscr("kb_scr", [8, 128, NT]); kbr_scr = dscr("kbr_scr", [8, 64, NT])
    vb_scr = dscr("vb_scr", [8, 128, 34, 128])
    o_scr = (nc.dram_tensor("o_scr", [16, 128, SEQ], BF16, kind="ExternalOutput").ap() if STAGE in (4, 5) else dscr("o_scr", [16, 128, SEQ]))
    x1_scr = out_d if STAGE == 5 else dscr("x1_scr", [SEQ, D], F32)
    dbg_d = None
    if STAGE < 99:
        dbg_d = nc.dram_tensor("dbg", [128, 8 * NT], F32, kind="ExternalOutput").ap()

    ARENA_F = 52400
    arena = es.enter_context(nc.sbuf_tensor("arena", [128, ARENA_F], F32))
    psum = [Tile(es.enter_context(nc.psum_tensor("ps%d" % i, [128, 512], F32))[:, :]) for i in range(8)]
    block = es.enter_context(nc.Block())

    class Alloc:
        def __init__(self, base=0):
            self.off = base

        def __call__(self, shape, dt=F32, parts=128):
            n = int(np.prod(shape[1:]))
            nf = n if dt in (F32, U32) else (n + 1) // 2
            nf = (nf + 7) // 8 * 8
            a = arena[:, self.off:self.off + nf]
            self.off += nf
            assert self.off <= ARENA_F, "SBUF arena overflow %d" % self.off
            if dt != F32:
                a = a.bitcast(dt)
            a = a[:, 0:n]
            a = a[0:shape[0]]
            if len(shape) == 3:
                a = a.rearrange("p (a b) -> p a b", a=shape[1])
            elif len(shape) == 4:
                a = a.rearrange("p (a b c) -> p a b c", a=shape[1], b=shape[2])
            return Tile(a)

    def PE(fn, r, w): S.add("pe", fn, r, w)
    def ACT(fn, r, w): S.add("act", fn, r, w)
    def DVE(fn, r, w): S.add("dve", fn, r, w)
    def POOL(fn, r, w): S.add("pool", fn, r, w)
    def DMA(out, in_, r, w, q="sp", nonc=False):
        def fn(e):
            if nonc:
                with nc.allow_non_contiguous_dma(reason="tiny vector load"):
                    return e.dma_start(out=out, in_=in_)
            return e.dma_start(out=out, in_=in_)
        return S.add(q, fn, r, w, dma=True)

    def mm(out, pairs, r, w):
        n = len(pairs)

        def fn(e):
            ins = None
            for i, (l, rh) in enumerate(pairs):
                ins = e.matmul(out, l, rh, start=(i == 0), stop=(i == n - 1))
            return ins
        PE(fn, r, w)

    def ts(eng, out, in0, s1, s2, op0, op1, r, w):
        if s2 is None:
            eng(lambda e: e.tensor_scalar(out=out, in0=in0, scalar1=s1, scalar2=None, op0=op0), r, w)
        else:
            eng(lambda e: e.tensor_scalar(out=out, in0=in0, scalar1=s1, scalar2=s2, op0=op0, op1=op1), r, w)

    def tt(eng, out, in0, in1, op, r, w):
        eng(lambda e: e.tensor_tensor(out=out, in0=in0, in1=in1, op=op), r, w)

    def stt(eng, out, in0, sc, in1, op0, op1, r, w):
        eng(lambda e: e.scalar_tensor_tensor(out=out, in0=in0, scalar=sc, in1=in1, op0=op0, op1=op1), r, w)

    def act(out, in_, func, r, w, bias=None, scale=None, accum=None):
        kw = {}
        if bias is not None: kw["bias"] = bias
        if scale is not None: kw["scale"] = scale
        if accum is not None: kw["accum_out"] = accum
        ACT(lambda e: e.activation(out=out, in_=in_, func=func, **kw), r, w)

    def rstd_from(out_t, out_ap, ss_ap, ss_deps, n):
        act(out_ap, ss_ap, AF.Ln, ss_deps + [eps_c], [out_t], bias=eps_c[0:out_ap.shape[0], 0:1], scale=1.0 / n)
        act(out_ap, out_ap, AF.Exp, [out_t], [out_t], scale=-0.5)

    P = Alloc(0)
    ident = P([128, 128]); ones_f = P([128, 128]); ones_b = P([128, 128], BF16)
    modT = P([128, 48, 2]); adabT = P([128, 48]); nmT = P([128, 8]); nfT = P([128, 8])
    A1 = P([128, 8, 2]); A2 = P([128, 8])
    g1bc = P([128, D]); g2bc = P([128, D])
    gqT = P([128, 2]); gkT = P([128, 2])
    gqaT = P([128, 2]); gkvaT = P([128, 1])
    gmqT = P([128, 3]); gmkT = P([128, 3])
    ss1 = P([128, 4]); rs1 = P([128, 4])
    zero_c = P([128, 1]); eps_c = P([128, 1])
    HT_OFF = P.off
    hT = P([128, 8, NT], BF16)
    PERSIST_END = P.off

    DMA(ident.ap, ident_d, [], [ident])
    POOL(lambda e: e.memset(ones_f.ap, 1.0), [], [ones_f])
    POOL(lambda e: e.memset(ones_b.ap, 1.0), [], [ones_b])
    POOL(lambda e: e.memset(zero_c.ap, 0.0), [], [zero_c])
    POOL(lambda e: e.memset(eps_c.ap, EPS), [], [eps_c])
    DMA(nmT.ap, norm_mix.rearrange("(j p) -> p j", p=128), [], [nmT], nonc=True)
    DMA(nfT.ap, norm_ffn.rearrange("(j p) -> p j", p=128), [], [nfT], nonc=True)

    def colvec(dst_t, col, src_d, lo, hi, p0=0):
        DMA(dst_t[p0:p0 + (hi - lo), col:col + 1], src_d[lo:hi].rearrange("(p o) -> p o", o=1), [], [dst_t], nonc=True)

    for (t_, d_) in ((gqT, gq_d), (gkT, gk_d)):
        colvec(t_, 0, d_, 0, 128)
        colvec(t_, 1, d_, 32, 64, 0); colvec(t_, 1, d_, 0, 32, 32)
        colvec(t_, 1, d_, 96, 128, 64); colvec(t_, 1, d_, 64, 96, 96)
    colvec(gqaT, 0, gqa_d, 0, 128); colvec(gqaT, 1, gqa_d, 128, 256)
    colvec(gkvaT, 0, gkva_d, 0, 128)
    for (t_, d_) in ((gmqT, gmq_d), (gmkT, gmk_d)):
        colvec(t_, 0, d_, 0, 128)
        colvec(t_, 1, d_, 128, 192, 0)
        colvec(t_, 2, d_, 144, 160, 0); colvec(t_, 2, d_, 128, 144, 16)
        colvec(t_, 2, d_, 176, 192, 32); colvec(t_, 2, d_, 160, 176, 48)

    T0 = Alloc(PERSIST_END)
    ccT = T0([128, 8, 2]); scT = T0([128, 8, 2])
    wblk = [T0([128, 8, 1024]) for _ in range(2)]
    modrow = T0([2, 6 * D]); adab2 = T0([2, 6 * D]); sel0 = T0([2, 128])
    abtmp = [T0([128, D]) for _ in range(2)]; nfbc = T0([128, D])
    ab_scr = dscr("ab_scr", [2, 128, D], F32)
    for r in range(2):
        DMA(ccT[:, :, r], cc_d[r].rearrange("(j p) -> p j", p=128), [], [ccT], nonc=True)
        DMA(adab2[r:r + 1, :], ada_b.rearrange("(o n) -> o n", o=1), [], [adab2])
    DMA(sel0.ap, sel0_d, [], [sel0])
    act(scT.ap, ccT.ap, AF.Silu, [ccT], [scT])
    ada_v = ada_w.rearrange("(kc p) n -> p kc n", p=128)
    for blk in range(6):
        wb = wblk[blk % 2]
        DMA(wb.ap, ada_v[:, :, blk * 1024:(blk + 1) * 1024], [], [wb])
        for half in range(2):
            pr = psum[(blk * 2 + half) % 4]
            cols = slice(blk * 1024 + half * 512, blk * 1024 + (half + 1) * 512)
            mm(pr[0:2, :], [(scT[:, kc, :], wb[:, kc, half * 512:(half + 1) * 512]) for kc in range(8)], [scT, wb], [pr])
            tt(DVE, modrow[:, cols], pr[0:2, :], adab2[:, cols], ALU.add, [pr, adab2], [modrow])
    pst = psum[4]

    def ftm(e):
        ins = None
        for j in range(48):
            ins = e.transpose(out=pst[:, 2 * j:2 * j + 2], in_=modrow[0:2, j * 128:(j + 1) * 128], identity=ident[0:2, 0:2])
        return ins
    PE(ftm, [modrow, ident], [pst])
    DVE(lambda e: e.tensor_copy(out=modT.ap, in_=pst[:, 0:96].rearrange("p (j r) -> p j r", r=2)), [pst], [modT])
    for blk in ((2, 5, 3, 4) if SPARSE else (2, 5)):
        gdst = {2: g1bc, 5: g2bc, 3: abtmp[0], 4: abtmp[1]}[blk]
        for half in range(2):
            pg = psum[5 + half]
            cols = slice(blk * 1024 + half * 512, blk * 1024 + (half + 1) * 512)
            mm(pg[:, :], [(sel0.ap, modrow[0:2, cols])], [sel0, modrow], [pg])
            if half == 0:
                ACT(lambda e, gdst=gdst, pg=pg: e.copy(out=gdst[:, 0:512], in_=pg[:, :]), [pg], [gdst])
            else:
                DVE(lambda e, gdst=gdst, pg=pg: e.tensor_copy(out=gdst[:, 512:1024], in_=pg[:, :]), [pg], [gdst])
        if blk == 4:
            DMA(nfbc.ap, norm_ffn.partition_broadcast(128), [], [nfbc])
            stt(DVE, gdst.ap, gdst.ap, 1.0, nfbc.ap, ALU.add, ALU.mult, [gdst, nfbc], [gdst])
        if blk in (3, 4):
            DMA(ab_scr[blk - 3], gdst.ap, [gdst], [])
    for r in range(2):
        stt(DVE, A1[:, :, r], modT[:, 8:16, r], 1.0, nmT.ap, ALU.add, ALU.mult, [modT, nmT], [A1])
    stt(DVE, A2.ap, modT[:, 32:40, 0], 1.0, nfT.ap, ALU.add, ALU.mult, [modT, nfT], [A2])

    S.barrier()
    T1 = Alloc(PERSIST_END)
    xt = [T1([128, D]) for _ in range(4)]
    xn = [T1([128, D]) for _ in range(4)]
    NB = {"xt": xt, "xn": xn}

    def nm_A(i, src_ap, src_deps=()):
        xt_, xn_ = NB["xt"][i % len(NB["xt"])], NB["xn"][i % len(NB["xn"])]
        ss_, rs_ = ss1[:, i % 4:i % 4 + 1], rs1[:, i % 4:i % 4 + 1]
        DMA(xt_.ap, src_ap, list(src_deps), [xt_])
        act(xn_.ap, xt_.ap, AF.Square, [xt_], [xn_, ss1], accum=ss_)
        return (xt_, xn_, ss_, rs_)

    def nm_B(cx):
        xt_, xn_, ss_, rs_ = cx
        rstd_from(rs1, rs_, ss_, [ss1], D)
        ACT(lambda e: e.activation(out=xn_.ap, in_=xt_.ap, func=AF.Copy, scale=rs_), [xt_, rs1], [xn_])

    def nm_C(cx, A_ap, S_ap, dst_fn, ps_pair):
        xt_, xn_, ss_, rs_ = cx
        for half in range(2):
            pst = ps_pair[half]

            def fn(e, half=half, pst=pst):
                ins = None
                for jj in range(4):
                    j = half * 4 + jj
                    ins = e.transpose(out=pst[:, jj * 128:(jj + 1) * 128], in_=xn_[:, j * 128:(j + 1) * 128], identity=ident.ap)
                return ins
            PE(fn, [xn_, ident], [pst])
            for jj in range(4):
                j = half * 4 + jj
                dst_ap, dst_t = dst_fn(j)
                if jj % 2 == 0:
                    ts(DVE, dst_ap, pst[:, jj * 128:(jj + 1) * 128], A_ap[:, j:j + 1], S_ap[:, j:j + 1], ALU.mult, ALU.add,
                       [pst, A1, A2, modT], [dst_t])
                else:
                    act(dst_ap, pst[:, jj * 128:(jj + 1) * 128], AF.Identity, [pst, A1, A2, modT], [dst_t],
                        bias=S_ap[:, j:j + 1], scale=A_ap[:, j:j + 1])

    def norm_mod_T(i, src_ap, A_ap, S_ap, dst_fn, ps_pair, extra_f32=None, src_deps=()):
        cx = nm_A(i, src_ap, src_deps)
        nm_B(cx)
        nm_C(cx, A_ap, S_ap, dst_fn, ps_pair)
        return cx[0]

    hT_chunks = [Tile(hT.ap) for _ in range(9)]

    p1ctx = {}
    for step in range(34 + 2):
        if step < 34:
            i = step
            src = ctx_d[i * 128:(i + 1) * 128, :] if i < 2 else x_d[(i - 2) * 128:(i - 1) * 128, :]
            p1ctx[i] = nm_A(i, src)
        if 0 <= step - 1 < 34:
            nm_B(p1ctx[step - 1])
        if 0 <= step - 2 < 34:
            i = step - 2
            r = 1 if i < 2 else 0
            c0 = i * 128
            chunk = hT_chunks[c0 // 512]
            nm_C(p1ctx[i], A1[:, :, r], modT[:, 0:8, r], lambda j, c0=c0, chunk=chunk: (hT[:, j, c0:c0 + 128], chunk),
                 (psum[(i % 4) * 2], psum[(i % 4) * 2 + 1]))

    final_deps = []

    def dbg_dump(src_tile, ncols, col0=0):
        pass

    if STAGE == 1:
        TD = Alloc(T1.off)
        for j in range(8):
            for c in range(9):
                w = min(512, NT - c * 512)
                tmp = TD([128, 512])
                ts(DVE, tmp[:, 0:w], hT[:, j, c * 512:c * 512 + w], 1.0, None, ALU.mult, None, [hT_chunks[c]], [tmp])
                final_deps.append(DMA(dbg_d[:, j * NT + c * 512:j * NT + c * 512 + w], tmp[:, 0:w], [tmp], []))
                if TD.off > ARENA_F - 600:
                    TD = Alloc(T1.off)
        return finish(nc, S, es, block, final_deps)


    def all_dmas():
        return [op for e_ in ENGS for op in S.ops[e_] if op.dma]

    w_in_v = w_in.rearrange("(kc p) n -> p kc n", p=128)
    chunks = [(c, c * 512, min(512, NT - c * 512)) for c in range(9)]

    def qstore(dst_h, src_t, c, c0, w, parts=128):
        if c == 0:
            return DMA(dst_h[:, 0:256], src_t[0:parts, 256:512], [src_t], [])
        return DMA(dst_h[:, c0 - 256:c0 - 256 + w], src_t[0:parts, 0:w], [src_t], [])

    S.barrier()
    T2 = Alloc(PERSIST_END)
    wqa = T2([128, 8, 1536], BF16); wrot = T2([128, 8, 1280], BF16)
    cosT = [T2([128, 512]) for _ in range(2)]; sinT = [T2([128, 512]) for _ in range(2)]
    sqb = [T2([128, 512], BF16) for _ in range(2)]; rsd = [T2([128, 512]) for _ in range(2)]
    t1b = [T2([128, 512]) for _ in range(2)]; t2b = [T2([128, 512]) for _ in range(2)]
    qob = [T2([128, 512], BF16) for _ in range(3)]
    vta = [T2([128, 4, 256], BF16) for _ in range(2)]
    DMA(wqa.ap, w_in_v[:, :, 0:1536], [], [wqa], q="pool")
    for kc in range(8):
        sv = wqa[:, kc, 0:1280].rearrange("p (h d) -> p h d", d=128)
        dv = wrot[:, kc, :].rearrange("p (h d) -> p h d", d=128)
        for (dlo, slo, neg) in ((0, 32, True), (32, 0, False), (64, 96, True), (96, 64, False)):
            if neg:
                ts(DVE, dv[:, :, dlo:dlo + 32], sv[:, :, slo:slo + 32], -1.0, None, ALU.mult, None, [wqa], [wrot])
            else:
                ACT(lambda e, dv=dv, sv=sv, dlo=dlo, slo=slo: e.copy(out=dv[:, :, dlo:dlo + 32], in_=sv[:, :, slo:slo + 32]), [wqa], [wrot])
    it = 0
    for (c, c0, w) in chunks:
        hc_ = hT_chunks[c]
        cs_, sn_ = cosT[c % 2], sinT[c % 2]
        DMA(cs_[:, 0:w], cosA_d[:, c0:c0 + w], [], [cs_])
        DMA(sn_[:, 0:w], sinA_d[:, c0:c0 + w], [], [sn_])
        for hh in range(10):
            raw, rot, ssp = psum[hh % 2], psum[2 + hh % 2], psum[4 + hh % 2]
            cols = slice(hh * 128, (hh + 1) * 128)
            mm(raw[:, 0:w], [(wqa[:, kc, cols], hT[:, kc, c0:c0 + w]) for kc in range(8)], [wqa, hc_], [raw])
            mm(rot[:, 0:w], [(wrot[:, kc, cols], hT[:, kc, c0:c0 + w]) for kc in range(8)], [wrot, hc_], [rot])
            sq_, rs_, t1_, t2_, qo_ = sqb[it % 2], rsd[it % 2], t1b[it % 2], t2b[it % 2], qob[it % 3]
            it += 1
            act(sq_[:, 0:w], raw[:, 0:w], AF.Square, [raw], [sq_])
            mm(ssp[:, 0:w], [(ones_b.ap, sq_[:, 0:w])], [ones_b, sq_], [ssp])
            rstd_from(rs_, rs_[:, 0:w], ssp[:, 0:w], [ssp], 128)
            g_ = gqT if hh < 8 else gkT
            stt(DVE, t1_[:, 0:w], raw[:, 0:w], g_[:, 0:1], cs_[:, 0:w], ALU.mult, ALU.mult, [raw, g_, cs_, sq_], [t1_])
            stt(DVE, t2_[:, 0:w], rot[:, 0:w], g_[:, 1:2], sn_[:, 0:w], ALU.mult, ALU.mult, [rot, g_, sn_], [t2_])
            tt(POOL, t1_[:, 0:w], t1_[:, 0:w], t2_[:, 0:w], ALU.add, [t1_, t2_], [t1_])
            tt(DVE, qo_[:, 0:w], t1_[:, 0:w], rs_[:, 0:w], ALU.mult, [t1_, rs_], [qo_])
            if hh < 8:
                qstore(qa_scr[hh], qo_, c, c0, w)
            else:
                DMA(ka_scr[hh - 8, :, c0:c0 + w], qo_[:, 0:w], [qo_], [])
        vt_ = vta[c % 2]
        nsub = w // 128
        for sub in range(nsub):
            pv = psum[6 + sub % 2]
            mm(pv[:, 0:256], [(hT[:, kc, c0 + sub * 128:c0 + (sub + 1) * 128], wqa[:, kc, 1280:1536]) for kc in range(8)],
               [wqa, hc_], [pv])
            ACT(lambda e, vt_=vt_, pv=pv, sub=sub: e.copy(out=vt_[:, sub, :], in_=pv[:, 0:256]), [pv], [vt_])
        for g in range(2):
            DMA(va_scr[g, :, c * 4:c * 4 + nsub, :], vt_[:, 0:nsub, g * 128:(g + 1) * 128], [vt_], [])

    if STAGE == 2:
        return finish(nc, S, es, block, all_dmas())

    S.barrier()
    T3 = Alloc(PERSIST_END)
    wm = T3([128, 8, 448], BF16); wkrot = T3([128, 8, 64], BF16)
    wqb_t = T3([128, 2, 1536], BF16); wqb_rot = T3([128, 2, 8, 64], BF16)
    wkvb_t = T3([128, 2048], BF16); wv_t = T3([128, 1024], BF16)
    cosBt = [T3([128, 512]) for _ in range(2)]; sinBt = [T3([128, 512]) for _ in range(2)]
    sqa = T3([128, 2, 512], BF16); qan = T3([128, 2, 512], BF16); ckvn = T3([128, 512], BF16)
    sqk = T3([128, 512], BF16); krsq = T3([128, 512], BF16); krr = T3([128, 512]); krt2 = T3([128, 512])
    rs3 = [T3([128, 512]) for _ in range(4)]
    sqn = [T3([128, 512], BF16) for _ in range(2)]; sqr = [T3([128, 512], BF16) for _ in range(2)]
    t13 = [T3([128, 512]) for _ in range(2)]; t23 = [T3([128, 512]) for _ in range(2)]
    o3 = [T3([128, 512], BF16) for _ in range(8)]
    vtb = [T3([128, 4, 1024], BF16) for _ in range(2)]
    DMA(wm.ap, w_in_v[:, :, 1536:1984], [], [wm], q="pool")
    DMA(wqb_t.ap, w_qb.rearrange("(c p) n -> p c n", p=128), [], [wqb_t], q="pool")
    DMA(wkvb_t.ap, w_kvb, [], [wkvb_t], q="pool")
    POOL(lambda e: e.tensor_copy(out=wv_t.ap.rearrange("p (h d) -> p h d", d=128),
                                 in_=wkvb_t.ap.rearrange("p (h t d) -> p h t d", t=2, d=128)[:, :, 1, :]), [wkvb_t], [wv_t])
    for (dlo, slo, neg) in ((0, 16, True), (16, 0, False), (32, 48, True), (48, 32, False)):
        for kc in range(8):
            if neg:
                ts(DVE, wkrot[:, kc, dlo:dlo + 16], wm[:, kc, 384 + slo:384 + slo + 16], -1.0, None, ALU.mult, None, [wm], [wkrot])
            else:
                ACT(lambda e, kc=kc, dlo=dlo, slo=slo: e.copy(out=wkrot[:, kc, dlo:dlo + 16], in_=wm[:, kc, 384 + slo:384 + slo + 16]), [wm], [wkrot])
        for c2 in range(2):
            sv = wqb_t[:, c2, :].rearrange("p (h d) -> p h d", d=192)
            if neg:
                ts(DVE, wqb_rot[:, c2, :, dlo:dlo + 16], sv[:, :, 128 + slo:128 + slo + 16], -1.0, None, ALU.mult, None, [wqb_t], [wqb_rot])
            else:
                ACT(lambda e, c2=c2, sv=sv, dlo=dlo, slo=slo: e.copy(out=wqb_rot[:, c2, :, dlo:dlo + 16], in_=sv[:, :, 128 + slo:128 + slo + 16]), [wqb_t], [wqb_rot])
    it = 0
    for (c, c0, w) in chunks:
        hc_ = hT_chunks[c]
        W = slice(0, w)
        cs_, sn_ = cosBt[c % 2], sinBt[c % 2]
        DMA(cs_[0:64, W], cosB_d[:, c0:c0 + w], [], [cs_])
        DMA(sn_[0:64, W], sinB_d[:, c0:c0 + w], [], [sn_])
        for c2 in range(2):
            mm(psum[c2][:, W], [(wm[:, kc, c2 * 128:(c2 + 1) * 128], hT[:, kc, c0:c0 + w]) for kc in range(8)], [wm, hc_], [psum[c2]])
            act(sqa[:, c2, W], psum[c2][:, W], AF.Square, [psum[c2]], [sqa])
        mm(psum[3][:, W], [(ones_b.ap, sqa[:, 0, W]), (ones_b.ap, sqa[:, 1, W])], [ones_b, sqa], [psum[3]])
        r_ = rs3[0]
        rstd_from(r_, r_[:, W], psum[3][:, W], [psum[3]], 256)
        for c2 in range(2):
            stt(DVE, qan[:, c2, W], psum[c2][:, W], gqaT[:, c2:c2 + 1], r_[:, W], ALU.mult, ALU.mult, [psum[c2], gqaT, r_, sqa], [qan])
        mm(psum[4][:, W], [(wm[:, kc, 256:384], hT[:, kc, c0:c0 + w]) for kc in range(8)], [wm, hc_], [psum[4]])
        act(sqk[:, W], psum[4][:, W], AF.Square, [psum[4]], [sqk])
        mm(psum[5][:, W], [(ones_b.ap, sqk[:, W])], [ones_b, sqk], [psum[5]])
        r_ = rs3[1]
        rstd_from(r_, r_[:, W], psum[5][:, W], [psum[5]], 128)
        stt(DVE, ckvn[:, W], psum[4][:, W], gkvaT[:, 0:1], r_[:, W], ALU.mult, ALU.mult, [psum[4], gkvaT, r_, sqk], [ckvn])
        mm(psum[6][0:64, W], [(wm[:, kc, 384:448], hT[:, kc, c0:c0 + w]) for kc in range(8)], [wm, hc_], [psum[6]])
        mm(psum[7][0:64, W], [(wkrot[:, kc, :], hT[:, kc, c0:c0 + w]) for kc in range(8)], [wkrot, hc_], [psum[7]])
        act(krsq[0:64, W], psum[6][0:64, W], AF.Square, [psum[6]], [krsq])
        stt(DVE, krr[0:64, W], psum[6][0:64, W], gmkT[0:64, 1:2], cs_[0:64, W], ALU.mult, ALU.mult, [psum[6], gmkT, cs_, krsq], [krr])
        stt(DVE, krt2[0:64, W], psum[7][0:64, W], gmkT[0:64, 2:3], sn_[0:64, W], ALU.mult, ALU.mult, [psum[7], gmkT, sn_], [krt2])
        tt(POOL, krr[0:64, W], krr[0:64, W], krt2[0:64, W], ALU.add, [krr, krt2], [krr])
        vt_ = vtb[c % 2]
        nsub = w // 128
        for sub in range(nsub):
            for half in range(2):
                pv = psum[6 + half]
                mm(pv[:, :], [(ckvn[:, sub * 128:(sub + 1) * 128], wv_t[:, half * 512:(half + 1) * 512])], [ckvn, wv_t], [pv])
                ACT(lambda e, vt_=vt_, pv=pv, sub=sub, half=half: e.copy(out=vt_[:, sub, half * 512:(half + 1) * 512], in_=pv[:, :]), [pv], [vt_])
        for h in range(8):
            DMA(vb_scr[h, :, c * 4:c * 4 + nsub, :], vt_[:, 0:nsub, h * 128:(h + 1) * 128], [vt_], [])
        for h in range(8):
            par = h % 2
            pn, pr, prr, pss = psum[par], psum[2 + par], psum[4 + par], psum[6 + par]
            sqn_, sqr_, t1_, t2_ = sqn[par], sqr[par], t13[par], t23[par]
            rq_ = rs3[2 + par]
            mm(pn[:, W], [(wqb_t[:, c2, h * 192:h * 192 + 128], qan[:, c2, W]) for c2 in range(2)], [wqb_t, qan], [pn])
            mm(pr[0:64, W], [(wqb_t[:, c2, h * 192 + 128:h * 192 + 192], qan[:, c2, W]) for c2 in range(2)], [wqb_t, qan], [pr])
            mm(prr[0:64, W], [(wqb_rot[:, c2, h, :], qan[:, c2, W]) for c2 in range(2)], [wqb_rot, qan], [prr])
            act(sqn_[:, W], pn[:, W], AF.Square, [pn], [sqn_])
            act(sqr_[0:64, W], pr[0:64, W], AF.Square, [pr], [sqr_])
            mm(pss[:, W], [(ones_b.ap, sqn_[:, W]), (ones_b[0:64, :], sqr_[0:64, W])], [ones_b, sqn_, sqr_], [pss])
            rstd_from(rq_, rq_[:, W], pss[:, W], [pss], 192)
            oq, oqr = o3[par * 4], o3[par * 4 + 1]
            stt(DVE, oq[:, W], pn[:, W], gmqT[:, 0:1], rq_[:, W], ALU.mult, ALU.mult, [pn, gmqT, rq_, sqn_], [oq])
            qstore(qb_scr[h], oq, c, c0, w)
            stt(DVE, t1_[0:64, W], pr[0:64, W], gmqT[0:64, 1:2], cs_[0:64, W], ALU.mult, ALU.mult, [pr, gmqT, cs_, sqr_], [t1_])
            stt(DVE, t2_[0:64, W], prr[0:64, W], gmqT[0:64, 2:3], sn_[0:64, W], ALU.mult, ALU.mult, [prr, gmqT, sn_], [t2_])
            tt(POOL, t1_[0:64, W], t1_[0:64, W], t2_[0:64, W], ALU.add, [t1_, t2_], [t1_])
            tt(DVE, oqr[0:64, W], t1_[0:64, W], rq_[0:64, W], ALU.mult, [t1_, rq_], [oqr])
            qstore(qbr_scr[h], oqr, c, c0, w, parts=64)
        for h in range(8):
            par = h % 2
            pk, psk = psum[h % 4], psum[4 + h % 4]
            sqn_, rk_ = sqn[par], rs3[2 + par]
            ok, okr = o3[par * 4 + 2], o3[par * 4 + 3]
            mm(pk[:, W], [(wkvb_t[:, h * 256:h * 256 + 128], ckvn[:, W])], [wkvb_t, ckvn], [pk])
            act(sqn_[:, W], pk[:, W], AF.Square, [pk], [sqn_])
            mm(psk[:, W], [(ones_b.ap, sqn_[:, W]), (ones_b[0:64, :], krsq[0:64, W])], [ones_b, sqn_, krsq], [psk])
            rstd_from(rk_, rk_[:, W], psk[:, W], [psk], 192)
            stt(DVE, ok[:, W], pk[:, W], gmkT[:, 0:1], rk_[:, W], ALU.mult, ALU.mult, [pk, gmkT, rk_, sqn_], [ok])
            DMA(kb_scr[h, :, c0:c0 + w], ok[:, W], [ok], [])
            tt(POOL, okr[0:64, W], krr[0:64, W], rk_[0:64, W], ALU.mult, [krr, rk_], [okr])
            DMA(kbr_scr[h, :, c0:c0 + w], okr[0:64, W], [okr], [])

    if STAGE == 3:
        return finish(nc, S, es, block, all_dmas())

    scr_tok = Tile(None)
    bar = S.add("sp", None, [], [scr_tok])
    for e_ in ENGS:
        for op in S.ops[e_]:
            if op.dma and op is not bar:
                bar.deps.append(op); op.needs_inc = True

    S.barrier()
    T4 = Alloc(PERSIST_END)
    qTb = [T4([128, SEQ], BF16) for _ in range(2)]; qrb = [T4([128, SEQ], BF16) for _ in range(2)]
    kTb = [T4([128, NT], BF16) for _ in range(2)]; krb = [T4([128, NT], BF16) for _ in range(2)]
    Vb = [T4([128, 34, 128], BF16) for _ in range(2)]
    pT = [T4([128, 512], BF16) for _ in range(4)]
    rinv = [T4([128, 512]) for _ in range(2)]
    obuf = [T4([128, 512], BF16) for _ in range(2)]
    o_stores = []
    LAG = 2
    zfill_ops = []
    if SPARSE:
        xs_d = dscr("xs_scr", [NBLK * BLK, D], BF16)
        zt4 = T4([128, 4096], BF16)
        POOL(lambda e: e.memset(zt4.ap, 0.0), [], [zt4])
        for zi in range(NBLK * BLK // 512):
            zfill_ops.append(DMA(xs_d[zi * 512:(zi + 1) * 512, :].rearrange("(p r) d -> p (r d)", p=128), zt4.ap, [zt4], [], q="pool"))
    for t_ in qrb + krb:
        POOL(lambda e, t_=t_: e.memset(t_[64:128, :], 0.0), [], [t_])
    for hd in range(16):
        b = hd % 2
        mla = hd >= 8
        h = hd - 8 if mla else hd
        qT_, kT_, V_, qr_, kr_ = qTb[b], kTb[b], Vb[b], qrb[b], krb[b]
        if not mla:
            g = h // 4
            DMA(qT_.ap, qa_scr[h], [scr_tok], [qT_])
            if h % 4 == 0:
                kg, vg = Tile(None), Tile(None)
                kT_g, V_g = kTb[g % 2], Vb[g % 2]
                DMA(kT_g.ap, ka_scr[g], [scr_tok], [kT_g])
                DMA(V_g.ap, va_scr[g], [scr_tok], [V_g])
            kT_, V_ = kT_g, V_g
            scale = 128 ** -0.5
        else:
            DMA(qT_.ap, qb_scr[h], [scr_tok], [qT_])
            DMA(qr_[0:64, :], qbr_scr[h], [scr_tok], [qr_])
            DMA(kT_.ap, kb_scr[h], [scr_tok], [kT_])
            DMA(kr_[0:64, :], kbr_scr[h], [scr_tok], [kr_])
            DMA(V_.ap, vb_scr[h], [scr_tok], [V_])
            scale = 192 ** -0.5
        for qi in range(8):
            o_ps = psum[4 + qi % 2]
            sum_ps = psum[6 + qi % 2]
            Q = slice(qi * 512, (qi + 1) * 512)
            for step in range(34 + LAG):
                if step < 34:
                    ti = step
                    s_ps = psum[ti % 4]
                    Kc = slice(ti * 128, (ti + 1) * 128)
                    pairs = [(kT_[:, Kc], qT_[:, Q])]
                    rds = [kT_, qT_]
                    if mla:
                        pairs.append((kr_[:, Kc], qr_[:, Q]))
                        rds += [kr_, qr_]
                    mm(s_ps[:, :], pairs, rds, [s_ps])
                    p_ = pT[ti % 4]
                    act(p_.ap, s_ps[:, :], AF.Exp, [s_ps], [p_], scale=scale)
                if step >= LAG:
                    ti = step - LAG
                    p_ = pT[ti % 4]
                    PE(lambda e, o_ps=o_ps, V_=V_, ti=ti, p_=p_: e.matmul(o_ps[:, :], V_[:, ti, :], p_.ap, start=(ti == 0), stop=(ti == 33)),
                       [V_, p_], [o_ps])
                    PE(lambda e, sum_ps=sum_ps, ti=ti, p_=p_: e.matmul(sum_ps[:, :], ones_b.ap, p_.ap, start=(ti == 0), stop=(ti == 33)),
                       [ones_b, p_], [sum_ps])
            ri, ob = rinv[qi % 2], obuf[qi % 2]
            DVE(lambda e, ri=ri, sum_ps=sum_ps: e.reciprocal(out=ri.ap, in_=sum_ps[:, :]), [sum_ps], [ri])
            tt(DVE, ob.ap, o_ps[:, :], ri.ap, ALU.mult, [o_ps, ri], [ob])
            o_stores.append(DMA(o_scr[hd, :, Q], ob.ap, [ob], []))

    o_tok = Tile(None)
    bar2 = S.add("sp", None, [], [o_tok])
    for op in o_stores:
        bar2.deps.append(op); op.needs_inc = True

    if STAGE == 4:
        return finish(nc, S, es, block, o_stores)

    S.barrier()
    T5 = Alloc(PERSIST_END)
    wg = T5([128, 8, 2048], BF16); woa_t = T5([128, 8, D], BF16); wob_t = T5([128, 8, D], BF16); wout_t = T5([128, 8, D], BF16)
    DMA(wg.ap, w_in_v[:, :, 1984:4032], [], [wg], q="pool")
    DMA(woa_t.ap, w_oa.rearrange("(kc p) n -> p kc n", p=128), [], [woa_t], q="pool")
    DMA(wob_t.ap, w_ob.rearrange("(kc p) n -> p kc n", p=128), [], [wob_t], q="pool")
    DMA(wout_t.ap, w_out.rearrange("(kc p) n -> p kc n", p=128), [], [wout_t], q="pool")
    oTt = [T5([128, 16, 512], BF16) for _ in range(1)]
    yT = [T5([128, 8, 512], BF16) for _ in range(1)]
    sgb = [T5([128, 512]) for _ in range(4)]
    xt5 = [T5([128, D]) for _ in range(1)]
    x1t = [T5([128, D]) for _ in range(1)]
    x1_stores = []
    for qi in range(8):
        Q = slice(qi * 512, (qi + 1) * 512)
        HQ = slice(256 + qi * 512, 256 + (qi + 1) * 512)
        hc_a, hc_b = hT_chunks[(256 + qi * 512) // 512], hT_chunks[(256 + qi * 512 + 511) // 512]
        oT_ = oTt[0]
        for hq in range(4):
            DMA(oT_[:, hq * 4:(hq + 1) * 4, :], o_scr[hq * 4:(hq + 1) * 4, :, Q].rearrange("h d t -> d h t"), [o_tok], [oT_])
        yT_ = yT[0]
        for m in range(8):
            pga, pgb, pA, pB = psum[m % 2], psum[2 + m % 2], psum[4], psum[5]
            M = slice(m * 128, (m + 1) * 128)
            mm(pga[:, :], [(wg[:, kc, m * 128:(m + 1) * 128], hT[:, kc, HQ]) for kc in range(8)], [wg, hc_a, hc_b], [pga])
            mm(pgb[:, :], [(wg[:, kc, D + m * 128:D + (m + 1) * 128], hT[:, kc, HQ]) for kc in range(8)], [wg, hc_a, hc_b], [pgb])
            mm(pA[:, :], [(woa_t[:, hh, M], oT_[:, hh, :]) for hh in range(8)], [woa_t, oT_], [pA])
            mm(pB[:, :], [(wob_t[:, hh, M], oT_[:, 8 + hh, :]) for hh in range(8)], [wob_t, oT_], [pB])
            sa, sb = sgb[(m % 2) * 2], sgb[(m % 2) * 2 + 1]
            act(sa.ap, pga[:, :], AF.Sigmoid, [pga], [sa])
            act(sb.ap, pgb[:, :], AF.Sigmoid, [pgb], [sb])
            tt(DVE, sa.ap, sa.ap, pA[:, :], ALU.mult, [sa, pA], [sa])
            tt(DVE, sb.ap, sb.ap, pB[:, :], ALU.mult, [sb, pB], [sb])
            tt(POOL, yT_[:, m, :], sa.ap, sb.ap, ALU.add, [sa, sb], [yT_])
        for sub in range(4):
            tok0 = qi * 512 + sub * 128
            xt_, x1_ = xt5[0], x1t[0]
            DMA(xt_.ap, x_d[tok0:tok0 + 128, :], [], [xt_])
            for n in range(2):
                po = psum[6 + n]
                mm(po[:, :], [(yT_[:, m, sub * 128:(sub + 1) * 128], wout_t[:, m, n * 512:(n + 1) * 512]) for m in range(8)], [yT_, wout_t], [po])
                tt(DVE, x1_[:, n * 512:(n + 1) * 512], po[:, :], g1bc[:, n * 512:(n + 1) * 512], ALU.mult, [po, g1bc], [x1_])
            tt(POOL, x1_.ap, x1_.ap, xt_.ap, ALU.add, [x1_, xt_], [x1_])
            x1_stores.append(DMA(x1_scr[tok0:tok0 + 128, :], x1_.ap, [x1_], []))
    x1_tok = Tile(None)
    bar3 = S.add("sp", None, [], [x1_tok])
    for op in x1_stores:
        bar3.deps.append(op); op.needs_inc = True
    if STAGE == 5:
        return finish(nc, S, es, block, x1_stores)


    S.barrier()
    if SPARSE:
        return sparse_moe(locals())

    T6 = Alloc(HT_OFF)
    rw_t = T6([128, 8, NEXP], BF16); rb_row = T6([1, NEXP], BF16); b1T = T6([128, 16, NEXP])
    b2rows = [T6([1, D], BF16) for _ in range(2)]
    gates = T6([128, 8, NEXP]); lgt = [T6([128, NEXP]) for _ in range(2)]; ext = [T6([128, NEXP]) for _ in range(2)]
    mk = [T6([128, NEXP]) for _ in range(2)]
    top8 = [T6([128, 8]) for _ in range(2)]; sm6 = [T6([128, 2]) for _ in range(2)]
    T6_MID = T6.off
    b1raw = T6([32, 2 * D])
    if not os.environ.get("MK_SKIP_SETUP"):
        DMA(rw_t.ap, rw_d.rearrange("(kc p) e -> p kc e", p=128), [], [rw_t], q="pool")
        DMA(rb_row.ap, rb_d.rearrange("(o e) -> o e", o=1), [], [rb_row], q="pool")
        DMA(b1raw.ap, eb1, [], [b1raw])
    for half in range(0 if os.environ.get("MK_SKIP_B1") else 2):
        pt = psum[half]

        def fnb(e, half=half, pt=pt):
            ins = None
            for f in range(8):
                fc = half * 8 + f
                ins = e.transpose(out=pt[:, f * 32:(f + 1) * 32], in_=b1raw[0:32, fc * 128:(fc + 1) * 128], identity=ident[0:32, 0:32])
            return ins
        PE(fnb, [b1raw, ident], [pt])
        DVE(lambda e, half=half, pt=pt: e.tensor_copy(out=b1T[:, half * 8:(half + 1) * 8, :], in_=pt[:, 0:256].rearrange("p (f e) -> p f e", e=NEXP)), [pt], [b1T])
    ts(DVE, b1T[:, 8:16, :], b1T[:, 8:16, :], 1.0, None, ALU.add, None, [b1T], [b1T])
    S.barrier()
    T6 = Alloc(T6_MID)
    h2T = T6([128, 8, 1024], BF16); h2f = T6([128, 8, 128]); acc = T6([128, 8, D])
    w1b = [T6([128, 8, 2 * D], BF16) for _ in range(2)]; w2t = T6([128, 8, D], BF16)
    actT = [T6([128, 8, 512], BF16) for _ in range(2)]
    ta = [T6([128, 512]) for _ in range(2)]; tsg = [T6([128, 512]) for _ in range(2)]; tl = [T6([128, 512]) for _ in range(2)]
    NB["xt"] = [T6([128, D]) for _ in range(2)]; NB["xn"] = [T6([128, D]) for _ in range(2)]
    h2chunks = [Tile(None) for _ in range(2)]
    final_stores = []
    nm_i = 0
    for g in range(int(os.environ.get("MK_NG", "4"))):
        for sub in range(8):
            tok0 = g * 1024 + sub * 128
            hch = h2chunks[sub // 4]
            if os.environ.get("MK_SKIP_NM"):
                continue
            norm_mod_T(nm_i, x1_scr[tok0:tok0 + 128, :], A2.ap, modT[:, 24:32, 0],
                       lambda j, sub=sub, hch=hch: (h2T[:, j, sub * 128:(sub + 1) * 128], hch),
                       (psum[(nm_i % 2) * 2], psum[(nm_i % 2) * 2 + 1]), src_deps=[x1_tok])
            nm_i += 1
            if os.environ.get("MK_SKIP_RT"):
                continue
            lg_ps = psum[4 + sub % 2]
            mm(lg_ps[:, 0:NEXP], [(h2T[:, kc, sub * 128:(sub + 1) * 128], rw_t[:, kc, :]) for kc in range(8)] + [(ones_b[0:1, :], rb_row[0:1, :])],
               [hch, rw_t, ones_b, rb_row], [lg_ps])
            lg, ex, mk_, t8, sm = lgt[sub % 2], ext[sub % 2], mk[sub % 2], top8[sub % 2], sm6[sub % 2]
            DVE(lambda e, lg=lg, lg_ps=lg_ps: e.tensor_copy(out=lg.ap, in_=lg_ps[:, 0:NEXP]), [lg_ps], [lg])
            DVE(lambda e, t8=t8, lg=lg: e.max(out=t8.ap, in_=lg.ap), [lg], [t8])
            ts(DVE, mk_.ap, lg.ap, t8[:, 3:4], None, ALU.is_ge, None, [lg, t8], [mk_])
            ts(DVE, sm[:, 0:1], t8[:, 0:1], -1.0, None, ALU.mult, None, [t8], [sm])
            act(ex.ap, lg.ap, AF.Exp, [lg, sm], [ex], bias=sm[:, 0:1])
            tt(DVE, ex.ap, ex.ap, mk_.ap, ALU.mult, [ex, mk_], [ex])
            DVE(lambda e, sm=sm, ex=ex: e.reduce_sum(out=sm[:, 1:2], in_=ex.ap, axis=AX.X), [ex], [sm])
            DVE(lambda e, sm=sm: e.reciprocal(out=sm[:, 1:2], in_=sm[:, 1:2]), [sm], [sm])
            ts(DVE, gates[:, sub, :], ex.ap, sm[:, 1:2], None, ALU.mult, None, [ex, sm], [gates])
        for ex_i in range(int(os.environ.get("MK_NE", "32"))):
            w1_ = w1b[ex_i % 2]
            b2r = b2rows[ex_i % 2]
            if not (os.environ.get("MK_NOW") and (g > 0 or ex_i > 1)):
                DMA(w1_.ap, ew1[ex_i].rearrange("(kc p) n -> p kc n", p=128), [], [w1_], q="pool")
                DMA(w2t.ap, ew2[ex_i].rearrange("(kc p) n -> p kc n", p=128), [], [w2t], q="pool")
            DMA(b2r.ap, eb2[ex_i:ex_i + 1, :], [], [b2r], q="pool")
            for rt in range(2):
                hch = h2chunks[rt]
                C = slice(rt * 512, (rt + 1) * 512)
                aT = actT[rt % 2]
                for fc in range(8):
                    pg, pl = psum[fc % 2], psum[2 + fc % 2]
                    a_, s_, l_ = ta[fc % 2], tsg[fc % 2], tl[fc % 2]
                    mm(pg[:, :], [(w1_[:, kc, fc * 128:(fc + 1) * 128], h2T[:, kc, C]) for kc in range(8)], [w1_, hch], [pg])
                    mm(pl[:, :], [(w1_[:, kc, D + fc * 128:D + (fc + 1) * 128], h2T[:, kc, C]) for kc in range(8)], [w1_, hch], [pl])
                    ts(DVE, a_.ap, pg[:, :], b1T[:, fc, ex_i:ex_i + 1], 7.0, ALU.add, ALU.min, [pg, b1T], [a_])
                    act(s_.ap, a_.ap, AF.Sigmoid, [a_], [s_], scale=1.702)
                    act(l_.ap, pl[:, :], AF.Identity, [pl, b1T], [l_], bias=b1T[:, 8 + fc, ex_i:ex_i + 1])
                    ts(DVE, l_.ap, l_.ap, -6.0, 8.0, ALU.max, ALU.min, [l_], [l_])
                    tt(DVE, s_.ap, a_.ap, s_.ap, ALU.mult, [a_, s_], [s_])
                    tt(POOL, aT[:, fc, :], s_.ap, l_.ap, ALU.mult, [s_, l_], [aT])
                for s4 in range(4):
                    sub = rt * 4 + s4
                    for n in range(2):
                        py = psum[4 + (s4 % 2) * 2 + n]
                        N = slice(n * 512, (n + 1) * 512)
                        mm(py[:, :], [(aT[:, fc, s4 * 128:(s4 + 1) * 128], w2t[:, fc, N]) for fc in range(8)] + [(ones_b[0:1, :], b2r[0:1, N])],
                           [aT, w2t, ones_b, b2r], [py])
                        if ex_i == 0:
                            ts(DVE, acc[:, sub, N], py[:, :], gates[:, sub, ex_i:ex_i + 1], None, ALU.mult, None, [py, gates], [acc])
                        else:
                            stt(DVE, acc[:, sub, N], py[:, :], gates[:, sub, ex_i:ex_i + 1], acc[:, sub, N], ALU.mult, ALU.add, [py, gates, acc], [acc])
        for sub in range(8):
            tok0 = g * 1024 + sub * 128
            xt_ = NB["xt"][sub % 2]
            DMA(xt_.ap, x1_scr[tok0:tok0 + 128, :], [x1_tok], [xt_])
            tt(DVE, acc[:, sub, :], acc[:, sub, :], g2bc.ap, ALU.mult, [acc, g2bc], [acc])
            tt(POOL, xt_.ap, xt_.ap, acc[:, sub, :], ALU.add, [xt_, acc], [xt_])
            final_stores.append(DMA(out_d[tok0:tok0 + 128, :], xt_.ap, [xt_], []))
    return finish(nc, S, es, block, final_stores)


def sparse_moe(L):
    g = dict(L)
    nc, S, es, block, Alloc, psum = g["nc"], g["S"], g["es"], g["block"], g["Alloc"], g["psum"]
    DMA, PE, ACT, DVE, POOL, mm, ts, tt, stt, act = (g[k] for k in ("DMA", "PE", "ACT", "DVE", "POOL", "mm", "ts", "tt", "stt", "act"))
    din, dscr, norm_mod_T, NB, finish = g["din"], g["dscr"], g["norm_mod_T"], g["NB"], finish_
    ident, ones_f, ones_b, modT, A2, g2bc, rs1 = g["ident"], g["ones_f"], g["ones_b"], g["modT"], g["A2"], g["g2bc"], g["rs1"]
    x1_scr, x1_tok, out_d, ab_scr = g["x1_scr"], g["x1_tok"], g["out_d"], g["ab_scr"]
    rw_d, rb_d, eb1, eb2, ew1, ew2 = g["rw_d"], g["rb_d"], g["eb1"], g["eb2"], g["ew1"], g["ew2"]
    IOA = bass.IndirectOffsetOnAxis

    NROWS = NBLK * BLK
    xs_d = g["xs_d"]
    ys_d = dscr("ys_scr", [NROWS, D], BF16)
    consts_d = g["consts_d"]
    ew1f = ew1.rearrange("e k n -> (e k) n"); ew2f = ew2.rearrange("e k n -> (e k) n")

    def IDMA(gather, dram, idx_ap, sb_ap, r, w):
        def fn(e):
            if gather:
                return e.indirect_dma_start(out=sb_ap, out_offset=None, in_=dram, in_offset=IOA(ap=idx_ap, axis=0))
            return e.indirect_dma_start(out=dram, out_offset=IOA(ap=idx_ap, axis=0), in_=sb_ap, in_offset=None)
        return S.add("pool", fn, r, w, dma=True)

    T6 = Alloc(g["HT_OFF"])
    cst = T6([128, NCONST])
    iota_r, pk = cst[:, 0:32], cst[:, 64:72]
    ustr_f = cst[:, 76:204]
    ustr_b = T6([128, 128], BF16); ident_b = T6([128, 128], BF16)
    rw_t = T6([128, 8, NEXP], BF16); rb_row = T6([1, NEXP], BF16)
    b1raw = T6([32, 2 * D]); b2_t = T6([32, D])
    A2bc = T6([128, D]); S2bc = T6([128, D])
    tot = T6([128, NEXP]); gates_all = T6([128, 32, NEXP]); gk_all = T6([128, 32, 4]); slots_u = T6([128, 32, 4], U32)
    ebv = T6([128, NBLK]); ohcol = T6([32, NBLK]); w_u = T6([128, 8, NBLK], U32)
    T6_MID = T6.off
    DMA(cst.ap, consts_d, [], [cst])
    DMA(rw_t.ap, rw_d.rearrange("(kc p) e -> p kc e", p=128), [], [rw_t], q="pool")
    DMA(rb_row.ap, rb_d.rearrange("(o e) -> o e", o=1), [], [rb_row], q="pool")
    DMA(b1raw.ap, eb1, [], [b1raw]); DMA(b2_t.ap, eb2, [], [b2_t])
    DMA(A2bc.ap, ab_scr[1], [], [A2bc]); DMA(S2bc.ap, ab_scr[0], [], [S2bc])
    ts(DVE, b1raw[:, D:2 * D], b1raw[:, D:2 * D], 1.0, None, ALU.add, None, [b1raw], [b1raw])
    DVE(lambda e: e.tensor_copy(out=ustr_b.ap, in_=ustr_f), [cst], [ustr_b])
    DVE(lambda e: e.tensor_copy(out=ident_b.ap, in_=ident.ap), [ident], [ident_b])
    POOL(lambda e: e.memset(tot.ap, 0.0), [], [tot])

    TA = Alloc(T6_MID)
    h2Tt = [TA([128, 8, 128], BF16) for _ in range(4)]
    h2tok = [TA([128, D], BF16) for _ in range(32)]
    tmpf = [TA([128, D]) for _ in range(3)]
    NB["xt"] = [TA([128, D]) for _ in range(4)]; NB["xn"] = [TA([128, D]) for _ in range(4)]
    lg_all = TA([128, 32, NEXP]); t8_all = TA([128, 32, 8]); posw = TA([128, 32, NEXP])
    ext = [TA([128, NEXP]) for _ in range(4)]; mkt = [TA([128, NEXP]) for _ in range(4)]
    mkb = [TA([128, NEXP], BF16) for _ in range(4)]; slotm = [TA([128, NEXP]) for _ in range(2)]
    sm6 = [TA([128, 2]) for _ in range(4)]
    oht = [TA([128, NEXP]) for _ in range(2)]; t32 = [TA([128, NEXP]) for _ in range(4)]
    slf = [TA([128, 4]) for _ in range(2)]
    zfill = g["zfill_ops"]
    nm_A, nm_B, nm_C = g["nm_A"], g["nm_B"], g["nm_C"]
    pactx = {}
    for step in range(32 + 2):
        if step < 32:
            pactx[step] = nm_A(step, x1_scr[step * 128:(step + 1) * 128, :], src_deps=[x1_tok])
        if 0 <= step - 1 < 32:
            nm_B(pactx[step - 1])
        if not (0 <= step - 2 < 32):
            continue
        i = step - 2
        tok0 = i * 128
        hT_ = h2Tt[i % 4]
        xt_ = pactx[i][0]
        nm_C(pactx[i], A2.ap, modT[:, 24:32, 0], lambda j, hT_=hT_: (hT_[:, j, :], hT_), (psum[(i % 2) * 2], psum[(i % 2) * 2 + 1]))
        rs_ = rs1[:, i % 4:i % 4 + 1]
        tf, hk = tmpf[i % 3], h2tok[i]
        stt(DVE, tf.ap, xt_.ap, rs_, A2bc.ap, ALU.mult, ALU.mult, [xt_, rs1, A2bc], [tf])
        tt(POOL, hk.ap, tf.ap, S2bc.ap, ALU.add, [tf, S2bc], [hk])
        lg_ps = psum[4 + i % 2]
        mm(lg_ps[:, 0:NEXP], [(hT_[:, kc, :], rw_t[:, kc, :]) for kc in range(8)] + [(ones_b[0:1, :], rb_row[0:1, :])],
           [hT_, rw_t, ones_b, rb_row], [lg_ps])
        ex, mk_, sm, mb = ext[i % 4], mkt[i % 4], sm6[i % 4], mkb[i % 4]
        lg_i, t8_i = lg_all[:, i, :], t8_all[:, i, :]
        DVE(lambda e, lg_i=lg_i, lg_ps=lg_ps: e.tensor_copy(out=lg_i, in_=lg_ps[:, 0:NEXP]), [lg_ps], [lg_all])
        DVE(lambda e, t8_i=t8_i, lg_i=lg_i: e.max(out=t8_i, in_=lg_i), [lg_all], [t8_all])
        ts(DVE, mk_.ap, lg_i, t8_all[:, i, 3:4], None, ALU.is_ge, None, [lg_all, t8_all], [mk_])
        ts(DVE, sm[:, 0:1], t8_all[:, i, 0:1], -1.0, None, ALU.mult, None, [t8_all], [sm])
        act(ex.ap, lg_i, AF.Exp, [lg_all, sm], [ex], bias=sm[:, 0:1])
        tt(DVE, ex.ap, ex.ap, mk_.ap, ALU.mult, [ex, mk_], [ex])
        DVE(lambda e, sm=sm, ex=ex: e.reduce_sum(out=sm[:, 1:2], in_=ex.ap, axis=AX.X), [ex], [sm])
        DVE(lambda e, sm=sm: e.reciprocal(out=sm[:, 1:2], in_=sm[:, 1:2]), [sm], [sm])
        ts(DVE, gates_all[:, i, :], ex.ap, sm[:, 1:2], None, ALU.mult, None, [ex, sm], [gates_all])
        DVE(lambda e, mb=mb, mk_=mk_: e.tensor_copy(out=mb.ap, in_=mk_.ap), [mk_], [mb])
        pos_ps, cs_ps = psum[6], psum[7]
        mm(pos_ps[:, 0:NEXP], [(ustr_b.ap, mb.ap)], [ustr_b, mb], [pos_ps])
        mm(cs_ps[:, 0:NEXP], [(ones_b.ap, mb.ap)], [ones_b, mb], [cs_ps])
        tt(DVE, posw[:, i, :], pos_ps[:, 0:NEXP], tot.ap, ALU.add, [pos_ps, tot], [posw])
        tt(DVE, tot.ap, tot.ap, cs_ps[:, 0:NEXP], ALU.add, [tot, cs_ps], [tot])

    tq, tm, pcv, pstart = (TA([128, NEXP]) for _ in range(4))
    pp = [TA([128, NEXP]) for _ in range(2)]
    cmpt = [TA([128, NEXP]) for _ in range(2)]
    wf = TA([128, 8, NBLK])
    ts(DVE, tq.ap, tot.ap, 0.0, None, ALU.is_gt, None, [tot], [tq])
    for m_ in range(1, ECAP // BLK):
        ts(DVE, tm.ap, tot.ap, float(m_ * BLK), None, ALU.is_gt, None, [tot], [tm])
        tt(DVE, tq.ap, tq.ap, tm.ap, ALU.add, [tq, tm], [tq])
    ts(DVE, pcv.ap, tq.ap, float(BLK), None, ALU.mult, None, [tq], [pcv])
    DVE(lambda e: e.tensor_copy(out=pp[0].ap, in_=pcv.ap), [pcv], [pp[0]])
    cur = 0
    for sh in (1, 2, 4, 8, 16):
        a_, b_ = pp[cur], pp[1 - cur]
        DVE(lambda e, a_=a_, b_=b_: e.tensor_copy(out=b_.ap, in_=a_.ap), [a_], [b_])
        tt(DVE, b_[:, sh:NEXP], a_[:, sh:NEXP], a_[:, 0:NEXP - sh], ALU.add, [a_], [b_])
        cur = 1 - cur
    pend = pp[cur]
    tt(DVE, pstart.ap, pend.ap, pcv.ap, ALU.subtract, [pend, pcv], [pstart])
    for b in range(NBLK):
        c_ = cmpt[b % 2]
        ts(DVE, c_.ap, pend.ap, float(b * BLK), None, ALU.is_le, None, [pend], [c_])
        DVE(lambda e, c_=c_, b=b: e.reduce_sum(out=ebv[:, b:b + 1], in_=c_.ap, axis=AX.X), [c_], [ebv])
    ts(DVE, ebv.ap, ebv.ap, float(NEXP - 1), None, ALU.min, None, [ebv], [ebv])
    for kc in range(8):
        ts(DVE, wf[:, kc, :], ebv.ap, float(D), pk[:, kc:kc + 1], ALU.mult, ALU.add, [ebv, cst], [wf])
        DVE(lambda e, kc=kc: e.tensor_copy(out=w_u[:, kc, :], in_=wf[:, kc, :]), [wf], [w_u])
    ts(DVE, ohcol.ap, ebv[0:32, :], cst[0:32, 72:73], None, ALU.is_equal, None, [ebv, cst], [ohcol])

    zb = S.add("pool", None, [], [])
    for op in zfill:
        zb.deps.append(op)
    scat = []
    for i in range(32):
        sl, sf, hk = slotm[i % 2], slf[i % 2], h2tok[i]
        tt(DVE, sl.ap, posw[:, i, :], pstart.ap, ALU.add, [posw, pstart], [sl])
        for k in range(4):
            oh, ta_, tb_ = oht[k % 2], t32[(k % 2) * 2], t32[(k % 2) * 2 + 1]
            ts(DVE, oh.ap, lg_all[:, i, :], t8_all[:, i, k:k + 1], None, ALU.is_equal, None, [lg_all, t8_all], [oh])
            tt(DVE, ta_.ap, oh.ap, sl.ap, ALU.mult, [oh, sl], [ta_])
            DVE(lambda e, sf=sf, ta_=ta_, k=k: e.reduce_sum(out=sf[:, k:k + 1], in_=ta_.ap, axis=AX.X), [ta_], [sf])
            tt(DVE, tb_.ap, oh.ap, gates_all[:, i, :], ALU.mult, [oh, gates_all], [tb_])
            DVE(lambda e, tb_=tb_, k=k, i=i: e.reduce_sum(out=gk_all[:, i, k:k + 1], in_=tb_.ap, axis=AX.X), [tb_], [gk_all])
        DVE(lambda e, sf=sf, i=i: e.tensor_copy(out=slots_u[:, i, :], in_=sf.ap), [sf], [slots_u])
        for k in range(4):
            scat.append(IDMA(False, xs_d, slots_u[:, i, k:k + 1], hk.ap, [hk, slots_u], []))
    xs_tok = Tile(None)
    bs = S.add("sp", None, [], [xs_tok])
    for op in scat:
        bs.deps.append(op)
    S.barrier()

    TB = Alloc(T6_MID)
    w1b = [TB([128, 8, 2 * D], BF16) for _ in range(2)]; w2b = [TB([128, 8, D], BF16) for _ in range(2)]
    xrows = [TB([128, 4, D], BF16) for _ in range(2)]; xTb = [TB([128, 8, BLK], BF16) for _ in range(2)]
    actT = [TB([128, 8, BLK], BF16) for _ in range(2)]
    ta = [TB([128, 512]) for _ in range(2)]; tsg = [TB([128, 512]) for _ in range(2)]; tl = [TB([128, 512]) for _ in range(2)]
    ysb = [TB([128, D], BF16) for _ in range(2)]
    b1c = [TB([128, 16]) for _ in range(2)]
    ysc = []
    def loads_xw1(b):
        xr, w1_ = xrows[b % 2], w1b[b % 2]
        DMA(xr.ap, xs_d[b * BLK:(b + 1) * BLK, :].rearrange("(j p) d -> p j d", p=128), [xs_tok], [xr])
        for kc in range(8):
            IDMA(True, ew1f, w_u[:, kc, b:b + 1], w1_[:, kc, :], [w_u], [w1_])

    def loads_w2(b):
        w2_ = w2b[b % 2]
        for kc in range(8):
            IDMA(True, ew2f, w_u[:, kc, b:b + 1], w2_[:, kc, :], [w_u], [w2_])

    def stage_pre(b):
        xr, xT_, bc = xrows[b % 2], xTb[b % 2], b1c[b % 2]
        pb = psum[4 + b % 2]

        def fb(e, pb=pb, b=b):
            ins = None
            for fc in range(16):
                ins = e.matmul(pb[:, fc:fc + 1], b1raw[0:32, fc * 128:(fc + 1) * 128], ohcol[0:32, b:b + 1], start=True, stop=True)
            return ins
        PE(fb, [b1raw, ohcol], [pb])
        DVE(lambda e, bc=bc, pb=pb: e.tensor_copy(out=bc.ap, in_=pb[:, 0:16]), [pb], [bc])
        for j in range(4):
            for half in range(2):
                pt = psum[6 + half]
                ptb = pt.ap.bitcast(BF16)

                def ft(e, ptb=ptb, xr=xr, j=j, half=half):
                    ins = None
                    for q in range(4):
                        kc = half * 4 + q
                        ins = e.transpose(out=ptb[:, q * 128:(q + 1) * 128], in_=xr[:, j, kc * 128:(kc + 1) * 128], identity=ident_b.ap)
                    return ins
                PE(ft, [xr, ident_b], [pt])
                src = ptb[:, 0:512].rearrange("p (q r) -> p q r", q=4)
                dst = xT_[:, half * 4:(half + 1) * 4, j * 128:(j + 1) * 128]
                if half == 0:
                    ACT(lambda e, dst=dst, src=src: e.copy(out=dst, in_=src), [pt], [xT_])
                else:
                    DVE(lambda e, dst=dst, src=src: e.tensor_copy(out=dst, in_=src), [pt], [xT_])

    def stage_w1(b):
        xT_, w1_, aT, bc = xTb[b % 2], w1b[b % 2], actT[b % 2], b1c[b % 2]
        for fc in range(8):
            pg, pl = psum[fc % 2], psum[2 + fc % 2]
            a_, s_, l_ = ta[fc % 2], tsg[fc % 2], tl[fc % 2]
            mm(pg[:, :], [(w1_[:, kc, fc * 128:(fc + 1) * 128], xT_[:, kc, :]) for kc in range(8)], [w1_, xT_], [pg])
            mm(pl[:, :], [(w1_[:, kc, D + fc * 128:D + (fc + 1) * 128], xT_[:, kc, :]) for kc in range(8)], [w1_, xT_], [pl])
            ts(DVE, a_.ap, pg[:, :], bc[:, fc:fc + 1], 7.0, ALU.add, ALU.min, [pg, bc], [a_])
            act(s_.ap, a_.ap, AF.Sigmoid, [a_], [s_], scale=1.702)
            act(l_.ap, pl[:, :], AF.Identity, [pl, bc], [l_], bias=bc[:, 8 + fc:9 + fc])
            ts(DVE, l_.ap, l_.ap, -6.0, 8.0, ALU.max, ALU.min, [l_], [l_])
            tt(DVE, s_.ap, a_.ap, s_.ap, ALU.mult, [a_, s_], [s_])
            tt(POOL, aT[:, fc, :], s_.ap, l_.ap, ALU.mult, [s_, l_], [aT])

    def stage_w2(b):
        w2_, aT = w2b[b % 2], actT[b % 2]
        for j in range(4):
            yb = ysb[j % 2]
            for n in range(2):
                py = psum[4 + (j % 2) * 2 + n]
                N = slice(n * 512, (n + 1) * 512)
                mm(py[:, :], [(aT[:, fc, j * 128:(j + 1) * 128], w2_[:, fc, N]) for fc in range(8)], [aT, w2_], [py])
                if n == 0:
                    ACT(lambda e, yb=yb, py=py, N=N: e.copy(out=yb[:, N], in_=py[:, :]), [py], [yb])
                else:
                    DVE(lambda e, yb=yb, py=py, N=N: e.tensor_copy(out=yb[:, N], in_=py[:, :]), [py], [yb])
            ysc.append(DMA(ys_d[b * BLK + j * 128:b * BLK + (j + 1) * 128, :], yb.ap, [yb], []))

    loads_xw1(0); loads_w2(0)
    stage_pre(0)
    for b in range(NBLK):
        if b + 1 < NBLK:
            loads_xw1(b + 1)
        stage_w1(b)
        if b >= 1:
            stage_w2(b - 1)
        if b + 1 < NBLK:
            loads_w2(b + 1)
            stage_pre(b + 1)
    stage_w2(NBLK - 1)
    ys_tok = Tile(None)
    bs2 = S.add("pool", None, [], [ys_tok])
    for op in ysc:
        bs2.deps.append(op)
    S.barrier()

    TC = Alloc(T6_MID)
    yk = [TC([128, D], BF16) for _ in range(8)]
    accc = [TC([128, D]) for _ in range(2)]
    gT = [TC([32, 128]) for _ in range(2)]
    xt6 = [TC([128, D]) for _ in range(2)]
    final_stores = []
    for i in range(32):
        tok0 = i * 128
        ac, gt, xt_ = accc[i % 2], gT[i % 2], xt6[i % 2]
        for k in range(4):
            IDMA(True, ys_d, slots_u[:, i, k:k + 1], yk[(i % 2) * 4 + k].ap, [ys_tok, slots_u], [yk[(i % 2) * 4 + k]])
        DMA(xt_.ap, x1_scr[tok0:tok0 + 128, :], [x1_tok], [xt_])
        pgt = psum[i % 2]
        PE(lambda e, pgt=pgt, i=i: e.transpose(out=pgt[0:32, 0:128], in_=gates_all[:, i, :], identity=ident.ap), [gates_all, ident], [pgt])
        DVE(lambda e, gt=gt, pgt=pgt: e.tensor_copy(out=gt.ap, in_=pgt[0:32, 0:128]), [pgt], [gt])
        for n in range(2):
            pbias = psum[2 + (i % 2) * 2 + n]
            N = slice(n * 512, (n + 1) * 512)
            mm(pbias[:, :], [(gt[0:32, :], b2_t[0:32, N])], [gt, b2_t], [pbias])
            stt(DVE, ac[:, N], yk[(i % 2) * 4][:, N], gk_all[:, i, 0:1], pbias[:, :], ALU.mult, ALU.add, [yk[(i % 2) * 4], gk_all, pbias], [ac])
        for k in range(1, 4):
            y_ = yk[(i % 2) * 4 + k]
            stt(DVE, ac.ap, y_.ap, gk_all[:, i, k:k + 1], ac.ap, ALU.mult, ALU.add, [y_, gk_all, ac], [ac])
        tt(DVE, ac.ap, ac.ap, g2bc.ap, ALU.mult, [ac, g2bc], [ac])
        tt(DVE, xt_.ap, xt_.ap, ac.ap, ALU.add, [xt_, ac], [xt_])
        final_stores.append(DMA(out_d[tok0:tok0 + 128, :], xt_.ap, [xt_], []))
    return finish(nc, S, es, block, final_stores)


def finish_(nc, S, es, block, final_deps):
    return finish(nc, S, es, block, final_deps)


def finish(nc, S, es, block, final_deps):
    fin = S.add("sp", None, [], [])
    for d in final_deps:
        fin.deps.append(d)
        d.needs_inc = True
    S.emit(nc, es, block)
    es.close()
    return nc, S


_CONSTS = None


def rope_tables():
    theta = 10000.0
    n_rows = SEQ // 64
    row = np.repeat(np.arange(n_rows), 64).astype(np.float32)
    col = np.tile(np.arange(64), n_rows).astype(np.float32)

    def tab(half):
        nf = half // 2
        inv = (theta ** (-(np.arange(0, half, 2, dtype=np.float32)) / half)).astype(np.float32)
        cos = np.ones((2 * half, NT), np.float32)
        sin = np.zeros((2 * half, NT), np.float32)
        for blk, pos in enumerate((row, col)):
            ang = (pos[None, :] * inv[:, None]).astype(np.float32)
            c, s = np.cos(ang).astype(np.float32), np.sin(ang).astype(np.float32)
            base = blk * half
            cos[base:base + nf, NCTX:] = c
            cos[base + nf:base + half, NCTX:] = c
            sin[base:base + nf, NCTX:] = s
            sin[base + nf:base + half, NCTX:] = s
        return cos, sin
    cA, sA = tab(64)
    cB, sB = tab(32)
    return cA, sA, cB, sB


def kernel(**inputs):
    cA, sA, cB, sB = rope_tables()
    nc, S = build_program()
    g = lambda k: np.ascontiguousarray(np.asarray(inputs[k], dtype=np.float32)[0])
    shared = {
        "ada_w": g("ada_w"), "ada_b": g("ada_b"), "norm_mix": g("norm_mix"), "norm_ffn": g("norm_ffn"),
        "w_in": g("w_in"), "gqa_q_norm": g("gqa_q_norm"), "gqa_k_norm": g("gqa_k_norm"),
        "mla_q_a_norm": g("mla_q_a_norm"), "mla_kv_a_norm": g("mla_kv_a_norm"),
        "mla_w_qb": g("mla_w_qb"), "mla_w_kvb": g("mla_w_kvb"), "mla_q_norm": g("mla_q_norm"),
        "mla_k_norm": g("mla_k_norm"), "w_o_gqa": g("w_o_gqa"), "w_o_mla": g("w_o_mla"), "w_out": g("w_out"),
        "router_w": g("router_w"), "router_b": g("router_b"), "expert_w1": g("expert_w1"),
        "expert_b1": g("expert_b1"), "expert_w2": g("expert_w2"), "expert_b2": g("expert_b2"),
        "ident": np.eye(128, dtype=np.float32), "sel0": np.stack([np.ones(128, np.float32), np.zeros(128, np.float32)]), "cosA": cA, "sinA": sA, "cosB": cB, "sinB": sB,
    }
    if SPARSE:
        cst = np.zeros((128, NCONST), np.float32)
        pidx = np.arange(128, dtype=np.float32)
        cst[:, 0:32] = np.arange(32, dtype=np.float32)[None, :]
        cst[:, 32:64] = (np.arange(32, dtype=np.float32) * ECAP)[None, :]
        cst[:, 64:72] = pidx[:, None] + 128.0 * np.arange(8, dtype=np.float32)[None, :]
        cst[:, 72:76] = pidx[:, None] + 128.0 * np.arange(4, dtype=np.float32)[None, :]
        cst[:, 76:204] = (pidx[:, None] < pidx[None, :]).astype(np.float32)
        cst[:, 204:204 + NBLK] = (np.arange(NBLK, dtype=np.float32) * BLK)[None, :]
        shared["consts"] = cst
    x = np.asarray(inputs["x"], np.float32); c = np.asarray(inputs["c"], np.float32)
    ctx = np.asarray(inputs["ctx"], np.float32); c_ctx = np.asarray(inputs["c_ctx"], np.float32)
    ncores = int(os.environ.get("MK_CORES", "8"))
    in_maps = []
    for b in range(ncores):
        m = dict(shared)
        m["x"] = np.ascontiguousarray(x[b]); m["ctx"] = np.ascontiguousarray(ctx[b])
        m["cc"] = np.ascontiguousarray(np.stack([c[b], c_ctx], 0))
        in_maps.append(m)
    res = run_bass_kernel_spmd(nc, in_maps, core_ids=list(range(ncores)))
    if STAGE < 99:
        return res.results
    if ncores < 8:
        return [r["out"] for r in res.results]
    return np.stack([r["out"] for r in res.results], 0).astype(np.float32)
```

```python
import os
import numpy as np
from contextlib import ExitStack
import concourse.bass as bass
import concourse.mybir as mybir
from concourse.bass_utils import run_bass_kernel_spmd

F32 = mybir.dt.float32
BF16 = mybir.dt.bfloat16
ALU = mybir.AluOpType
AF = mybir.ActivationFunctionType
AX = mybir.AxisListType
U32 = mybir.dt.uint32
SPARSE = os.environ.get("MK_SPARSE", "1") == "1"
BLK = 512
NBLK = 16384 // BLK + 32
ECAP = 4096
ZROW = 32 * ECAP
NCONST = 76 + 128 + NBLK

D = 1024
SEQ = 4096
NCTX = 256
NT = SEQ + NCTX
EPS = 1e-6
NEXP = 32
STAGE = int(os.environ.get("MK_STAGE", "99"))


class Tok:
    __slots__ = ("w", "wd", "rs", "rd")

    def __init__(self):
        self.w = None
        self.wd = []
        self.rs = {}
        self.rd = []


class Tile:
    def __init__(self, ap):
        self.ap = ap
        self.tok = Tok()

    def __getitem__(self, k):
        return self.ap[k]


class Op:
    __slots__ = ("eng", "fn", "deps", "dma", "needs_inc", "seg", "val", "sem", "barred", "seq")


ENGS = ("pe", "act", "dve", "pool", "sp")
SEG = 4000
NDMASEM = 40


class Sched:
    def __init__(self):
        self.ops = {e: [] for e in ENGS}
        self.nseq = 0

    def add(self, eng, fn, reads=(), writes=(), dma=False):
        op = Op()
        op.eng, op.fn, op.dma, op.deps, op.needs_inc = eng, fn, dma, [], False
        op.seg = op.val = op.sem = None
        seen = set()

        def dep(d, raw):
            if d is None or id(d) in seen:
                return
            if d.fn is None:
                if d.eng != eng:
                    for dd in d.deps:
                        dep(dd, raw)
                return
            if not d.dma and not dma and d.eng == eng:
                if eng == "pe" or raw is None:
                    return
            seen.add(id(d))
            op.deps.append(d)
            d.needs_inc = True

        for t in reads:
            dep(t.tok.w, True)
            for d in t.tok.wd:
                dep(d, True)
        for t in writes:
            k = t.tok
            if dma:
                if k.w is not None:
                    dep(k.w, None)
            else:
                dep(k.w, None)
                for d in k.wd:
                    dep(d, None)
            for r in k.rs.values():
                dep(r, False)
            for r in k.rd:
                dep(r, False)
        for t in reads:
            if dma:
                t.tok.rd.append(op)
            else:
                t.tok.rs[eng] = op
        for t in writes:
            k = t.tok
            had_readers = bool(k.rs) or bool(k.rd)
            if dma:
                if had_readers or k.w is not None:
                    k.w, k.wd = None, [op]
                else:
                    k.wd.append(op)
            else:
                k.w, k.wd = op, []
            k.rs, k.rd = {}, []
        op.seq = self.nseq
        self.nseq += 1
        self.ops[eng].append(op)
        return op

    def barrier(self):
        lasts = []
        for e in ENGS:
            for op in reversed(self.ops[e]):
                if not op.dma and op.fn is not None:
                    lasts.append(op)
                    break
        dmas = [op for e in ENGS for op in self.ops[e] if op.dma and not getattr(op, "barred", False)]
        for op in dmas:
            op.barred = True
        for e in ENGS:
            b = self.add(e, None, [], [])
            for d in lasts:
                if d.eng != e:
                    b.deps.append(d); d.needs_inc = True
            for d in dmas:
                b.deps.append(d)

    def emit(self, nc, es, block):
        dsems = [es.enter_context(nc.semaphore("dq%d" % i)) for i in range(NDMASEM)]
        dcount = [0] * NDMASEM
        csems = {}
        pools = {"sp": list(range(0, 24)), "pool": list(range(24, 36)), "act": list(range(36, 40))}
        for e in ENGS:
            cnt = 0
            di = 0
            for op in self.ops[e]:
                if op.dma:
                    pl = pools[e]
                    op.sem = pl[di % len(pl)]
                    dcount[op.sem] += 16
                    op.val = dcount[op.sem]
                    di += 1
                elif op.needs_inc:
                    cnt += 1
                    op.seg, op.val = (cnt - 1) // SEG, (cnt - 1) % SEG + 1
                    if (e, op.seg) not in csems:
                        csems[(e, op.seg)] = es.enter_context(nc.semaphore("c_%s_%d" % (e, op.seg)))
        self.n_inst = {e: len(self.ops[e]) for e in ENGS}

        def run(e, eng):
            waited = {}
            nw = 0
            for op in self.ops[e]:
                for d in op.deps:
                    if d.dma:
                        key, val, sem = ("d", d.sem), d.val, dsems[d.sem]
                        if waited.get(key, 0) >= val:
                            continue
                        waited[key] = val
                    else:
                        key, val, sem = ("c", d.eng), (d.seg, d.val), csems[(d.eng, d.seg)]
                        if waited.get(key, (-1, 0)) >= val:
                            continue
                        waited[key] = val
                        val = d.val
                    eng.wait_ge(sem, val)
                    nw += 1
                if op.dma and op.val > 16:
                    key = ("d", op.sem)
                    if waited.get(key, 0) < op.val - 16:
                        eng.wait_ge(dsems[op.sem], op.val - 16)
                        waited[key] = op.val - 16
                if op.fn is None:
                    continue
                ins = op.fn(eng)
                if op.dma:
                    ins.then_inc(dsems[op.sem], 16)
                elif op.needs_inc:
                    ins.then_inc(csems[(e, op.seg)], 1)
            self.n_inst[e] = (len(self.ops[e]), nw)

        @block.tensor
        def _(eng):
            run("pe", eng)

        @block.scalar
        def _(eng):
            run("act", eng)

        @block.vector
        def _(eng):
            run("dve", eng)

        @block.gpsimd
        def _(eng):
            run("pool", eng)

        @block.sync
        def _(eng):
            run("sp", eng)


def build_program():
    nc = bass.Bass("TRN2", target_bir_lowering=False)
    S = Sched()
    es = ExitStack()

    def din(name, shape, dt=F32):
        return nc.dram_tensor(name, list(shape), dt, kind="ExternalInput").ap()

    def dscr(name, shape, dt=BF16):
        if STAGE in (2, 3):
            return nc.dram_tensor(name, list(shape), dt, kind="ExternalOutput").ap()
        return nc.dram_tensor(name, list(shape), dt).ap()

    x_d = din("x", [SEQ, D]); ctx_d = din("ctx", [NCTX, D]); cc_d = din("cc", [2, D])
    ada_w = din("ada_w", [D, 6 * D]); ada_b = din("ada_b", [6 * D])
    norm_mix = din("norm_mix", [D]); norm_ffn = din("norm_ffn", [D])
    w_in = din("w_in", [D, 4032])
    gq_d = din("gqa_q_norm", [128]); gk_d = din("gqa_k_norm", [128])
    gqa_d = din("mla_q_a_norm", [256]); gkva_d = din("mla_kv_a_norm", [128])
    w_qb = din("mla_w_qb", [256, 1536]); w_kvb = din("mla_w_kvb", [128, 2048])
    gmq_d = din("mla_q_norm", [192]); gmk_d = din("mla_k_norm", [192])
    w_oa = din("w_o_gqa", [D, D]); w_ob = din("w_o_mla", [D, D]); w_out = din("w_out", [D, D])
    rw_d = din("router_w", [D, NEXP]); rb_d = din("router_b", [NEXP])
    ew1 = din("expert_w1", [NEXP, D, 2 * D]); eb1 = din("expert_b1", [NEXP, 2 * D])
    ew2 = din("expert_w2", [NEXP, D, D]); eb2 = din("expert_b2", [NEXP, D])
    ident_d = din("ident", [128, 128]); sel0_d = din("sel0", [2, 128])
    cosA_d = din("cosA", [128, NT]); sinA_d = din("sinA", [128, NT])
    cosB_d = din("cosB", [64, NT]); sinB_d = din("sinB", [64, NT])
    consts_d = din("consts", [128, NCONST]) if SPARSE else None
    out_d = nc.dram_tensor("out", [SEQ, D], F32, kind="ExternalOutput").ap()

    qa_scr = dscr("qa_scr", [8, 128, SEQ]); ka_scr = dscr("ka_scr", [2, 128, NT])
    va_scr = dscr("va_scr", [2, 128, 34, 128])
    qb_scr = dscr("qb_scr", [8, 128, SEQ]); qbr_scr = dscr("qbr_scr", [8, 64, SEQ])
    kb_scr = dscr("kb_scr", [8, 128, NT]); kbr_scr = dscr("kbr_scr", [8, 64, NT])
    vb_scr = dscr("vb_scr", [8, 128, 34, 128])
    o_scr = (nc.dram_tensor("o_scr", [16, 128, SEQ], BF16, kind="ExternalOutput").ap() if STAGE in (4, 5) else dscr("o_scr", [16, 128, SEQ]))
    x1_scr = out_d if STAGE == 5 else dscr("x1_scr", [SEQ, D], F32)
    dbg_d = None
    if STAGE < 99:
        dbg_d = nc.dram_tensor("dbg", [128, 8 * NT], F32, kind="ExternalOutput").ap()

    ARENA_F = 52400
    arena = es.enter_context(nc.sbuf_tensor("arena", [128, ARENA_F], F32))
    psum = [Tile(es.enter_context(nc.psum_tensor("ps%d" % i, [128, 512], F32))[:, :]) for i in range(8)]
    block = es.enter_context(nc.Block())

    class Alloc:
        def __init__(self, base=0):
            self.off = base

        def __call__(self, shape, dt=F32, parts=128):
            n = int(np.prod(shape[1:]))
            nf = n if dt in (F32, U32) else (n + 1) // 2
            nf = (nf + 7) // 8 * 8
            a = arena[:, self.off:self.off + nf]
            self.off += nf
            assert self.off <= ARENA_F, "SBUF arena overflow %d" % self.off
            if dt != F32:
                a = a.bitcast(dt)
            a = a[:, 0:n]
            a = a[0:shape[0]]
            if len(shape) == 3:
                a = a.rearrange("p (a b) -> p a b", a=shape[1])
            elif len(shape) == 4:
                a = a.rearrange("p (a b c) -> p a b c", a=shape[1], b=shape[2])
            return Tile(a)

    def PE(fn, r, w): S.add("pe", fn, r, w)
    def ACT(fn, r, w): S.add("act", fn, r, w)
    def DVE(fn, r, w): S.add("dve", fn, r, w)
    def POOL(fn, r, w): S.add("pool", fn, r, w)
    def DMA(out, in_, r, w, q="sp", nonc=False):
        def fn(e):
            if nonc:
                with nc.allow_non_contiguous_dma(reason="tiny vector load"):
                    return e.dma_start(out=out, in_=in_)
            return e.dma_start(out=out, in_=in_)
        return S.add(q, fn, r, w, dma=True)

    def mm(out, pairs, r, w):
        n = len(pairs)

        def fn(e):
            ins = None
            for i, (l, rh) in enumerate(pairs):
                ins = e.matmul(out, l, rh, start=(i == 0), stop=(i == n - 1))
            return ins
        PE(fn, r, w)

    def ts(eng, out, in0, s1, s2, op0, op1, r, w):
        if s2 is None:
            eng(lambda e: e.tensor_scalar(out=out, in0=in0, scalar1=s1, scalar2=None, op0=op0), r, w)
        else:
            eng(lambda e: e.tensor_scalar(out=out, in0=in0, scalar1=s1, scalar2=s2, op0=op0, op1=op1), r, w)

    def tt(eng, out, in0, in1, op, r, w):
        eng(lambda e: e.tensor_tensor(out=out, in0=in0, in1=in1, op=op), r, w)

    def stt(eng, out, in0, sc, in1, op0, op1, r, w):
        eng(lambda e: e.scalar_tensor_tensor(out=out, in0=in0, scalar=sc, in1=in1, op0=op0, op1=op1), r, w)

    def act(out, in_, func, r, w, bias=None, scale=None, accum=None):
        kw = {}
        if bias is not None: kw["bias"] = bias
        if scale is not None: kw["scale"] = scale
        if accum is not None: kw["accum_out"] = accum
        ACT(lambda e: e.activation(out=out, in_=in_, func=func, **kw), r, w)

    def rstd_from(out_t, out_ap, ss_ap, ss_deps, n):
        act(out_ap, ss_ap, AF.Ln, ss_deps + [eps_c], [out_t], bias=eps_c[0:out_ap.shape[0], 0:1], scale=1.0 / n)
        act(out_ap, out_ap, AF.Exp, [out_t], [out_t], scale=-0.5)

    P = Alloc(0)
    ident = P([128, 128]); ones_f = P([128, 128]); ones_b = P([128, 128], BF16)
    modT = P([128, 48, 2]); adabT = P([128, 48]); nmT = P([128, 8]); nfT = P([128, 8])
    A1 = P([128, 8, 2]); A2 = P([128, 8])
    g1bc = P([128, D]); g2bc = P([128, D])
    gqT = P([128, 2]); gkT = P([128, 2])
    gqaT = P([128, 2]); gkvaT = P([128, 1])
    gmqT = P([128, 3]); gmkT = P([128, 3])
    ss1 = P([128, 4]); rs1 = P([128, 4])
    zero_c = P([128, 1]); eps_c = P([128, 1])
    HT_OFF = P.off
    hT = P([128, 8, NT], BF16)
    PERSIST_END = P.off

    DMA(ident.ap, ident_d, [], [ident])
    POOL(lambda e: e.memset(ones_f.ap, 1.0), [], [ones_f])
    POOL(lambda e: e.memset(ones_b.ap, 1.0), [], [ones_b])
    POOL(lambda e: e.memset(zero_c.ap, 0.0), [], [zero_c])
    POOL(lambda e: e.memset(eps_c.ap, EPS), [], [eps_c])
    DMA(nmT.ap, norm_mix.rearrange("(j p) -> p j", p=128), [], [nmT], nonc=True)
    DMA(nfT.ap, norm_ffn.rearrange("(j p) -> p j", p=128), [], [nfT], nonc=True)

    def colvec(dst_t, col, src_d, lo, hi, p0=0):
        DMA(dst_t[p0:p0 + (hi - lo), col:col + 1], src_d[lo:hi].rearrange("(p o) -> p o", o=1), [], [dst_t], nonc=True)

    for (t_, d_) in ((gqT, gq_d), (gkT, gk_d)):
        colvec(t_, 0, d_, 0, 128)
        colvec(t_, 1, d_, 32, 64, 0); colvec(t_, 1, d_, 0, 32, 32)
        colvec(t_, 1, d_, 96, 128, 64); colvec(t_, 1, d_, 64, 96, 96)
    colvec(gqaT, 0, gqa_d, 0, 128); colvec(gqaT, 1, gqa_d, 128, 256)
    colvec(gkvaT, 0, gkva_d, 0, 128)
    for (t_, d_) in ((gmqT, gmq_d), (gmkT, gmk_d)):
        colvec(t_, 0, d_, 0, 128)
        colvec(t_, 1, d_, 128, 192, 0)
        colvec(t_, 2, d_, 144, 160, 0); colvec(t_, 2, d_, 128, 144, 16)
        colvec(t_, 2, d_, 176, 192, 32); colvec(t_, 2, d_, 160, 176, 48)

    T0 = Alloc(PERSIST_END)
    ccT = T0([128, 8, 2]); scT = T0([128, 8, 2])
    wblk = [T0([128, 8, 1024]) for _ in range(2)]
    modrow = T0([2, 6 * D]); adab2 = T0([2, 6 * D]); sel0 = T0([2, 128])
    abtmp = [T0([128, D]) for _ in range(2)]; nfbc = T0([128, D])
    ab_scr = dscr("ab_scr", [2, 128, D], F32)
    for r in range(2):
        DMA(ccT[:, :, r], cc_d[r].rearrange("(j p) -> p j", p=128), [], [ccT], nonc=True)
        DMA(adab2[r:r + 1, :], ada_b.rearrange("(o n) -> o n", o=1), [], [adab2])
    DMA(sel0.ap, sel0_d, [], [sel0])
    act(scT.ap, ccT.ap, AF.Silu, [ccT], [scT])
    ada_v = ada_w.rearrange("(kc p) n -> p kc n", p=128)
    for blk in range(6):
        wb = wblk[blk % 2]
        DMA(wb.ap, ada_v[:, :, blk * 1024:(blk + 1) * 1024], [], [wb])
        for half in range(2):
            pr = psum[(blk * 2 + half) % 4]
            cols = slice(blk * 1024 + half * 512, blk * 1024 + (half + 1) * 512)
            mm(pr[0:2, :], [(scT[:, kc, :], wb[:, kc, half * 512:(half + 1) * 512]) for kc in range(8)], [scT, wb], [pr])
            tt(DVE, modrow[:, cols], pr[0:2, :], adab2[:, cols], ALU.add, [pr, adab2], [modrow])
    pst = psum[4]

    def ftm(e):
        ins = None
        for j in range(48):
            ins = e.transpose(out=pst[:, 2 * j:2 * j + 2], in_=modrow[0:2, j * 128:(j + 1) * 128], identity=ident[0:2, 0:2])
        return ins
    PE(ftm, [modrow, ident], [pst])
    DVE(lambda e: e.tensor_copy(out=modT.ap, in_=pst[:, 0:96].rearrange("p (j r) -> p j r", r=2)), [pst], [modT])
    for blk in ((2, 5, 3, 4) if SPARSE else (2, 5)):
        gdst = {2: g1bc, 5: g2bc, 3: abtmp[0], 4: abtmp[1]}[blk]
        for half in range(2):
            pg = psum[5 + half]
            cols = slice(blk * 1024 + half * 512, blk * 1024 + (half + 1) * 512)
            mm(pg[:, :], [(sel0.ap, modrow[0:2, cols])], [sel0, modrow], [pg])
            if half == 0:
                ACT(lambda e, gdst=gdst, pg=pg: e.copy(out=gdst[:, 0:512], in_=pg[:, :]), [pg], [gdst])
            else:
                DVE(lambda e, gdst=gdst, pg=pg: e.tensor_copy(out=gdst[:, 512:1024], in_=pg[:, :]), [pg], [gdst])
        if blk == 4:
            DMA(nfbc.ap, norm_ffn.partition_broadcast(128), [], [nfbc])
            stt(DVE, gdst.ap, gdst.ap, 1.0, nfbc.ap, ALU.add, ALU.mult, [gdst, nfbc], [gdst])
        if blk in (3, 4):
            DMA(ab_scr[blk - 3], gdst.ap, [gdst], [])
    for r in range(2):
        stt(DVE, A1[:, :, r], modT[:, 8:16, r], 1.0, nmT.ap, ALU.add, ALU.mult, [modT, nmT], [A1])
    stt(DVE, A2.ap, modT[:, 32:40, 0], 1.0, nfT.ap, ALU.add, ALU.mult, [modT, nfT], [A2])

    S.barrier()
    T1 = Alloc(PERSIST_END)
    xt = [T1([128, D]) for _ in range(4)]
    xn = [T1([128, D]) for _ in range(4)]
    NB = {"xt": xt, "xn": xn}

    def nm_A(i, src_ap, src_deps=()):
        xt_, xn_ = NB["xt"][i % len(NB["xt"])], NB["xn"][i % len(NB["xn"])]
        ss_, rs_ = ss1[:, i % 4:i % 4 + 1], rs1[:, i % 4:i % 4 + 1]
        DMA(xt_.ap, src_ap, list(src_deps), [xt_])
        act(xn_.ap, xt_.ap, AF.Square, [xt_], [xn_, ss1], accum=ss_)
        return (xt_, xn_, ss_, rs_)

    def nm_B(cx):
        xt_, xn_, ss_, rs_ = cx
        rstd_from(rs1, rs_, ss_, [ss1], D)
        ACT(lambda e: e.activation(out=xn_.ap, in_=xt_.ap, func=AF.Copy, scale=rs_), [xt_, rs1], [xn_])

    def nm_C(cx, A_ap, S_ap, dst_fn, ps_pair):
        xt_, xn_, ss_, rs_ = cx
        for half in range(2):
            pst = ps_pair[half]

            def fn(e, half=half, pst=pst):
                ins = None
                for jj in range(4):
                    j = half * 4 + jj
                    ins = e.transpose(out=pst[:, jj * 128:(jj + 1) * 128], in_=xn_[:, j * 128:(j + 1) * 128], identity=ident.ap)
                return ins
            PE(fn, [xn_, ident], [pst])
            for jj in range(4):
                j = half * 4 + jj
                dst_ap, dst_t = dst_fn(j)
                if jj % 2 == 0:
                    ts(DVE, dst_ap, pst[:, jj * 128:(jj + 1) * 128], A_ap[:, j:j + 1], S_ap[:, j:j + 1], ALU.mult, ALU.add,
                       [pst, A1, A2, modT], [dst_t])
                else:
                    act(dst_ap, pst[:, jj * 128:(jj + 1) * 128], AF.Identity, [pst, A1, A2, modT], [dst_t],
                        bias=S_ap[:, j:j + 1], scale=A_ap[:, j:j + 1])

    def norm_mod_T(i, src_ap, A_ap, S_ap, dst_fn, ps_pair, extra_f32=None, src_deps=()):
        cx = nm_A(i, src_ap, src_deps)
        nm_B(cx)
        nm_C(cx, A_ap, S_ap, dst_fn, ps_pair)
        return cx[0]

    hT_chunks = [Tile(hT.ap) for _ in range(9)]

    p1ctx = {}
    for step in range(34 + 2):
        if step < 34:
            i = step
            src = ctx_d[i * 128:(i + 1) * 128, :] if i < 2 else x_d[(i - 2) * 128:(i - 1) * 128, :]
            p1ctx[i] = nm_A(i, src)
        if 0 <= step - 1 < 34:
            nm_B(p1ctx[step - 1])
        if 0 <= step - 2 < 34:
            i = step - 2
            r = 1 if i < 2 else 0
            c0 = i * 128
            chunk = hT_chunks[c0 // 512]
            nm_C(p1ctx[i], A1[:, :, r], modT[:, 0:8, r], lambda j, c0=c0, chunk=chunk: (hT[:, j, c0:c0 + 128], chunk),
                 (psum[(i % 4) * 2], psum[(i % 4) * 2 + 1]))

    final_deps = []

    def dbg_dump(src_tile, ncols, col0=0):
        pass

    if STAGE == 1:
        TD = Alloc(T1.off)
        for j in range(8):
            for c in range(9):
                w = min(512, NT - c * 512)
                tmp = TD([128, 512])
                ts(DVE, tmp[:, 0:w], hT[:, j, c * 512:c * 512 + w], 1.0, None, ALU.mult, None, [hT_chunks[c]], [tmp])
                final_deps.append(DMA(dbg_d[:, j * NT + c * 512:j * NT + c * 512 + w], tmp[:, 0:w], [tmp], []))
                if TD.off > ARENA_F - 600:
                    TD = Alloc(T1.off)
        return finish(nc, S, es, block, final_deps)


    def all_dmas():
        return [op for e_ in ENGS for op in S.ops[e_] if op.dma]

    w_in_v = w_in.rearrange("(kc p) n -> p kc n", p=128)
    chunks = [(c, c * 512, min(512, NT - c * 512)) for c in range(9)]

    def qstore(dst_h, src_t, c, c0, w, parts=128):
        if c == 0:
            return DMA(dst_h[:, 0:256], src_t[0:parts, 256:512], [src_t], [])
        return DMA(dst_h[:, c0 - 256:c0 - 256 + w], src_t[0:parts, 0:w], [src_t], [])

    S.barrier()
    T2 = Alloc(PERSIST_END)
    wqa = T2([128, 8, 1536], BF16); wrot = T2([128, 8, 1280], BF16)
    cosT = [T2([128, 512]) for _ in range(2)]; sinT = [T2([128, 512]) for _ in range(2)]
    sqb = [T2([128, 512], BF16) for _ in range(2)]; rsd = [T2([128, 512]) for _ in range(2)]
    t1b = [T2([128, 512]) for _ in range(2)]; t2b = [T2([128, 512]) for _ in range(2)]
    qob = [T2([128, 512], BF16) for _ in range(3)]
    vta = [T2([128, 4, 256], BF16) for _ in range(2)]
    DMA(wqa.ap, w_in_v[:, :, 0:1536], [], [wqa], q="pool")
    for kc in range(8):
        sv = wqa[:, kc, 0:1280].rearrange("p (h d) -> p h d", d=128)
        dv = wrot[:, kc, :].rearrange("p (h d) -> p h d", d=128)
        for (dlo, slo, neg) in ((0, 32, True), (32, 0, False), (64, 96, True), (96, 64, False)):
            if neg:
                ts(DVE, dv[:, :, dlo:dlo + 32], sv[:, :, slo:slo + 32], -1.0, None, ALU.mult, None, [wqa], [wrot])
            else:
                ACT(lambda e, dv=dv, sv=sv, dlo=dlo, slo=slo: e.copy(out=dv[:, :, dlo:dlo + 32], in_=sv[:, :, slo:slo + 32]), [wqa], [wrot])
    it = 0
    for (c, c0, w) in chunks:
        hc_ = hT_chunks[c]
        cs_, sn_ = cosT[c % 2], sinT[c % 2]
        DMA(cs_[:, 0:w], cosA_d[:, c0:c0 + w], [], [cs_])
        DMA(sn_[:, 0:w], sinA_d[:, c0:c0 + w], [], [sn_])
        for hh in range(10):
            raw, rot, ssp = psum[hh % 2], psum[2 + hh % 2], psum[4 + hh % 2]
            cols = slice(hh * 128, (hh + 1) * 128)
            mm(raw[:, 0:w], [(wqa[:, kc, cols], hT[:, kc, c0:c0 + w]) for kc in range(8)], [wqa, hc_], [raw])
            mm(rot[:, 0:w], [(wrot[:, kc, cols], hT[:, kc, c0:c0 + w]) for kc in range(8)], [wrot, hc_], [rot])
            sq_, rs_, t1_, t2_, qo_ = sqb[it % 2], rsd[it % 2], t1b[it % 2], t2b[it % 2], qob[it % 3]
            it += 1
            act(sq_[:, 0:w], raw[:, 0:w], AF.Square, [raw], [sq_])
            mm(ssp[:, 0:w], [(ones_b.ap, sq_[:, 0:w])], [ones_b, sq_], [ssp])
            rstd_from(rs_, rs_[:, 0:w], ssp[:, 0:w], [ssp], 128)
            g_ = gqT if hh < 8 else gkT
            stt(DVE, t1_[:, 0:w], raw[:, 0:w], g_[:, 0:1], cs_[:, 0:w], ALU.mult, ALU.mult, [raw, g_, cs_, sq_], [t1_])
            stt(DVE, t2_[:, 0:w], rot[:, 0:w], g_[:, 1:2], sn_[:, 0:w], ALU.mult, ALU.mult, [rot, g_, sn_], [t2_])
            tt(POOL, t1_[:, 0:w], t1_[:, 0:w], t2_[:, 0:w], ALU.add, [t1_, t2_], [t1_])
            tt(DVE, qo_[:, 0:w], t1_[:, 0:w], rs_[:, 0:w], ALU.mult, [t1_, rs_], [qo_])
            if hh < 8:
                qstore(qa_scr[hh], qo_, c, c0, w)
            else:
                DMA(ka_scr[hh - 8, :, c0:c0 + w], qo_[:, 0:w], [qo_], [])
        vt_ = vta[c % 2]
        nsub = w // 128
        for sub in range(nsub):
            pv = psum[6 + sub % 2]
            mm(pv[:, 0:256], [(hT[:, kc, c0 + sub * 128:c0 + (sub + 1) * 128], wqa[:, kc, 1280:1536]) for kc in range(8)],
               [wqa, hc_], [pv])
            ACT(lambda e, vt_=vt_, pv=pv, sub=sub: e.copy(out=vt_[:, sub, :], in_=pv[:, 0:256]), [pv], [vt_])
        for g in range(2):
            DMA(va_scr[g, :, c * 4:c * 4 + nsub, :], vt_[:, 0:nsub, g * 128:(g + 1) * 128], [vt_], [])

    if STAGE == 2:
        return finish(nc, S, es, block, all_dmas())

    S.barrier()
    T3 = Alloc(PERSIST_END)
    wm = T3([128, 8, 448], BF16); wkrot = T3([128, 8, 64], BF16)
    wqb_t = T3([128, 2, 1536], BF16); wqb_rot = T3([128, 2, 8, 64], BF16)
    wkvb_t = T3([128, 2048], BF16); wv_t = T3([128, 1024], BF16)
    cosBt = [T3([128, 512]) for _ in range(2)]; sinBt = [T3([128, 512]) for _ in range(2)]
    sqa = T3([128, 2, 512], BF16); qan = T3([128, 2, 512], BF16); ckvn = T3([128, 512], BF16)
    sqk = T3([128, 512], BF16); krsq = T3([128, 512], BF16); krr = T3([128, 512]); krt2 = T3([128, 512])
    rs3 = [T3([128, 512]) for _ in range(4)]
    sqn = [T3([128, 512], BF16) for _ in range(2)]; sqr = [T3([128, 512], BF16) for _ in range(2)]
    t13 = [T3([128, 512]) for _ in range(2)]; t23 = [T3([128, 512]) for _ in range(2)]
    o3 = [T3([128, 512], BF16) for _ in range(8)]
    vtb = [T3([128, 4, 1024], BF16) for _ in range(2)]
    DMA(wm.ap, w_in_v[:, :, 1536:1984], [], [wm], q="pool")
    DMA(wqb_t.ap, w_qb.rearrange("(c p) n -> p c n", p=128), [], [wqb_t], q="pool")
    DMA(wkvb_t.ap, w_kvb, [], [wkvb_t], q="pool")
    POOL(lambda e: e.tensor_copy(out=wv_t.ap.rearrange("p (h d) -> p h d", d=128),
                                 in_=wkvb_t.ap.rearrange("p (h t d) -> p h t d", t=2, d=128)[:, :, 1, :]), [wkvb_t], [wv_t])
    for (dlo, slo, neg) in ((0, 16, True), (16, 0, False), (32, 48, True), (48, 32, False)):
        for kc in range(8):
            if neg:
                ts(DVE, wkrot[:, kc, dlo:dlo + 16], wm[:, kc, 384 + slo:384 + slo + 16], -1.0, None, ALU.mult, None, [wm], [wkrot])
            else:
                ACT(lambda e, kc=kc, dlo=dlo, slo=slo: e.copy(out=wkrot[:, kc, dlo:dlo + 16], in_=wm[:, kc, 384 + slo:384 + slo + 16]), [wm], [wkrot])
        for c2 in range(2):
            sv = wqb_t[:, c2, :].rearrange("p (h d) -> p h d", d=192)
            if neg:
                ts(DVE, wqb_rot[:, c2, :, dlo:dlo + 16], sv[:, :, 128 + slo:128 + slo + 16], -1.0, None, ALU.mult, None, [wqb_t], [wqb_rot])
            else:
                ACT(lambda e, c2=c2, sv=sv, dlo=dlo, slo=slo: e.copy(out=wqb_rot[:, c2, :, dlo:dlo + 16], in_=sv[:, :, 128 + slo:128 + slo + 16]), [wqb_t], [wqb_rot])
    it = 0
    for (c, c0, w) in chunks:
        hc_ = hT_chunks[c]
        W = slice(0, w)
        cs_, sn_ = cosBt[c % 2], sinBt[c % 2]
        DMA(cs_[0:64, W], cosB_d[:, c0:c0 + w], [], [cs_])
        DMA(sn_[0:64, W], sinB_d[:, c0:c0 + w], [], [sn_])
        for c2 in range(2):
            mm(psum[c2][:, W], [(wm[:, kc, c2 * 128:(c2 + 1) * 128], hT[:, kc, c0:c0 + w]) for kc in range(8)], [wm, hc_], [psum[c2]])
            act(sqa[:, c2, W], psum[c2][:, W], AF.Square, [psum[c2]], [sqa])
        mm(psum[3][:, W], [(ones_b.ap, sqa[:, 0, W]), (ones_b.ap, sqa[:, 1, W])], [ones_b, sqa], [psum[3]])
        r_ = rs3[0]
        rstd_from(r_, r_[:, W], psum[3][:, W], [psum[3]], 256)
        for c2 in range(2):
            stt(DVE, qan[:, c2, W], psum[c2][:, W], gqaT[:, c2:c2 + 1], r_[:, W], ALU.mult, ALU.mult, [psum[c2], gqaT, r_, sqa], [qan])
        mm(psum[4][:, W], [(wm[:, kc, 256:384], hT[:, kc, c0:c0 + w]) for kc in range(8)], [wm, hc_], [psum[4]])
        act(sqk[:, W], psum[4][:, W], AF.Square, [psum[4]], [sqk])
        mm(psum[5][:, W], [(ones_b.ap, sqk[:, W])], [ones_b, sqk], [psum[5]])
        r_ = rs3[1]
        rstd_from(r_, r_[:, W], psum[5][:, W], [psum[5]], 128)
        stt(DVE, ckvn[:, W], psum[4][:, W], gkvaT[:, 0:1], r_[:, W], ALU.mult, ALU.mult, [psum[4], gkvaT, r_, sqk], [ckvn])
        mm(psum[6][0:64, W], [(wm[:, kc, 384:448], hT[:, kc, c0:c0 + w]) for kc in range(8)], [wm, hc_], [psum[6]])
        mm(psum[7][0:64, W], [(wkrot[:, kc, :], hT[:, kc, c0:c0 + w]) for kc in range(8)], [wkrot, hc_], [psum[7]])
        act(krsq[0:64, W], psum[6][0:64, W], AF.Square, [psum[6]], [krsq])
        stt(DVE, krr[0:64, W], psum[6][0:64, W], gmkT[0:64, 1:2], cs_[0:64, W], ALU.mult, ALU.mult, [psum[6], gmkT, cs_, krsq], [krr])
        stt(DVE, krt2[0:64, W], psum[7][0:64, W], gmkT[0:64, 2:3], sn_[0:64, W], ALU.mult, ALU.mult, [psum[7], gmkT, sn_], [krt2])
        tt(POOL, krr[0:64, W], krr[0:64, W], krt2[0:64, W], ALU.add, [krr, krt2], [krr])
        vt_ = vtb[c % 2]
        nsub = w // 128
        for sub in range(nsub):
            for half in range(2):
                pv = psum[6 + half]
                mm(pv[:, :], [(ckvn[:, sub * 128:(sub + 1) * 128], wv_t[:, half * 512:(half + 1) * 512])], [ckvn, wv_t], [pv])
                ACT(lambda e, vt_=vt_, pv=pv, sub=sub, half=half: e.copy(out=vt_[:, sub, half * 512:(half + 1) * 512], in_=pv[:, :]), [pv], [vt_])
        for h in range(8):
            DMA(vb_scr[h, :, c * 4:c * 4 + nsub, :], vt_[:, 0:nsub, h * 128:(h + 1) * 128], [vt_], [])
        for h in range(8):
            par = h % 2
            pn, pr, prr, pss = psum[par], psum[2 + par], psum[4 + par], psum[6 + par]
            sqn_, sqr_, t1_, t2_ = sqn[par], sqr[par], t13[par], t23[par]
            rq_ = rs3[2 + par]
            mm(pn[:, W], [(wqb_t[:, c2, h * 192:h * 192 + 128], qan[:, c2, W]) for c2 in range(2)], [wqb_t, qan], [pn])
            mm(pr[0:64, W], [(wqb_t[:, c2, h * 192 + 128:h * 192 + 192], qan[:, c2, W]) for c2 in range(2)], [wqb_t, qan], [pr])
            mm(prr[0:64, W], [(wqb_rot[:, c2, h, :], qan[:, c2, W]) for c2 in range(2)], [wqb_rot, qan], [prr])
            act(sqn_[:, W], pn[:, W], AF.Square, [pn], [sqn_])
            act(sqr_[0:64, W], pr[0:64, W], AF.Square, [pr], [sqr_])
            mm(pss[:, W], [(ones_b.ap, sqn_[:, W]), (ones_b[0:64, :], sqr_[0:64, W])], [ones_b, sqn_, sqr_], [pss])
            rstd_from(rq_, rq_[:, W], pss[:, W], [pss], 192)
            oq, oqr = o3[par * 4], o3[par * 4 + 1]
            stt(DVE, oq[:, W], pn[:, W], gmqT[:, 0:1], rq_[:, W], ALU.mult, ALU.mult, [pn, gmqT, rq_, sqn_], [oq])
            qstore(qb_scr[h], oq, c, c0, w)
            stt(DVE, t1_[0:64, W], pr[0:64, W], gmqT[0:64, 1:2], cs_[0:64, W], ALU.mult, ALU.mult, [pr, gmqT, cs_, sqr_], [t1_])
            stt(DVE, t2_[0:64, W], prr[0:64, W], gmqT[0:64, 2:3], sn_[0:64, W], ALU.mult, ALU.mult, [prr, gmqT, sn_], [t2_])
            tt(POOL, t1_[0:64, W], t1_[0:64, W], t2_[0:64, W], ALU.add, [t1_, t2_], [t1_])
            tt(DVE, oqr[0:64, W], t1_[0:64, W], rq_[0:64, W], ALU.mult, [t1_, rq_], [oqr])
            qstore(qbr_scr[h], oqr, c, c0, w, parts=64)
        for h in range(8):
            par = h % 2
            pk, psk = psum[h % 4], psum[4 + h % 4]
            sqn_, rk_ = sqn[par], rs3[2 + par]
            ok, okr = o3[par * 4 + 2], o3[par * 4 + 3]
            mm(pk[:, W], [(wkvb_t[:, h * 256:h * 256 + 128], ckvn[:, W])], [wkvb_t, ckvn], [pk])
            act(sqn_[:, W], pk[:, W], AF.Square, [pk], [sqn_])
            mm(psk[:, W], [(ones_b.ap, sqn_[:, W]), (ones_b[0:64, :], krsq[0:64, W])], [ones_b, sqn_, krsq], [psk])
            rstd_from(rk_, rk_[:, W], psk[:, W], [psk], 192)
            stt(DVE, ok[:, W], pk[:, W], gmkT[:, 0:1], rk_[:, W], ALU.mult, ALU.mult, [pk, gmkT, rk_, sqn_], [ok])
            DMA(kb_scr[h, :, c0:c0 + w], ok[:, W], [ok], [])
            tt(POOL, okr[0:64, W], krr[0:64, W], rk_[0:64, W], ALU.mult, [krr, rk_], [okr])
            DMA(kbr_scr[h, :, c0:c0 + w], okr[0:64, W], [okr], [])

    if STAGE == 3:
        return finish(nc, S, es, block, all_dmas())

    scr_tok = Tile(None)
    bar = S.add("sp", None, [], [scr_tok])
    for e_ in ENGS:
        for op in S.ops[e_]:
            if op.dma and op is not bar:
                bar.deps.append(op); op.needs_inc = True

    S.barrier()
    T4 = Alloc(PERSIST_END)
    qTb = [T4([128, SEQ], BF16) for _ in range(2)]; qrb = [T4([128, SEQ], BF16) for _ in range(2)]
    kTb = [T4([128, NT], BF16) for _ in range(2)]; krb = [T4([128, NT], BF16) for _ in range(2)]
    Vb = [T4([128, 34, 128], BF16) for _ in range(2)]
    pT = [T4([128, 512], BF16) for _ in range(4)]
    rinv = [T4([128, 512]) for _ in range(2)]
    obuf = [T4([128, 512], BF16) for _ in range(2)]
    o_stores = []
    LAG = 2
    zfill_ops = []
    if SPARSE:
        xs_d = dscr("xs_scr", [NBLK * BLK, D], BF16)
        zt4 = T4([128, 4096], BF16)
        POOL(lambda e: e.memset(zt4.ap, 0.0), [], [zt4])
        for zi in range(NBLK * BLK // 512):
            zfill_ops.append(DMA(xs_d[zi * 512:(zi + 1) * 512, :].rearrange("(p r) d -> p (r d)", p=128), zt4.ap, [zt4], [], q="pool"))
    for t_ in qrb + krb:
        POOL(lambda e, t_=t_: e.memset(t_[64:128, :], 0.0), [], [t_])
    def head_bufs(hd):
        b = hd % 2
        if hd < 8:
            g_ = hd // 4
            return qTb[b], kTb[g_ % 2], Vb[g_ % 2], qrb[b], krb[b]
        return qTb[b], kTb[b], Vb[b], qrb[b], krb[b]

    def head_loads(hd):
        qT_, kT_, V_, qr_, kr_ = head_bufs(hd)
        if hd < 8:
            DMA(qT_.ap, qa_scr[hd], [scr_tok], [qT_])
            if hd % 4 == 0:
                DMA(kT_.ap, ka_scr[hd // 4], [scr_tok], [kT_])
                DMA(V_.ap, va_scr[hd // 4], [scr_tok], [V_])
        else:
            h_ = hd - 8
            DMA(qT_.ap, qb_scr[h_], [scr_tok], [qT_])
            DMA(qr_[0:64, :], qbr_scr[h_], [scr_tok], [qr_])
            DMA(kT_.ap, kb_scr[h_], [scr_tok], [kT_])
            DMA(kr_[0:64, :], kbr_scr[h_], [scr_tok], [kr_])
            DMA(V_.ap, vb_scr[h_], [scr_tok], [V_])

    head_loads(0)
    for hd in range(16):
        mla = hd >= 8
        h = hd - 8 if mla else hd
        qT_, kT_, V_, qr_, kr_ = head_bufs(hd)
        scale = 192 ** -0.5 if mla else 128 ** -0.5
        if hd + 1 < 16:
            head_loads(hd + 1)
        for qi in range(8):
            o_ps = psum[4 + qi % 2]
            sum_ps = psum[6 + qi % 2]
            Q = slice(qi * 512, (qi + 1) * 512)
            for step in range(34 + LAG):
                if step < 34:
                    ti = step
                    s_ps = psum[ti % 4]
                    Kc = slice(ti * 128, (ti + 1) * 128)
                    pairs = [(kT_[:, Kc], qT_[:, Q])]
                    rds = [kT_, qT_]
                    if mla:
                        pairs.append((kr_[:, Kc], qr_[:, Q]))
                        rds += [kr_, qr_]
                    mm(s_ps[:, :], pairs, rds, [s_ps])
                    p_ = pT[ti % 4]
                    act(p_.ap, s_ps[:, :], AF.Exp, [s_ps], [p_], scale=scale)
                if step >= LAG:
                    ti = step - LAG
                    p_ = pT[ti % 4]
                    PE(lambda e, o_ps=o_ps, V_=V_, ti=ti, p_=p_: e.matmul(o_ps[:, :], V_[:, ti, :], p_.ap, start=(ti == 0), stop=(ti == 33)),
                       [V_, p_], [o_ps])
                    PE(lambda e, sum_ps=sum_ps, ti=ti, p_=p_: e.matmul(sum_ps[:, :], ones_b.ap, p_.ap, start=(ti == 0), stop=(ti == 33)),
                       [ones_b, p_], [sum_ps])
            ri, ob = rinv[qi % 2], obuf[qi % 2]
            DVE(lambda e, ri=ri, sum_ps=sum_ps: e.reciprocal(out=ri.ap, in_=sum_ps[:, :]), [sum_ps], [ri])
            tt(DVE, ob.ap, o_ps[:, :], ri.ap, ALU.mult, [o_ps, ri], [ob])
            o_stores.append(DMA(o_scr[hd, :, Q], ob.ap, [ob], []))

    o_tok = Tile(None)
    bar2 = S.add("sp", None, [], [o_tok])
    for op in o_stores:
        bar2.deps.append(op); op.needs_inc = True

    if STAGE == 4:
        return finish(nc, S, es, block, o_stores)

    S.barrier()
    T5 = Alloc(PERSIST_END)
    wg = T5([128, 8, 2048], BF16); woa_t = T5([128, 8, D], BF16); wob_t = T5([128, 8, D], BF16); wout_t = T5([128, 8, D], BF16)
    DMA(wg.ap, w_in_v[:, :, 1984:4032], [], [wg], q="pool")
    DMA(woa_t.ap, w_oa.rearrange("(kc p) n -> p kc n", p=128), [], [woa_t], q="pool")
    DMA(wob_t.ap, w_ob.rearrange("(kc p) n -> p kc n", p=128), [], [wob_t], q="pool")
    DMA(wout_t.ap, w_out.rearrange("(kc p) n -> p kc n", p=128), [], [wout_t], q="pool")
    oTt = [T5([128, 16, 512], BF16) for _ in range(1)]
    yT = [T5([128, 8, 512], BF16) for _ in range(1)]
    sgb = [T5([128, 512]) for _ in range(4)]
    xt5 = [T5([128, D]) for _ in range(1)]
    x1t = [T5([128, D]) for _ in range(1)]
    x1_stores = []
    for qi in range(8):
        Q = slice(qi * 512, (qi + 1) * 512)
        HQ = slice(256 + qi * 512, 256 + (qi + 1) * 512)
        hc_a, hc_b = hT_chunks[(256 + qi * 512) // 512], hT_chunks[(256 + qi * 512 + 511) // 512]
        oT_ = oTt[0]
        for hq in range(4):
            DMA(oT_[:, hq * 4:(hq + 1) * 4, :], o_scr[hq * 4:(hq + 1) * 4, :, Q].rearrange("h d t -> d h t"), [o_tok], [oT_])
        yT_ = yT[0]
        for m in range(8):
            pga, pgb, pA, pB = psum[m % 2], psum[2 + m % 2], psum[4], psum[5]
            M = slice(m * 128, (m + 1) * 128)
            mm(pga[:, :], [(wg[:, kc, m * 128:(m + 1) * 128], hT[:, kc, HQ]) for kc in range(8)], [wg, hc_a, hc_b], [pga])
            mm(pgb[:, :], [(wg[:, kc, D + m * 128:D + (m + 1) * 128], hT[:, kc, HQ]) for kc in range(8)], [wg, hc_a, hc_b], [pgb])
            mm(pA[:, :], [(woa_t[:, hh, M], oT_[:, hh, :]) for hh in range(8)], [woa_t, oT_], [pA])
            mm(pB[:, :], [(wob_t[:, hh, M], oT_[:, 8 + hh, :]) for hh in range(8)], [wob_t, oT_], [pB])
            sa, sb = sgb[(m % 2) * 2], sgb[(m % 2) * 2 + 1]
            act(sa.ap, pga[:, :], AF.Sigmoid, [pga], [sa])
            act(sb.ap, pgb[:, :], AF.Sigmoid, [pgb], [sb])
            tt(DVE, sa.ap, sa.ap, pA[:, :], ALU.mult, [sa, pA], [sa])
            tt(DVE, sb.ap, sb.ap, pB[:, :], ALU.mult, [sb, pB], [sb])
            tt(POOL, yT_[:, m, :], sa.ap, sb.ap, ALU.add, [sa, sb], [yT_])
        for sub in range(4):
            tok0 = qi * 512 + sub * 128
            xt_, x1_ = xt5[0], x1t[0]
            DMA(xt_.ap, x_d[tok0:tok0 + 128, :], [], [xt_])
            for n in range(2):
                po = psum[6 + n]
                mm(po[:, :], [(yT_[:, m, sub * 128:(sub + 1) * 128], wout_t[:, m, n * 512:(n + 1) * 512]) for m in range(8)], [yT_, wout_t], [po])
                tt(DVE, x1_[:, n * 512:(n + 1) * 512], po[:, :], g1bc[:, n * 512:(n + 1) * 512], ALU.mult, [po, g1bc], [x1_])
            tt(POOL, x1_.ap, x1_.ap, xt_.ap, ALU.add, [x1_, xt_], [x1_])
            x1_stores.append(DMA(x1_scr[tok0:tok0 + 128, :], x1_.ap, [x1_], []))
    x1_tok = Tile(None)
    bar3 = S.add("sp", None, [], [x1_tok])
    for op in x1_stores:
        bar3.deps.append(op); op.needs_inc = True
    if STAGE == 5:
        return finish(nc, S, es, block, x1_stores)


    S.barrier()
    if SPARSE:
        return sparse_moe(locals())

    T6 = Alloc(HT_OFF)
    rw_t = T6([128, 8, NEXP], BF16); rb_row = T6([1, NEXP], BF16); b1T = T6([128, 16, NEXP])
    b2rows = [T6([1, D], BF16) for _ in range(2)]
    gates = T6([128, 8, NEXP]); lgt = [T6([128, NEXP]) for _ in range(2)]; ext = [T6([128, NEXP]) for _ in range(2)]
    mk = [T6([128, NEXP]) for _ in range(2)]
    top8 = [T6([128, 8]) for _ in range(2)]; sm6 = [T6([128, 2]) for _ in range(2)]
    T6_MID = T6.off
    b1raw = T6([32, 2 * D])
    if not os.environ.get("MK_SKIP_SETUP"):
        DMA(rw_t.ap, rw_d.rearrange("(kc p) e -> p kc e", p=128), [], [rw_t], q="pool")
        DMA(rb_row.ap, rb_d.rearrange("(o e) -> o e", o=1), [], [rb_row], q="pool")
        DMA(b1raw.ap, eb1, [], [b1raw])
    for half in range(0 if os.environ.get("MK_SKIP_B1") else 2):
        pt = psum[half]

        def fnb(e, half=half, pt=pt):
            ins = None
            for f in range(8):
                fc = half * 8 + f
                ins = e.transpose(out=pt[:, f * 32:(f + 1) * 32], in_=b1raw[0:32, fc * 128:(fc + 1) * 128], identity=ident[0:32, 0:32])
            return ins
        PE(fnb, [b1raw, ident], [pt])
        DVE(lambda e, half=half, pt=pt: e.tensor_copy(out=b1T[:, half * 8:(half + 1) * 8, :], in_=pt[:, 0:256].rearrange("p (f e) -> p f e", e=NEXP)), [pt], [b1T])
    ts(DVE, b1T[:, 8:16, :], b1T[:, 8:16, :], 1.0, None, ALU.add, None, [b1T], [b1T])
    S.barrier()
    T6 = Alloc(T6_MID)
    h2T = T6([128, 8, 1024], BF16); h2f = T6([128, 8, 128]); acc = T6([128, 8, D])
    w1b = [T6([128, 8, 2 * D], BF16) for _ in range(2)]; w2t = T6([128, 8, D], BF16)
    actT = [T6([128, 8, 512], BF16) for _ in range(2)]
    ta = [T6([128, 512]) for _ in range(2)]; tsg = [T6([128, 512]) for _ in range(2)]; tl = [T6([128, 512]) for _ in range(2)]
    NB["xt"] = [T6([128, D]) for _ in range(2)]; NB["xn"] = [T6([128, D]) for _ in range(2)]
    h2chunks = [Tile(None) for _ in range(2)]
    final_stores = []
    nm_i = 0
    for g in range(int(os.environ.get("MK_NG", "4"))):
        for sub in range(8):
            tok0 = g * 1024 + sub * 128
            hch = h2chunks[sub // 4]
            if os.environ.get("MK_SKIP_NM"):
                continue
            norm_mod_T(nm_i, x1_scr[tok0:tok0 + 128, :], A2.ap, modT[:, 24:32, 0],
                       lambda j, sub=sub, hch=hch: (h2T[:, j, sub * 128:(sub + 1) * 128], hch),
                       (psum[(nm_i % 2) * 2], psum[(nm_i % 2) * 2 + 1]), src_deps=[x1_tok])
            nm_i += 1
            if os.environ.get("MK_SKIP_RT"):
                continue
            lg_ps = psum[4 + sub % 2]
            mm(lg_ps[:, 0:NEXP], [(h2T[:, kc, sub * 128:(sub + 1) * 128], rw_t[:, kc, :]) for kc in range(8)] + [(ones_b[0:1, :], rb_row[0:1, :])],
               [hch, rw_t, ones_b, rb_row], [lg_ps])
            lg, ex, mk_, t8, sm = lgt[sub % 2], ext[sub % 2], mk[sub % 2], top8[sub % 2], sm6[sub % 2]
            DVE(lambda e, lg=lg, lg_ps=lg_ps: e.tensor_copy(out=lg.ap, in_=lg_ps[:, 0:NEXP]), [lg_ps], [lg])
            DVE(lambda e, t8=t8, lg=lg: e.max(out=t8.ap, in_=lg.ap), [lg], [t8])
            ts(DVE, mk_.ap, lg.ap, t8[:, 3:4], None, ALU.is_ge, None, [lg, t8], [mk_])
            ts(DVE, sm[:, 0:1], t8[:, 0:1], -1.0, None, ALU.mult, None, [t8], [sm])
            act(ex.ap, lg.ap, AF.Exp, [lg, sm], [ex], bias=sm[:, 0:1])
            tt(DVE, ex.ap, ex.ap, mk_.ap, ALU.mult, [ex, mk_], [ex])
            DVE(lambda e, sm=sm, ex=ex: e.reduce_sum(out=sm[:, 1:2], in_=ex.ap, axis=AX.X), [ex], [sm])
            DVE(lambda e, sm=sm: e.reciprocal(out=sm[:, 1:2], in_=sm[:, 1:2]), [sm], [sm])
            ts(DVE, gates[:, sub, :], ex.ap, sm[:, 1:2], None, ALU.mult, None, [ex, sm], [gates])
        for ex_i in range(int(os.environ.get("MK_NE", "32"))):
            w1_ = w1b[ex_i % 2]
            b2r = b2rows[ex_i % 2]
            if not (os.environ.get("MK_NOW") and (g > 0 or ex_i > 1)):
                DMA(w1_.ap, ew1[ex_i].rearrange("(kc p) n -> p kc n", p=128), [], [w1_], q="pool")
                DMA(w2t.ap, ew2[ex_i].rearrange("(kc p) n -> p kc n", p=128), [], [w2t], q="pool")
            DMA(b2r.ap, eb2[ex_i:ex_i + 1, :], [], [b2r], q="pool")
            for rt in range(2):
                hch = h2chunks[rt]
                C = slice(rt * 512, (rt + 1) * 512)
                aT = actT[rt % 2]
                for fc in range(8):
                    pg, pl = psum[fc % 2], psum[2 + fc % 2]
                    a_, s_, l_ = ta[fc % 2], tsg[fc % 2], tl[fc % 2]
                    mm(pg[:, :], [(w1_[:, kc, fc * 128:(fc + 1) * 128], h2T[:, kc, C]) for kc in range(8)], [w1_, hch], [pg])
                    mm(pl[:, :], [(w1_[:, kc, D + fc * 128:D + (fc + 1) * 128], h2T[:, kc, C]) for kc in range(8)], [w1_, hch], [pl])
                    ts(DVE, a_.ap, pg[:, :], b1T[:, fc, ex_i:ex_i + 1], 7.0, ALU.add, ALU.min, [pg, b1T], [a_])
                    act(s_.ap, a_.ap, AF.Sigmoid, [a_], [s_], scale=1.702)
                    act(l_.ap, pl[:, :], AF.Identity, [pl, b1T], [l_], bias=b1T[:, 8 + fc, ex_i:ex_i + 1])
                    ts(DVE, l_.ap, l_.ap, -6.0, 8.0, ALU.max, ALU.min, [l_], [l_])
                    tt(DVE, s_.ap, a_.ap, s_.ap, ALU.mult, [a_, s_], [s_])
                    tt(POOL, aT[:, fc, :], s_.ap, l_.ap, ALU.mult, [s_, l_], [aT])
                for s4 in range(4):
                    sub = rt * 4 + s4
                    for n in range(2):
                        py = psum[4 + (s4 % 2) * 2 + n]
                        N = slice(n * 512, (n + 1) * 512)
                        mm(py[:, :], [(aT[:, fc, s4 * 128:(s4 + 1) * 128], w2t[:, fc, N]) for fc in range(8)] + [(ones_b[0:1, :], b2r[0:1, N])],
                           [aT, w2t, ones_b, b2r], [py])
                        if ex_i == 0:
                            ts(DVE, acc[:, sub, N], py[:, :], gates[:, sub, ex_i:ex_i + 1], None, ALU.mult, None, [py, gates], [acc])
                        else:
                            stt(DVE, acc[:, sub, N], py[:, :], gates[:, sub, ex_i:ex_i + 1], acc[:, sub, N], ALU.mult, ALU.add, [py, gates, acc], [acc])
        for sub in range(8):
            tok0 = g * 1024 + sub * 128
            xt_ = NB["xt"][sub % 2]
            DMA(xt_.ap, x1_scr[tok0:tok0 + 128, :], [x1_tok], [xt_])
            tt(DVE, acc[:, sub, :], acc[:, sub, :], g2bc.ap, ALU.mult, [acc, g2bc], [acc])
            tt(POOL, xt_.ap, xt_.ap, acc[:, sub, :], ALU.add, [xt_, acc], [xt_])
            final_stores.append(DMA(out_d[tok0:tok0 + 128, :], xt_.ap, [xt_], []))
    return finish(nc, S, es, block, final_stores)


def sparse_moe(L):
    g = dict(L)
    nc, S, es, block, Alloc, psum = g["nc"], g["S"], g["es"], g["block"], g["Alloc"], g["psum"]
    DMA, PE, ACT, DVE, POOL, mm, ts, tt, stt, act = (g[k] for k in ("DMA", "PE", "ACT", "DVE", "POOL", "mm", "ts", "tt", "stt", "act"))
    din, dscr, norm_mod_T, NB, finish = g["din"], g["dscr"], g["norm_mod_T"], g["NB"], finish_
    ident, ones_f, ones_b, modT, A2, g2bc, rs1 = g["ident"], g["ones_f"], g["ones_b"], g["modT"], g["A2"], g["g2bc"], g["rs1"]
    x1_scr, x1_tok, out_d, ab_scr = g["x1_scr"], g["x1_tok"], g["out_d"], g["ab_scr"]
    rw_d, rb_d, eb1, eb2, ew1, ew2 = g["rw_d"], g["rb_d"], g["eb1"], g["eb2"], g["ew1"], g["ew2"]
    IOA = bass.IndirectOffsetOnAxis

    NROWS = NBLK * BLK
    xs_d = g["xs_d"]
    ys_d = dscr("ys_scr", [NROWS, D], BF16)
    consts_d = g["consts_d"]
    ew1f = ew1.rearrange("e k n -> (e k) n"); ew2f = ew2.rearrange("e k n -> (e k) n")

    def IDMA(gather, dram, idx_ap, sb_ap, r, w):
        def fn(e):
            if gather:
                return e.indirect_dma_start(out=sb_ap, out_offset=None, in_=dram, in_offset=IOA(ap=idx_ap, axis=0))
            return e.indirect_dma_start(out=dram, out_offset=IOA(ap=idx_ap, axis=0), in_=sb_ap, in_offset=None)
        return S.add("pool", fn, r, w, dma=True)

    T6 = Alloc(g["HT_OFF"])
    cst = T6([128, NCONST])
    iota_r, pk = cst[:, 0:32], cst[:, 64:72]
    ustr_f = cst[:, 76:204]
    ustr_b = T6([128, 128], BF16); ident_b = T6([128, 128], BF16)
    rw_t = T6([128, 8, NEXP], BF16); rb_row = T6([1, NEXP], BF16)
    b1raw = T6([32, 2 * D]); b2_t = T6([32, D])
    A2bc = T6([128, D]); S2bc = T6([128, D])
    tot = T6([128, NEXP]); gates_all = T6([128, 32, NEXP]); gk_all = T6([128, 32, 4]); slots_u = T6([128, 32, 4], U32)
    ebv = T6([128, NBLK]); ohcol = T6([32, NBLK]); w_u = T6([128, 8, NBLK], U32)
    T6_MID = T6.off
    DMA(cst.ap, consts_d, [], [cst])
    DMA(rw_t.ap, rw_d.rearrange("(kc p) e -> p kc e", p=128), [], [rw_t], q="pool")
    DMA(rb_row.ap, rb_d.rearrange("(o e) -> o e", o=1), [], [rb_row], q="pool")
    DMA(b1raw.ap, eb1, [], [b1raw]); DMA(b2_t.ap, eb2, [], [b2_t])
    DMA(A2bc.ap, ab_scr[1], [], [A2bc]); DMA(S2bc.ap, ab_scr[0], [], [S2bc])
    ts(DVE, b1raw[:, D:2 * D], b1raw[:, D:2 * D], 1.0, None, ALU.add, None, [b1raw], [b1raw])
    DVE(lambda e: e.tensor_copy(out=ustr_b.ap, in_=ustr_f), [cst], [ustr_b])
    DVE(lambda e: e.tensor_copy(out=ident_b.ap, in_=ident.ap), [ident], [ident_b])
    POOL(lambda e: e.memset(tot.ap, 0.0), [], [tot])

    TA = Alloc(T6_MID)
    h2Tt = [TA([128, 8, 128], BF16) for _ in range(4)]
    h2tok = [TA([128, D], BF16) for _ in range(32)]
    tmpf = [TA([128, D]) for _ in range(3)]
    NB["xt"] = [TA([128, D]) for _ in range(4)]; NB["xn"] = [TA([128, D]) for _ in range(4)]
    lg_all = TA([128, 32, NEXP]); t8_all = TA([128, 32, 8]); posw = TA([128, 32, NEXP])
    ext = [TA([128, NEXP]) for _ in range(4)]; mkt = [TA([128, NEXP]) for _ in range(4)]
    mkb = [TA([128, NEXP], BF16) for _ in range(4)]; slotm = [TA([128, NEXP]) for _ in range(2)]
    sm6 = [TA([128, 2]) for _ in range(4)]
    oht = [TA([128, NEXP]) for _ in range(2)]; t32 = [TA([128, NEXP]) for _ in range(4)]
    slf = [TA([128, 4]) for _ in range(2)]
    zfill = g["zfill_ops"]
    nm_A, nm_B, nm_C = g["nm_A"], g["nm_B"], g["nm_C"]
    pactx = {}
    for step in range(32 + 2):
        if step < 32:
            pactx[step] = nm_A(step, x1_scr[step * 128:(step + 1) * 128, :], src_deps=[x1_tok])
        if 0 <= step - 1 < 32:
            nm_B(pactx[step - 1])
        if not (0 <= step - 2 < 32):
            continue
        i = step - 2
        tok0 = i * 128
        hT_ = h2Tt[i % 4]
        xt_ = pactx[i][0]
        nm_C(pactx[i], A2.ap, modT[:, 24:32, 0], lambda j, hT_=hT_: (hT_[:, j, :], hT_), (psum[(i % 2) * 2], psum[(i % 2) * 2 + 1]))
        rs_ = rs1[:, i % 4:i % 4 + 1]
        tf, hk = tmpf[i % 3], h2tok[i]
        stt(DVE, tf.ap, xt_.ap, rs_, A2bc.ap, ALU.mult, ALU.mult, [xt_, rs1, A2bc], [tf])
        tt(POOL, hk.ap, tf.ap, S2bc.ap, ALU.add, [tf, S2bc], [hk])
        lg_ps = psum[4 + i % 2]
        mm(lg_ps[:, 0:NEXP], [(hT_[:, kc, :], rw_t[:, kc, :]) for kc in range(8)] + [(ones_b[0:1, :], rb_row[0:1, :])],
           [hT_, rw_t, ones_b, rb_row], [lg_ps])
        ex, mk_, sm, mb = ext[i % 4], mkt[i % 4], sm6[i % 4], mkb[i % 4]
        lg_i, t8_i = lg_all[:, i, :], t8_all[:, i, :]
        DVE(lambda e, lg_i=lg_i, lg_ps=lg_ps: e.tensor_copy(out=lg_i, in_=lg_ps[:, 0:NEXP]), [lg_ps], [lg_all])
        DVE(lambda e, t8_i=t8_i, lg_i=lg_i: e.max(out=t8_i, in_=lg_i), [lg_all], [t8_all])
        ts(DVE, mk_.ap, lg_i, t8_all[:, i, 3:4], None, ALU.is_ge, None, [lg_all, t8_all], [mk_])
        ts(DVE, sm[:, 0:1], t8_all[:, i, 0:1], -1.0, None, ALU.mult, None, [t8_all], [sm])
        act(ex.ap, lg_i, AF.Exp, [lg_all, sm], [ex], bias=sm[:, 0:1])
        tt(DVE, ex.ap, ex.ap, mk_.ap, ALU.mult, [ex, mk_], [ex])
        DVE(lambda e, sm=sm, ex=ex: e.reduce_sum(out=sm[:, 1:2], in_=ex.ap, axis=AX.X), [ex], [sm])
        DVE(lambda e, sm=sm: e.reciprocal(out=sm[:, 1:2], in_=sm[:, 1:2]), [sm], [sm])
        ts(DVE, gates_all[:, i, :], ex.ap, sm[:, 1:2], None, ALU.mult, None, [ex, sm], [gates_all])
        DVE(lambda e, mb=mb, mk_=mk_: e.tensor_copy(out=mb.ap, in_=mk_.ap), [mk_], [mb])
        pos_ps, cs_ps = psum[6], psum[7]
        mm(pos_ps[:, 0:NEXP], [(ustr_b.ap, mb.ap)], [ustr_b, mb], [pos_ps])
        mm(cs_ps[:, 0:NEXP], [(ones_b.ap, mb.ap)], [ones_b, mb], [cs_ps])
        tt(DVE, posw[:, i, :], pos_ps[:, 0:NEXP], tot.ap, ALU.add, [pos_ps, tot], [posw])
        tt(DVE, tot.ap, tot.ap, cs_ps[:, 0:NEXP], ALU.add, [tot, cs_ps], [tot])

    tq, tm, pcv, pstart = (TA([128, NEXP]) for _ in range(4))
    pp = [TA([128, NEXP]) for _ in range(2)]
    cmpt = [TA([128, NEXP]) for _ in range(2)]
    wf = TA([128, 8, NBLK])
    ts(DVE, tq.ap, tot.ap, 0.0, None, ALU.is_gt, None, [tot], [tq])
    for m_ in range(1, ECAP // BLK):
        ts(DVE, tm.ap, tot.ap, float(m_ * BLK), None, ALU.is_gt, None, [tot], [tm])
        tt(DVE, tq.ap, tq.ap, tm.ap, ALU.add, [tq, tm], [tq])
    ts(DVE, pcv.ap, tq.ap, float(BLK), None, ALU.mult, None, [tq], [pcv])
    DVE(lambda e: e.tensor_copy(out=pp[0].ap, in_=pcv.ap), [pcv], [pp[0]])
    cur = 0
    for sh in (1, 2, 4, 8, 16):
        a_, b_ = pp[cur], pp[1 - cur]
        DVE(lambda e, a_=a_, b_=b_: e.tensor_copy(out=b_.ap, in_=a_.ap), [a_], [b_])
        tt(DVE, b_[:, sh:NEXP], a_[:, sh:NEXP], a_[:, 0:NEXP - sh], ALU.add, [a_], [b_])
        cur = 1 - cur
    pend = pp[cur]
    tt(DVE, pstart.ap, pend.ap, pcv.ap, ALU.subtract, [pend, pcv], [pstart])
    for b in range(NBLK):
        c_ = cmpt[b % 2]
        ts(DVE, c_.ap, pend.ap, float(b * BLK), None, ALU.is_le, None, [pend], [c_])
        DVE(lambda e, c_=c_, b=b: e.reduce_sum(out=ebv[:, b:b + 1], in_=c_.ap, axis=AX.X), [c_], [ebv])
    ts(DVE, ebv.ap, ebv.ap, float(NEXP - 1), None, ALU.min, None, [ebv], [ebv])
    for kc in range(8):
        ts(DVE, wf[:, kc, :], ebv.ap, float(D), pk[:, kc:kc + 1], ALU.mult, ALU.add, [ebv, cst], [wf])
        DVE(lambda e, kc=kc: e.tensor_copy(out=w_u[:, kc, :], in_=wf[:, kc, :]), [wf], [w_u])
    ts(DVE, ohcol.ap, ebv[0:32, :], cst[0:32, 72:73], None, ALU.is_equal, None, [ebv, cst], [ohcol])

    zb = S.add("pool", None, [], [])
    for op in zfill:
        zb.deps.append(op)
    scat = []
    for i in range(32):
        sl, sf, hk = slotm[i % 2], slf[i % 2], h2tok[i]
        tt(DVE, sl.ap, posw[:, i, :], pstart.ap, ALU.add, [posw, pstart], [sl])
        for k in range(4):
            oh, ta_, tb_ = oht[k % 2], t32[(k % 2) * 2], t32[(k % 2) * 2 + 1]
            ts(DVE, oh.ap, lg_all[:, i, :], t8_all[:, i, k:k + 1], None, ALU.is_equal, None, [lg_all, t8_all], [oh])
            tt(DVE, ta_.ap, oh.ap, sl.ap, ALU.mult, [oh, sl], [ta_])
            DVE(lambda e, sf=sf, ta_=ta_, k=k: e.reduce_sum(out=sf[:, k:k + 1], in_=ta_.ap, axis=AX.X), [ta_], [sf])
            tt(DVE, tb_.ap, oh.ap, gates_all[:, i, :], ALU.mult, [oh, gates_all], [tb_])
            DVE(lambda e, tb_=tb_, k=k, i=i: e.reduce_sum(out=gk_all[:, i, k:k + 1], in_=tb_.ap, axis=AX.X), [tb_], [gk_all])
        DVE(lambda e, sf=sf, i=i: e.tensor_copy(out=slots_u[:, i, :], in_=sf.ap), [sf], [slots_u])
        for k in range(4):
            scat.append(IDMA(False, xs_d, slots_u[:, i, k:k + 1], hk.ap, [hk, slots_u], []))
    xs_tok = Tile(None)
    bs = S.add("sp", None, [], [xs_tok])
    for op in scat:
        bs.deps.append(op)
    S.barrier()

    TB = Alloc(T6_MID)
    w1b = [TB([128, 8, 2 * D], BF16) for _ in range(2)]; w2b = [TB([128, 8, D], BF16) for _ in range(2)]
    xrows = [TB([128, 4, D], BF16) for _ in range(2)]; xTb = [TB([128, 8, BLK], BF16) for _ in range(2)]
    actT = [TB([128, 8, BLK], BF16) for _ in range(2)]
    ta = [TB([128, 512]) for _ in range(2)]; tsg = [TB([128, 512]) for _ in range(2)]; tl = [TB([128, 512]) for _ in range(2)]
    ysb = [TB([128, D], BF16) for _ in range(2)]
    b1c = [TB([128, 16]) for _ in range(2)]
    ysc = []
    def loads_xw1(b):
        xr, w1_ = xrows[b % 2], w1b[b % 2]
        DMA(xr.ap, xs_d[b * BLK:(b + 1) * BLK, :].rearrange("(j p) d -> p j d", p=128), [xs_tok], [xr])
        for kc in range(8):
            IDMA(True, ew1f, w_u[:, kc, b:b + 1], w1_[:, kc, :], [w_u], [w1_])

    def loads_w2(b):
        w2_ = w2b[b % 2]
        for kc in range(8):
            IDMA(True, ew2f, w_u[:, kc, b:b + 1], w2_[:, kc, :], [w_u], [w2_])

    def stage_pre(b):
        xr, xT_, bc = xrows[b % 2], xTb[b % 2], b1c[b % 2]
        pb = psum[4 + b % 2]

        def fb(e, pb=pb, b=b):
            ins = None
            for fc in range(16):
                ins = e.matmul(pb[:, fc:fc + 1], b1raw[0:32, fc * 128:(fc + 1) * 128], ohcol[0:32, b:b + 1], start=True, stop=True)
            return ins
        PE(fb, [b1raw, ohcol], [pb])
        DVE(lambda e, bc=bc, pb=pb: e.tensor_copy(out=bc.ap, in_=pb[:, 0:16]), [pb], [bc])
        for j in range(4):
            for half in range(2):
                pt = psum[6 + half]
                ptb = pt.ap.bitcast(BF16)

                def ft(e, ptb=ptb, xr=xr, j=j, half=half):
                    ins = None
                    for q in range(4):
                        kc = half * 4 + q
                        ins = e.transpose(out=ptb[:, q * 128:(q + 1) * 128], in_=xr[:, j, kc * 128:(kc + 1) * 128], identity=ident_b.ap)
                    return ins
                PE(ft, [xr, ident_b], [pt])
                src = ptb[:, 0:512].rearrange("p (q r) -> p q r", q=4)
                dst = xT_[:, half * 4:(half + 1) * 4, j * 128:(j + 1) * 128]
                if half == 0:
                    ACT(lambda e, dst=dst, src=src: e.copy(out=dst, in_=src), [pt], [xT_])
                else:
                    DVE(lambda e, dst=dst, src=src: e.tensor_copy(out=dst, in_=src), [pt], [xT_])

    def stage_w1(b):
        xT_, w1_, aT, bc = xTb[b % 2], w1b[b % 2], actT[b % 2], b1c[b % 2]
        for fc in range(8):
            pg, pl = psum[fc % 2], psum[2 + fc % 2]
            a_, s_, l_ = ta[fc % 2], tsg[fc % 2], tl[fc % 2]
            mm(pg[:, :], [(w1_[:, kc, fc * 128:(fc + 1) * 128], xT_[:, kc, :]) for kc in range(8)], [w1_, xT_], [pg])
            mm(pl[:, :], [(w1_[:, kc, D + fc * 128:D + (fc + 1) * 128], xT_[:, kc, :]) for kc in range(8)], [w1_, xT_], [pl])
            ts(DVE, a_.ap, pg[:, :], bc[:, fc:fc + 1], 7.0, ALU.add, ALU.min, [pg, bc], [a_])
            act(s_.ap, a_.ap, AF.Sigmoid, [a_], [s_], scale=1.702)
            act(l_.ap, pl[:, :], AF.Identity, [pl, bc], [l_], bias=bc[:, 8 + fc:9 + fc])
            ts(DVE, l_.ap, l_.ap, -6.0, 8.0, ALU.max, ALU.min, [l_], [l_])
            tt(DVE, s_.ap, a_.ap, s_.ap, ALU.mult, [a_, s_], [s_])
            tt(POOL, aT[:, fc, :], s_.ap, l_.ap, ALU.mult, [s_, l_], [aT])

    def stage_w2(b):
        w2_, aT = w2b[b % 2], actT[b % 2]
        for j in range(4):
            yb = ysb[j % 2]
            for n in range(2):
                py = psum[4 + (j % 2) * 2 + n]
                N = slice(n * 512, (n + 1) * 512)
                mm(py[:, :], [(aT[:, fc, j * 128:(j + 1) * 128], w2_[:, fc, N]) for fc in range(8)], [aT, w2_], [py])
                if n == 0:
                    ACT(lambda e, yb=yb, py=py, N=N: e.copy(out=yb[:, N], in_=py[:, :]), [py], [yb])
                else:
                    DVE(lambda e, yb=yb, py=py, N=N: e.tensor_copy(out=yb[:, N], in_=py[:, :]), [py], [yb])
            ysc.append(DMA(ys_d[b * BLK + j * 128:b * BLK + (j + 1) * 128, :], yb.ap, [yb], []))

    loads_xw1(0); loads_w2(0)
    stage_pre(0)
    for b in range(NBLK):
        if b + 1 < NBLK:
            loads_xw1(b + 1)
        stage_w1(b)
        if b >= 1:
            stage_w2(b - 1)
        if b + 1 < NBLK:
            loads_w2(b + 1)
            stage_pre(b + 1)
    stage_w2(NBLK - 1)
    ys_tok = Tile(None)
    bs2 = S.add("pool", None, [], [ys_tok])
    for op in ysc:
        bs2.deps.append(op)
    S.barrier()

    TC = Alloc(T6_MID)
    yk = [TC([128, D], BF16) for _ in range(8)]
    accc = [TC([128, D]) for _ in range(2)]
    gT = [TC([32, 128]) for _ in range(2)]
    xt6 = [TC([128, D]) for _ in range(2)]
    final_stores = []
    for i in range(32):
        tok0 = i * 128
        ac, gt, xt_ = accc[i % 2], gT[i % 2], xt6[i % 2]
        for k in range(4):
            IDMA(True, ys_d, slots_u[:, i, k:k + 1], yk[(i % 2) * 4 + k].ap, [ys_tok, slots_u], [yk[(i % 2) * 4 + k]])
        DMA(xt_.ap, x1_scr[tok0:tok0 + 128, :], [x1_tok], [xt_])
        pgt = psum[i % 2]
        PE(lambda e, pgt=pgt, i=i: e.transpose(out=pgt[0:32, 0:128], in_=gates_all[:, i, :], identity=ident.ap), [gates_all, ident], [pgt])
        DVE(lambda e, gt=gt, pgt=pgt: e.tensor_copy(out=gt.ap, in_=pgt[0:32, 0:128]), [pgt], [gt])
        for n in range(2):
            pbias = psum[2 + (i % 2) * 2 + n]
            N = slice(n * 512, (n + 1) * 512)
            mm(pbias[:, :], [(gt[0:32, :], b2_t[0:32, N])], [gt, b2_t], [pbias])
            stt(DVE, ac[:, N], yk[(i % 2) * 4][:, N], gk_all[:, i, 0:1], pbias[:, :], ALU.mult, ALU.add, [yk[(i % 2) * 4], gk_all, pbias], [ac])
        for k in range(1, 4):
            y_ = yk[(i % 2) * 4 + k]
            stt(DVE, ac.ap, y_.ap, gk_all[:, i, k:k + 1], ac.ap, ALU.mult, ALU.add, [y_, gk_all, ac], [ac])
        tt(DVE, ac.ap, ac.ap, g2bc.ap, ALU.mult, [ac, g2bc], [ac])
        tt(DVE, xt_.ap, xt_.ap, ac.ap, ALU.add, [xt_, ac], [xt_])
        final_stores.append(DMA(out_d[tok0:tok0 + 128, :], xt_.ap, [xt_], []))
    return finish(nc, S, es, block, final_stores)


def finish_(nc, S, es, block, final_deps):
    return finish(nc, S, es, block, final_deps)


def finish(nc, S, es, block, final_deps):
    fin = S.add("sp", None, [], [])
    for d in final_deps:
        fin.deps.append(d)
        d.needs_inc = True
    S.emit(nc, es, block)
    es.close()
    return nc, S


_CONSTS = None


def rope_tables():
    theta = 10000.0
    n_rows = SEQ // 64
    row = np.repeat(np.arange(n_rows), 64).astype(np.float32)
    col = np.tile(np.arange(64), n_rows).astype(np.float32)

    def tab(half):
        nf = half // 2
        inv = (theta ** (-(np.arange(0, half, 2, dtype=np.float32)) / half)).astype(np.float32)
        cos = np.ones((2 * half, NT), np.float32)
        sin = np.zeros((2 * half, NT), np.float32)
        for blk, pos in enumerate((row, col)):
            ang = (pos[None, :] * inv[:, None]).astype(np.float32)
            c, s = np.cos(ang).astype(np.float32), np.sin(ang).astype(np.float32)
            base = blk * half
            cos[base:base + nf, NCTX:] = c
            cos[base + nf:base + half, NCTX:] = c
            sin[base:base + nf, NCTX:] = s
            sin[base + nf:base + half, NCTX:] = s
        return cos, sin
    cA, sA = tab(64)
    cB, sB = tab(32)
    return cA, sA, cB, sB


def kernel(**inputs):
    cA, sA, cB, sB = rope_tables()
    nc, S = build_program()
    g = lambda k: np.ascontiguousarray(np.asarray(inputs[k], dtype=np.float32)[0])
    shared = {
        "ada_w": g("ada_w"), "ada_b": g("ada_b"), "norm_mix": g("norm_mix"), "norm_ffn": g("norm_ffn"),
        "w_in": g("w_in"), "gqa_q_norm": g("gqa_q_norm"), "gqa_k_norm": g("gqa_k_norm"),
        "mla_q_a_norm": g("mla_q_a_norm"), "mla_kv_a_norm": g("mla_kv_a_norm"),
        "mla_w_qb": g("mla_w_qb"), "mla_w_kvb": g("mla_w_kvb"), "mla_q_norm": g("mla_q_norm"),
        "mla_k_norm": g("mla_k_norm"), "w_o_gqa": g("w_o_gqa"), "w_o_mla": g("w_o_mla"), "w_out": g("w_out"),
        "router_w": g("router_w"), "router_b": g("router_b"), "expert_w1": g("expert_w1"),
        "expert_b1": g("expert_b1"), "expert_w2": g("expert_w2"), "expert_b2": g("expert_b2"),
        "ident": np.eye(128, dtype=np.float32), "sel0": np.stack([np.ones(128, np.float32), np.zeros(128, np.float32)]), "cosA": cA, "sinA": sA, "cosB": cB, "sinB": sB,
    }
    if SPARSE:
        cst = np.zeros((128, NCONST), np.float32)
        pidx = np.arange(128, dtype=np.float32)
        cst[:, 0:32] = np.arange(32, dtype=np.float32)[None, :]
        cst[:, 32:64] = (np.arange(32, dtype=np.float32) * ECAP)[None, :]
        cst[:, 64:72] = pidx[:, None] + 128.0 * np.arange(8, dtype=np.float32)[None, :]
        cst[:, 72:76] = pidx[:, None] + 128.0 * np.arange(4, dtype=np.float32)[None, :]
        cst[:, 76:204] = (pidx[:, None] < pidx[None, :]).astype(np.float32)
        cst[:, 204:204 + NBLK] = (np.arange(NBLK, dtype=np.float32) * BLK)[None, :]
        shared["consts"] = cst
    x = np.asarray(inputs["x"], np.float32); c = np.asarray(inputs["c"], np.float32)
    ctx = np.asarray(inputs["ctx"], np.float32); c_ctx = np.asarray(inputs["c_ctx"], np.float32)
    ncores = int(os.environ.get("MK_CORES", "8"))
    in_maps = []
    for b in range(ncores):
        m = dict(shared)
        m["x"] = np.ascontiguousarray(x[b]); m["ctx"] = np.ascontiguousarray(ctx[b])
        m["cc"] = np.ascontiguousarray(np.stack([c[b], c_ctx], 0))
        in_maps.append(m)
    res = run_bass_kernel_spmd(nc, in_maps, core_ids=list(range(ncores)))
    if STAGE < 99:
        return res.results
    if ncores < 8:
        return [r["out"] for r in res.results]
    return np.stack([r["out"] for r in res.results], 0).astype(np.float32)
```
